# Optimizing a Trainium2 kernel written in Bass

```python
import math
import jax
import jax.numpy as jnp
from jax import lax
import numpy as np

D_MODEL = 1024
BATCH = 4
SEQ = 8192
DEPTH = 2

ROPE_THETA = 10000.0
NORM_EPS = 1e-6
Q_BLOCK = 128
NEG_INF = -1e30
FORCE_SCORE = 1e6

DA_HEADS = 4
DA_HEAD_DIM = 64
DA_V_DIM = 2 * DA_HEAD_DIM
SSM_HEADS = 8
SSM_HEAD_DIM = 64
SSM_D_INNER = SSM_HEADS * SSM_HEAD_DIM
SSM_GROUPS = 2
SSM_STATE = 128
SSM_CONV = 4
SSM_CHUNK = 256
SSM_CONV_CH = SSM_D_INNER + 2 * SSM_GROUPS * SSM_STATE
NSA_HEADS = 8
NSA_KV_GROUPS = 2
NSA_HEAD_DIM = 64
NSA_CMP_BLOCK = 32
NSA_CMP_STRIDE = 16
NSA_SEL_BLOCK = 64
NSA_TOP_N = 16
NSA_WINDOW = 512
NSA_KV = NSA_KV_GROUPS * NSA_HEAD_DIM
MLA_HEADS = 4
MLA_Q_RANK = 256
MLA_KV_RANK = 128
MLA_NOPE_DIM = 128
MLA_ROPE_DIM = 64
MLA_V_DIM = 128
D_FF = 2816
N_EXPERTS = 8
TOP_K = 2

EVEN_SPLITS = (DA_HEADS * 2 * DA_HEAD_DIM, DA_HEADS * 2 * DA_HEAD_DIM, DA_HEADS * DA_V_DIM,
               SSM_D_INNER, SSM_CONV_CH, SSM_HEADS)
EVEN_IN = sum(EVEN_SPLITS)
EVEN_MIX = DA_HEADS * DA_V_DIM + SSM_D_INNER
ODD_SPLITS = (NSA_HEADS * NSA_HEAD_DIM, NSA_KV, NSA_KV, NSA_KV, NSA_KV, NSA_KV, NSA_KV,
              NSA_HEADS * 3, MLA_Q_RANK, MLA_KV_RANK, MLA_ROPE_DIM)
ODD_IN = sum(ODD_SPLITS)
ODD_MIX = NSA_HEADS * NSA_HEAD_DIM + MLA_HEADS * MLA_V_DIM

kernel_name = 'hybrid_diffattn_ssd_nsa_mla_moe'


def rms_norm(t, gain):
    tf = t.astype(jnp.float32)
    y = tf * lax.rsqrt(jnp.mean(tf * tf, axis=-1, keepdims=True) + NORM_EPS)
    return (y * gain.astype(jnp.float32)).astype(t.dtype)


def split_cols(t, sizes):
    return jnp.split(t, [int(v) for v in np.cumsum(sizes)[:-1]], axis=-1)


def rope_tables(seq_len, dim):
    inv_freq = 1.0 / (ROPE_THETA ** (jnp.arange(0, dim, 2, dtype=jnp.float32) / dim))
    ang = jnp.arange(seq_len, dtype=jnp.float32)[:, None] * inv_freq[None, :]
    return jnp.cos(ang), jnp.sin(ang)


def apply_rope(t, cos, sin):
    half = t.shape[-1] // 2
    c = cos[:, None, :].astype(t.dtype)
    s = sin[:, None, :].astype(t.dtype)
    t1, t2 = t[..., :half], t[..., half:]
    return jnp.concatenate([t1 * c - t2 * s, t2 * c + t1 * s], axis=-1)


def swiglu(h, w_gate, w_up, w_down):
    return (jax.nn.silu(h @ w_gate) * (h @ w_up)) @ w_down


def causal_attention_blocked(q, k, v, scale):
    b, s, h, dq = q.shape
    nb = s // Q_BLOCK
    qb = q.reshape(b, nb, Q_BLOCK, h, dq).transpose(1, 0, 2, 3, 4)
    k_pos = jnp.arange(s)

    def one_block(args):
        q_blk, blk = args
        q_pos = blk * Q_BLOCK + jnp.arange(Q_BLOCK)
        causal = k_pos[None, :] <= q_pos[:, None]
        sc = jnp.einsum('bqhd,bkhd->bhqk', q_blk, k).astype(jnp.float32) * scale
        p = jax.nn.softmax(jnp.where(causal, sc, NEG_INF), axis=-1)
        return jnp.einsum('bhqk,bkhd->bqhd', p.astype(v.dtype), v)

    o = lax.map(one_block, (qb, jnp.arange(nb)))
    return o.transpose(1, 0, 2, 3, 4).reshape(b, s, h, v.shape[-1])


def diff_attention(q, k, v, q_gain, k_gain, lam, subln_gain, layer_idx):
    b, s, _ = q.shape
    h, d = DA_HEADS, DA_HEAD_DIM
    cos, sin = rope_tables(s, d)
    q = apply_rope(rms_norm(q.reshape(b, s, 2 * h, d), q_gain), cos, sin).reshape(b, s, h, 2, d)
    k = apply_rope(rms_norm(k.reshape(b, s, 2 * h, d), k_gain), cos, sin).reshape(b, s, h, 2, d)
    v = v.reshape(b, s, h, DA_V_DIM)
    lam_init = 0.8 - 0.6 * math.exp(-0.3 * layer_idx)
    lf = lam.astype(jnp.float32)
    lam_full = jnp.exp(jnp.sum(lf[0] * lf[1])) - jnp.exp(jnp.sum(lf[2] * lf[3])) + lam_init
    scale = d ** -0.5
    nb = s // Q_BLOCK
    qb = q.reshape(b, nb, Q_BLOCK, h, 2, d).transpose(1, 0, 2, 3, 4, 5)
    k_pos = jnp.arange(s)

    def one_block(args):
        q_blk, blk = args
        q_pos = blk * Q_BLOCK + jnp.arange(Q_BLOCK)
        causal = k_pos[None, :] <= q_pos[:, None]
        sc = jnp.einsum('bqhcd,bkhcd->bhcqk', q_blk, k).astype(jnp.float32) * scale
        p = jax.nn.softmax(jnp.where(causal, sc, NEG_INF), axis=-1)
        attn = p[:, :, 0] - lam_full * p[:, :, 1]
        return jnp.einsum('bhqk,bkhe->bqhe', attn.astype(v.dtype), v)

    o = lax.map(one_block, (qb, jnp.arange(nb)))
    o = o.transpose(1, 0, 2, 3, 4).reshape(b, s, h, DA_V_DIM)
    o = rms_norm(o, subln_gain) * (1.0 - lam_init)
    return o.reshape(b, s, h * DA_V_DIM)


def causal_depthwise_conv(t, w, bias):
    width, ch = w.shape
    y = lax.conv_general_dilated(t, w[:, None, :].astype(t.dtype), window_strides=(1,),
                                 padding=[(width - 1, 0)],
                                 dimension_numbers=('NWC', 'WIO', 'NWC'),
                                 feature_group_count=ch)
    return y + bias.astype(t.dtype)


def segsum(a):
    t = a.shape[-1]
    cs = jnp.cumsum(a, axis=-1)
    diff = cs[..., :, None] - cs[..., None, :]
    return jnp.where(jnp.tril(jnp.ones((t, t), dtype=bool)), diff, -jnp.inf)


def ssd_chunked_scan(xh, dt, a, bmat, cmat):
    b, L, h, p = xh.shape
    n = bmat.shape[-1]
    nc = L // SSM_CHUNK
    xs = (xh * dt[..., None]).reshape(b, nc, SSM_CHUNK, h, p)
    ad = (dt * a).reshape(b, nc, SSM_CHUNK, h).transpose(0, 3, 1, 2)
    bc = bmat.reshape(b, nc, SSM_CHUNK, h, n)
    cc = cmat.reshape(b, nc, SSM_CHUNK, h, n)
    a_cs = jnp.cumsum(ad, axis=-1)
    decay_in = jnp.exp(segsum(ad))
    y_diag = jnp.einsum('bclhn,bcshn,bhcls,bcshp->bclhp', cc, bc, decay_in, xs)
    decay_to_end = jnp.exp(a_cs[..., -1:] - a_cs)
    chunk_states = jnp.einsum('bclhn,bhcl,bclhp->bchpn', bc, decay_to_end, xs)
    chunk_decay = jnp.exp(a_cs[..., -1])

    def step(state, inp):
        st, dec = inp
        return state * dec[:, :, None, None] + st, state

    init = jnp.zeros((b, h, p, n), xs.dtype)
    _, prev = lax.scan(step, init, (chunk_states.transpose(1, 0, 2, 3, 4),
                                    chunk_decay.transpose(2, 0, 1)))
    prev = prev.transpose(1, 0, 2, 3, 4)
    y_off = jnp.einsum('bclhn,bchpn,bhcl->bclhp', cc, prev, jnp.exp(a_cs))
    return (y_diag + y_off).reshape(b, L, h, p)


def mamba2_ssd(z, xbc, dt_raw, conv_w, conv_b, dt_bias, a_log, d_skip, norm_gain):
    b, s, _ = z.shape
    f32 = jnp.float32
    xbc = jax.nn.silu(causal_depthwise_conv(xbc, conv_w, conv_b))
    xs, bm, cm = split_cols(xbc, (SSM_D_INNER, SSM_GROUPS * SSM_STATE, SSM_GROUPS * SSM_STATE))
    rep = SSM_HEADS // SSM_GROUPS
    xh = xs.reshape(b, s, SSM_HEADS, SSM_HEAD_DIM)
    bm = jnp.repeat(bm.reshape(b, s, SSM_GROUPS, SSM_STATE), rep, axis=2)
    cm = jnp.repeat(cm.reshape(b, s, SSM_GROUPS, SSM_STATE), rep, axis=2)
    dt = jax.nn.softplus(dt_raw.astype(f32) + dt_bias.astype(f32))
    a = -jnp.exp(a_log.astype(f32))
    pad = (-s) % SSM_CHUNK

    def padf(t):
        return jnp.pad(t.astype(f32), [(0, 0), (0, pad)] + [(0, 0)] * (t.ndim - 2))

    y = ssd_chunked_scan(padf(xh), padf(dt), a, padf(bm), padf(cm))[:, :s]
    y = y + xh.astype(f32) * d_skip.astype(f32)[:, None]
    y = y.reshape(b, s, SSM_D_INNER) * jax.nn.silu(z.astype(f32))
    y = rms_norm(y.reshape(b, s, SSM_GROUPS, SSM_D_INNER // SSM_GROUPS),
                 norm_gain.reshape(SSM_GROUPS, SSM_D_INNER // SSM_GROUPS))
    return y.reshape(b, s, SSM_D_INNER).astype(z.dtype)


def compress_blocks(t, pos_emb, w1, w2):
    b, s, g, d = t.shape
    r = NSA_CMP_BLOCK // NSA_CMP_STRIDE
    n_chunks = s // NSA_CMP_STRIDE
    n_cmp = n_chunks - r + 1
    ch = t.reshape(b, n_chunks, NSA_CMP_STRIDE, g, d)
    blocks = jnp.concatenate([ch[:, j:j + n_cmp] for j in range(r)], axis=2)
    blocks = blocks + pos_emb[None, None, :, None, :].astype(t.dtype)
    flat = blocks.transpose(0, 1, 3, 2, 4).reshape(b, n_cmp, g, NSA_CMP_BLOCK * d)
    return jax.nn.silu(flat @ w1) @ w2


def nsa_overlap(n_cmp, n_sel):
    c_start = np.arange(n_cmp)[:, None] * NSA_CMP_STRIDE
    s_start = np.arange(n_sel)[None, :] * NSA_SEL_BLOCK
    hit = (c_start < s_start + NSA_SEL_BLOCK) & (c_start + NSA_CMP_BLOCK > s_start)
    return jnp.asarray(hit.astype(np.float32))


def _take_blocks(blocks, idx):
    return blocks[idx]


_gather_blocks = jax.vmap(jax.vmap(_take_blocks))


def nsa_attention(q, kc, vc, ksl, vsl, kwn, vwn, gate_logits, q_gain, k_gain, cmp_pos, cmp_w1, cmp_w2):
    b, s, _ = q.shape
    h, g, d = NSA_HEADS, NSA_KV_GROUPS, NSA_HEAD_DIM
    r = h // g
    scale = d ** -0.5
    cos, sin = rope_tables(s, d)
    q = apply_rope(rms_norm(q.reshape(b, s, h, d), q_gain), cos, sin).reshape(b, s, g, r, d)
    n_cmp = s // NSA_CMP_STRIDE - NSA_CMP_BLOCK // NSA_CMP_STRIDE + 1
    cmp_end = jnp.arange(n_cmp) * NSA_CMP_STRIDE + NSA_CMP_BLOCK - 1
    ck = compress_blocks(kc.reshape(b, s, g, d), cmp_pos[0], cmp_w1[0], cmp_w2[0])
    ck = apply_rope(rms_norm(ck, k_gain[0]), cos[cmp_end], sin[cmp_end])
    cv = compress_blocks(vc.reshape(b, s, g, d), cmp_pos[1], cmp_w1[1], cmp_w2[1])
    n_sel = s // NSA_SEL_BLOCK
    n_top = min(NSA_TOP_N, n_sel)
    ks_full = apply_rope(rms_norm(ksl.reshape(b, s, g, d), k_gain[1]), cos, sin)
    ks_blocks = ks_full.reshape(b, n_sel, NSA_SEL_BLOCK, g, d).transpose(0, 3, 1, 2, 4)
    vs_blocks = vsl.reshape(b, n_sel, NSA_SEL_BLOCK, g, d).transpose(0, 3, 1, 2, 4)
    overlap = nsa_overlap(n_cmp, n_sel)
    sel_idx = jnp.arange(n_sel)
    sel_start = sel_idx * NSA_SEL_BLOCK
    kw = apply_rope(rms_norm(kwn.reshape(b, s, g, d), k_gain[2]), cos, sin)
    pad_w = ((0, 0), (NSA_WINDOW, 0), (0, 0), (0, 0))
    kw_pad = jnp.pad(kw, pad_w)
    vw_pad = jnp.pad(vwn.reshape(b, s, g, d), pad_w)
    gates = jax.nn.sigmoid(gate_logits.astype(jnp.float32)).reshape(b, s, g, r, 3)
    nb = s // Q_BLOCK
    qb = q.reshape(b, nb, Q_BLOCK, g, r, d).transpose(1, 0, 2, 3, 4, 5)
    gb = gates.reshape(b, nb, Q_BLOCK, g, r, 3).transpose(1, 0, 2, 3, 4, 5)

    def one_block(args):
        q_blk, g_blk, blk = args
        q_pos = blk * Q_BLOCK + jnp.arange(Q_BLOCK)
        valid_c = cmp_end[None, :] <= q_pos[:, None]
        s_c = jnp.einsum('bqgrd,bngd->bgrqn', q_blk, ck).astype(jnp.float32) * scale
        p_c = jax.nn.softmax(jnp.where(valid_c, s_c, NEG_INF), axis=-1) * valid_c
        o_c = jnp.einsum('bgrqn,bngd->bqgrd', p_c.astype(cv.dtype), cv)
        imp = jnp.einsum('bgrqn,nj->bgqj', p_c, overlap)
        cur = (q_pos // NSA_SEL_BLOCK)[:, None]
        forced = (sel_idx[None, :] == 0) | (sel_idx[None, :] == cur) | (sel_idx[None, :] == cur - 1)
        future = sel_start[None, :] > q_pos[:, None]
        imp = jnp.where(future, -FORCE_SCORE, jnp.where(forced, FORCE_SCORE, imp))
        _, idx = lax.top_k(imp, n_top)
        k_sel = _gather_blocks(ks_blocks, idx).reshape(b, g, Q_BLOCK, n_top * NSA_SEL_BLOCK, d)
        v_sel = _gather_blocks(vs_blocks, idx).reshape(b, g, Q_BLOCK, n_top * NSA_SEL_BLOCK, d)
        pos_sel = (idx[..., None] * NSA_SEL_BLOCK + jnp.arange(NSA_SEL_BLOCK)).reshape(
            b, g, Q_BLOCK, n_top * NSA_SEL_BLOCK)
        valid_s = pos_sel <= q_pos[None, None, :, None]
        s_s = jnp.einsum('bqgrd,bgqkd->bgrqk', q_blk, k_sel).astype(jnp.float32) * scale
        p_s = jax.nn.softmax(jnp.where(valid_s[:, :, None], s_s, NEG_INF), axis=-1)
        o_s = jnp.einsum('bgrqk,bgqkd->bqgrd', p_s.astype(v_sel.dtype), v_sel)
        start = blk * Q_BLOCK
        k_win = lax.dynamic_slice_in_dim(kw_pad, start, NSA_WINDOW + Q_BLOCK, axis=1)
        v_win = lax.dynamic_slice_in_dim(vw_pad, start, NSA_WINDOW + Q_BLOCK, axis=1)
        pos_w = start - NSA_WINDOW + jnp.arange(NSA_WINDOW + Q_BLOCK)
        valid_w = ((pos_w[None, :] <= q_pos[:, None]) & (pos_w[None, :] > q_pos[:, None] - NSA_WINDOW)
                   & (pos_w[None, :] >= 0))
        s_w = jnp.einsum('bqgrd,bkgd->bgrqk', q_blk, k_win).astype(jnp.float32) * scale
        p_w = jax.nn.softmax(jnp.where(valid_w, s_w, NEG_INF), axis=-1)
        o_w = jnp.einsum('bgrqk,bkgd->bqgrd', p_w.astype(v_win.dtype), v_win)
        out = g_blk[..., 0:1] * o_c + g_blk[..., 1:2] * o_s + g_blk[..., 2:3] * o_w
        return out.astype(q_blk.dtype)

    o = lax.map(one_block, (qb, gb, jnp.arange(nb)))
    return o.transpose(1, 0, 2, 3, 4, 5).reshape(b, s, h * d)


def mla_attention(c_q, c_kv, k_rope, cq_gain, ckv_gain, w_uq, w_ukv, qn_gain, qr_gain, kn_gain, kr_gain):
    b, s, _ = c_q.shape
    h = MLA_HEADS
    cos, sin = rope_tables(s, MLA_ROPE_DIM)
    q = (rms_norm(c_q, cq_gain) @ w_uq).reshape(b, s, h, MLA_NOPE_DIM + MLA_ROPE_DIM)
    kv = (rms_norm(c_kv, ckv_gain) @ w_ukv).reshape(b, s, h, MLA_NOPE_DIM + MLA_V_DIM)
    q_nope = rms_norm(q[..., :MLA_NOPE_DIM], qn_gain)
    q_rope = apply_rope(rms_norm(q[..., MLA_NOPE_DIM:], qr_gain), cos, sin)
    k_nope = rms_norm(kv[..., :MLA_NOPE_DIM], kn_gain)
    v = kv[..., MLA_NOPE_DIM:]
    k_r = apply_rope(rms_norm(k_rope.reshape(b, s, 1, MLA_ROPE_DIM), kr_gain), cos, sin)
    q_full = jnp.concatenate([q_nope, q_rope], axis=-1)
    k_full = jnp.concatenate([k_nope, jnp.broadcast_to(k_r, (b, s, h, MLA_ROPE_DIM))], axis=-1)
    o = causal_attention_blocked(q_full, k_full, v, (MLA_NOPE_DIM + MLA_ROPE_DIM) ** -0.5)
    return o.reshape(b, s, h * MLA_V_DIM)


def moe_swiglu(h, router, w_gate, w_up, w_down):
    b, s, d = h.shape
    t = h.reshape(b * s, d)
    logits = (t @ router).astype(jnp.float32)
    top_val, top_idx = lax.top_k(logits, TOP_K)
    top_w = jax.nn.softmax(top_val, axis=-1)
    gates = jnp.sum(jax.nn.one_hot(top_idx, N_EXPERTS, dtype=jnp.float32) * top_w[..., None], axis=1)
    out = jnp.zeros_like(t)
    for e in range(N_EXPERTS):
        out = out + gates[:, e:e + 1].astype(t.dtype) * swiglu(t, w_gate[e], w_up[e], w_down[e])
    return out.reshape(b, s, d)


def setup_inputs(seed: int = 0) -> dict:
    key = jax.random.key(seed)
    ks = jax.random.split(key, 40)
    f32 = jnp.float32
    n_ev = (DEPTH + 1) // 2
    n_od = DEPTH // 2

    def nrm(k, shape, scale):
        return jax.random.normal(k, shape, f32) * scale

    def gain(k, shape):
        return 1.0 + 0.1 * jax.random.normal(k, shape, f32)

    dt0 = jnp.exp(jax.random.uniform(ks[9], (n_ev, SSM_HEADS), f32, math.log(1e-3), math.log(1e-1)))
    return {
        'x': jax.random.normal(ks[0], (BATCH, SEQ, D_MODEL), f32),
        'ev_norm_mix': gain(ks[1], (n_ev, D_MODEL)),
        'ev_w_in': nrm(ks[2], (n_ev, D_MODEL, EVEN_IN), D_MODEL ** -0.5),
        'da_q_gain': gain(ks[3], (n_ev, DA_HEAD_DIM)),
        'da_k_gain': gain(ks[4], (n_ev, DA_HEAD_DIM)),
        'da_lambda': nrm(ks[5], (n_ev, 4, DA_HEAD_DIM), 0.1),
        'da_subln_gain': gain(ks[6], (n_ev, DA_V_DIM)),
        'ssm_conv_w': nrm(ks[7], (n_ev, SSM_CONV, SSM_CONV_CH), SSM_CONV ** -0.5),
        'ssm_conv_b': nrm(ks[8], (n_ev, SSM_CONV_CH), 0.02),
        'ssm_dt_bias': dt0 + jnp.log(-jnp.expm1(-dt0)),
        'ssm_a_log': jnp.log(jax.random.uniform(ks[10], (n_ev, SSM_HEADS), f32, 1.0, 16.0)),
        'ssm_d': gain(ks[11], (n_ev, SSM_HEADS)),
        'ssm_norm_gain': gain(ks[12], (n_ev, SSM_D_INNER)),
        'ev_w_out': nrm(ks[13], (n_ev, EVEN_MIX, D_MODEL), EVEN_MIX ** -0.5),
        'ev_norm_ffn': gain(ks[14], (n_ev, D_MODEL)),
        'ffn_w_gate': nrm(ks[15], (n_ev, D_MODEL, D_FF), D_MODEL ** -0.5),
        'ffn_w_up': nrm(ks[16], (n_ev, D_MODEL, D_FF), D_MODEL ** -0.5),
        'ffn_w_down': nrm(ks[17], (n_ev, D_FF, D_MODEL), D_FF ** -0.5),
        'od_norm_mix': gain(ks[18], (n_od, D_MODEL)),
        'od_w_in': nrm(ks[19], (n_od, D_MODEL, ODD_IN), D_MODEL ** -0.5),
        'nsa_q_gain': gain(ks[20], (n_od, NSA_HEAD_DIM)),
        'nsa_k_gain': gain(ks[21], (n_od, 3, NSA_HEAD_DIM)),
        'nsa_cmp_pos': nrm(ks[22], (n_od, 2, NSA_CMP_BLOCK, NSA_HEAD_DIM), 0.1),
        'nsa_cmp_w1': nrm(ks[23], (n_od, 2, NSA_CMP_BLOCK * NSA_HEAD_DIM, NSA_HEAD_DIM),
                          (NSA_CMP_BLOCK * NSA_HEAD_DIM) ** -0.5),
        'nsa_cmp_w2': nrm(ks[24], (n_od, 2, NSA_HEAD_DIM, NSA_HEAD_DIM), NSA_HEAD_DIM ** -0.5),
        'mla_cq_gain': gain(ks[25], (n_od, MLA_Q_RANK)),
        'mla_ckv_gain': gain(ks[26], (n_od, MLA_KV_RANK)),
        'mla_w_uq': nrm(ks[27], (n_od, MLA_Q_RANK, MLA_HEADS * (MLA_NOPE_DIM + MLA_ROPE_DIM)), MLA_Q_RANK ** -0.5),
        'mla_w_ukv': nrm(ks[28], (n_od, MLA_KV_RANK, MLA_HEADS * (MLA_NOPE_DIM + MLA_V_DIM)), MLA_KV_RANK ** -0.5),
        'mla_qn_gain': gain(ks[29], (n_od, MLA_NOPE_DIM)),
        'mla_qr_gain': gain(ks[30], (n_od, MLA_ROPE_DIM)),
        'mla_kn_gain': gain(ks[31], (n_od, MLA_NOPE_DIM)),
        'mla_kr_gain': gain(ks[32], (n_od, MLA_ROPE_DIM)),
        'od_w_out': nrm(ks[33], (n_od, ODD_MIX, D_MODEL), ODD_MIX ** -0.5),
        'od_norm_ffn': gain(ks[34], (n_od, D_MODEL)),
        'moe_router': nrm(ks[35], (n_od, D_MODEL, N_EXPERTS), D_MODEL ** -0.5),
        'moe_w_gate': nrm(ks[36], (n_od, N_EXPERTS, D_MODEL, D_FF), D_MODEL ** -0.5),
        'moe_w_up': nrm(ks[37], (n_od, N_EXPERTS, D_MODEL, D_FF), D_MODEL ** -0.5),
        'moe_w_down': nrm(ks[38], (n_od, N_EXPERTS, D_FF, D_MODEL), D_FF ** -0.5),
    }


def reference(x, ev_norm_mix, ev_w_in, da_q_gain, da_k_gain, da_lambda, da_subln_gain,
              ssm_conv_w, ssm_conv_b, ssm_dt_bias, ssm_a_log, ssm_d, ssm_norm_gain,
              ev_w_out, ev_norm_ffn, ffn_w_gate, ffn_w_up, ffn_w_down,
              od_norm_mix, od_w_in, nsa_q_gain, nsa_k_gain, nsa_cmp_pos, nsa_cmp_w1, nsa_cmp_w2,
              mla_cq_gain, mla_ckv_gain, mla_w_uq, mla_w_ukv, mla_qn_gain, mla_qr_gain,
              mla_kn_gain, mla_kr_gain, od_w_out, od_norm_ffn,
              moe_router, moe_w_gate, moe_w_up, moe_w_down):
    for layer in range(DEPTH):
        i = layer // 2
        if layer % 2 == 0:
            h = rms_norm(x, ev_norm_mix[i])
            q, k, v, z, xbc, dt = split_cols(h @ ev_w_in[i], EVEN_SPLITS)
            a_out = diff_attention(q, k, v, da_q_gain[i], da_k_gain[i], da_lambda[i],
                                   da_subln_gain[i], layer)
            b_out = mamba2_ssd(z, xbc, dt, ssm_conv_w[i], ssm_conv_b[i], ssm_dt_bias[i],
                               ssm_a_log[i], ssm_d[i], ssm_norm_gain[i])
            x = x + jnp.concatenate([a_out, b_out], axis=-1) @ ev_w_out[i]
            x = x + swiglu(rms_norm(x, ev_norm_ffn[i]), ffn_w_gate[i], ffn_w_up[i], ffn_w_down[i])
        else:
            h = rms_norm(x, od_norm_mix[i])
            q, kc, vc, ksl, vsl, kwn, vwn, gl, cq, ckv, kr = split_cols(h @ od_w_in[i], ODD_SPLITS)
            c_out = nsa_attention(q, kc, vc, ksl, vsl, kwn, vwn, gl, nsa_q_gain[i], nsa_k_gain[i],
                                  nsa_cmp_pos[i], nsa_cmp_w1[i], nsa_cmp_w2[i])
            d_out = mla_attention(cq, ckv, kr, mla_cq_gain[i], mla_ckv_gain[i], mla_w_uq[i],
                                  mla_w_ukv[i], mla_qn_gain[i], mla_qr_gain[i], mla_kn_gain[i],
                                  mla_kr_gain[i])
            x = x + jnp.concatenate([c_out, d_out], axis=-1) @ od_w_out[i]
            x = x + moe_swiglu(rms_norm(x, od_norm_ffn[i]), moe_router[i], moe_w_gate[i],
                               moe_w_up[i], moe_w_down[i])
    return x
```

```python
from concourse.bass_utils import run_bass_kernel_spmd

from contextlib import ExitStack
import numpy as np
import concourse.bass as bass
import concourse.mybir as mybir

F32 = mybir.dt.float32
BF16 = mybir.dt.bfloat16
AF = mybir.ActivationFunctionType
ALU = mybir.AluOpType
AX = mybir.AxisListType

ENGS = ("pe", "dve", "act", "pool", "sp")
NRING = 8


class Buf:
    __slots__ = ("t", "lw", "rd", "name")

    def __init__(self, t, name=""):
        self.t = t
        self.lw = None
        self.rd = []
        self.name = name

    def __getitem__(self, k):
        return self.t[k]


class Op:
    __slots__ = ("eng", "fn", "deps", "sig", "idx", "dma", "ring", "ruse", "cc")

    def __init__(self, eng, fn, dma=False):
        self.eng = eng
        self.fn = fn
        self.deps = []
        self.sig = False
        self.idx = None
        self.dma = dma
        self.ring = None
        self.ruse = None
        self.cc = False


class Prog:
    def __init__(self, nc, same_engine_sync=True):
        self.nc = nc
        self.es = ExitStack()
        self.pes = None
        self.ops = {e: [] for e in ENGS}
        self.same = same_engine_sync
        self.nbuf = 0
        self.tag = "p0"
        self.persist = []
        self.sem = {e: self.es.enter_context(nc.semaphore(f"s_{e}")) for e in ENGS}
        self.ring = {"sp": [self.es.enter_context(nc.semaphore(f"r_sp{i}")) for i in range(NRING)]}
        self.ccsem = self.es.enter_context(nc.semaphore("s_cc"))
        self.cnt = {e: 0 for e in ENGS}
        self.nd = {e: 0 for e in ENGS}
        self.ncc = 0
        self.waited = {e: {} for e in ENGS}
        self.engh = {"pe": nc.tensor, "dve": nc.vector, "act": nc.scalar, "pool": nc.gpsimd, "sp": nc.sync}

    def begin_phase(self, tag):
        self.tag = tag
        self.pes = ExitStack()

    def end_phase(self):
        self._emit_ops()
        self._barrier()
        self.pes.close()
        self.pes = None
        for b in self.persist:
            b.lw = None
            b.rd = []

    def sb(self, shape, dt=F32, name=None):
        self.nbuf += 1
        name = f"{self.tag}_{name or 'sb'}_{self.nbuf}"
        t = self.pes.enter_context(self.nc.sbuf_tensor(name, list(shape), dt))
        return Buf(t, name)

    def ps(self, shape, dt=F32, name=None):
        self.nbuf += 1
        name = f"{self.tag}_{name or 'ps'}_{self.nbuf}"
        t = self.pes.enter_context(self.nc.psum_tensor(name, list(shape), dt))
        return Buf(t, name)

    def sub(self, ap, name=""):
        return Buf(ap, name)

    def dram(self, ap, name=""):
        b = Buf(ap, name)
        self.persist.append(b)
        return b

    def op(self, eng, fn, reads=(), writes=(), dma=False, cc=False):
        o = Op(eng, fn, dma)
        o.cc = cc
        deps = []
        for b in reads:
            if b.lw is not None:
                deps.append(b.lw)
        for b in writes:
            if b.lw is not None:
                deps.append(b.lw)
            deps.extend(b.rd)
        seen = set()
        for d in deps:
            if id(d) in seen or d is o:
                continue
            seen.add(id(d))
            if d.eng == eng and not d.dma and not d.cc:
                if eng == "pe" or not self.same:
                    continue
            d.sig = True
            o.deps.append(d)
        for b in reads:
            b.rd.append(o)
        for b in writes:
            b.lw = o
            b.rd = []
        if dma or cc:
            o.sig = True
        self.ops[eng].append(o)
        return o

    def mm(self, out, lhsT, rhs, start, stop, reads, writes):
        return self.op("pe", lambda e: e.matmul(out, lhsT=lhsT, rhs=rhs, start=start, stop=stop), reads, writes)

    def tr(self, out, in_, ident, reads, writes):
        return self.op("pe", lambda e: e.transpose(out=out, in_=in_, identity=ident), reads, writes)

    def actv(self, out, in_, func, reads, writes, **kw):
        return self.op("act", lambda e: e.activation(out=out, in_=in_, func=func, **kw), reads, writes)

    def cp(self, eng, out, in_, reads, writes):
        if eng == "act":
            return self.op("act", lambda e: e.copy(out=out, in_=in_), reads, writes)
        return self.op(eng, lambda e: e.tensor_copy(out=out, in_=in_), reads, writes)

    def tt(self, eng, out, in0, in1, op, reads, writes):
        return self.op(eng, lambda e: e.tensor_tensor(out=out, in0=in0, in1=in1, op=op), reads, writes)

    def ts(self, eng, out, in0, s1, op0, reads, writes, s2=None, op1=None, **kw):
        if op1 is None:
            return self.op(eng, lambda e: e.tensor_scalar(out=out, in0=in0, scalar1=s1, scalar2=None, op0=op0, **kw), reads, writes)
        return self.op(eng, lambda e: e.tensor_scalar(out=out, in0=in0, scalar1=s1, scalar2=s2, op0=op0, op1=op1, **kw), reads, writes)

    def stt(self, eng, out, in0, scalar, in1, op0, op1, reads, writes):
        return self.op(eng, lambda e: e.scalar_tensor_tensor(out=out, in0=in0, scalar=scalar, in1=in1, op0=op0, op1=op1), reads, writes)

    def dma(self, out, in_, reads=(), writes=(), eng="sp", **kw):
        return self.op(eng, lambda e: e.dma_start(out=out, in_=in_, **kw), reads, writes, dma=True)

    def _emit_ops(self):
        for e in ENGS:
            ops = self.ops[e]
            if ops and not ops[-1].dma and not ops[-1].cc:
                ops[-1].sig = True
            for o in ops:
                if o.dma:
                    o.ring = self.nd[e] % NRING
                    o.ruse = self.nd[e] // NRING
                    self.nd[e] += 1
                elif o.cc:
                    self.ncc += 1
                    o.idx = self.ncc
                elif o.sig:
                    self.cnt[e] += 1
                    o.idx = self.cnt[e]
        for e in ENGS:
            h = self.engh[e]
            waited = self.waited[e]
            for o in self.ops[e]:
                need = {}
                for d in o.deps:
                    if d.dma:
                        key = ("r", d.eng, d.ring)
                        val = 16 * (d.ruse + 1)
                    elif d.cc:
                        key = ("c",)
                        val = d.idx
                    else:
                        key = ("s", d.eng)
                        val = d.idx
                    if need.get(key, 0) < val:
                        need[key] = val
                if o.dma and o.ruse > 0:
                    key = ("r", e, o.ring)
                    val = 16 * o.ruse
                    if need.get(key, 0) < val:
                        need[key] = val
                for key, val in need.items():
                    if waited.get(key, 0) >= val:
                        continue
                    waited[key] = val
                    h.wait_ge(self._semof(key), val)
                ins = o.fn(h)
                if o.dma:
                    ins.then_inc(self.ring[e][o.ring], 16)
                elif o.cc:
                    ins.then_inc(self.ccsem)
                elif o.sig:
                    ins.then_inc(self.sem[e], 1)
            self.ops[e] = []

    def _semof(self, key):
        if key[0] == "s":
            return self.sem[key[1]]
        if key[0] == "c":
            return self.ccsem
        return self.ring[key[1]][key[2]]

    def _barrier(self):
        targets = {}
        for e in ENGS:
            if self.cnt[e] > 0:
                targets[("s", e)] = self.cnt[e]
        for q in self.ring:
            for r in range(NRING):
                n = len(range(r, self.nd[q], NRING))
                if n:
                    targets[("r", q, r)] = 16 * n
        if self.ncc:
            targets[("c",)] = self.ncc
        for e in ENGS:
            h = self.engh[e]
            waited = self.waited[e]
            for key, val in targets.items():
                if key == ("s", e) or waited.get(key, 0) >= val:
                    continue
                waited[key] = val
                h.wait_ge(self._semof(key), val)


D = 1024
DFF = 2816
NFG = 11
EPS = 1e-6


def emit_ffn(ctx, n_exp, ntok=4096):
    moe = n_exp > 1
    nc, P, tag = ctx.nc, ctx.P, ctx.tag
    NHALF = 2
    HT = ntok // NHALF
    NT = HT // 128
    NTB = HT // 512
    xin = ctx.xin
    Gd = ctx.G
    S_ = ctx.S
    sel_d = ctx.sel_d
    wout_d = nc.dram_tensor(tag + "wout_l", [128, 8 * D], F32, kind="ExternalInput").ap()
    gain_d = nc.dram_tensor(tag + "gain_l", [128, 8], F32, kind="ExternalInput").ap()
    ident_d = nc.dram_tensor(tag + "ident", [128, 128], F32, kind="ExternalInput").ap()
    wg_d = nc.dram_tensor(tag + "wg_l", [n_exp * NFG, 128, 2048], F32, kind="ExternalInput").ap()
    wu_d = nc.dram_tensor(tag + "wu_l", [n_exp * NFG, 128, 2048], F32, kind="ExternalInput").ap()
    wd_d = nc.dram_tensor(tag + "wd_l", [n_exp * NFG, 128, 2048], F32, kind="ExternalInput").ap()
    if moe:
        router_d = nc.dram_tensor(tag + "router_l", [128, 8 * 8], F32, kind="ExternalInput").ap()
    y = ctx.out

    identf = P.sb([128, 128], F32, "identf")
    identb = P.sb([128, 128], BF16, "identb")
    gain = P.sb([128, 8], F32, "gain")
    P.dma(identf[:], ident_d, writes=[identf])
    P.cp("dve", identb[:], identf[:], [identf], [identb])
    P.dma(gain[:], gain_d, writes=[gain])
    if moe:
        router = P.sb([128, 8, 8], F32, "router")
        P.dma(router[:].rearrange("p c e -> p (c e)"), router_d, writes=[router])

    sel = P.sb([128, 2], F32, "sel")
    P.dma(sel[:], sel_d, writes=[sel])
    acc_t = P.sb([128, NT, D], F32, "acc")
    acc = [P.sub(acc_t[:, t, :], f"acc{t}") for t in range(NT)]
    hT_t = P.sb([128, 8, HT], BF16, "hT")
    hT = [P.sub(hT_t[:, :, tb * 512:(tb + 1) * 512], f"hT{tb}") for tb in range(NTB)]
    gates_t = P.sb([128, NT, 8], F32, "gates")
    gates = [P.sub(gates_t[:, t, :], f"gates{t}") for t in range(NT)]
    wbuf = [P.sb([128, 6144], BF16, f"wbuf{i}") for i in range(2)]
    wgb = [P.sub(wbuf[i][:, 0:2048], f"wg{i}") for i in range(2)]
    wub = [P.sub(wbuf[i][:, 2048:4096], f"wu{i}") for i in range(2)]
    wdb = [P.sub(wbuf[i][:, 4096:6144], f"wd{i}") for i in range(2)]
    stage = [P.sb([128, 2048], F32, f"stage{i}") for i in range(3)]
    woutb = P.sb([128, 8, D], BF16, "woutb")
    actT = [[P.sb([128, 512], BF16, f"actT{i}{j}") for j in range(2)] for i in range(2)]
    sil = [P.sb([128, 512], BF16, f"sil{j}") for j in range(2)]
    X = [P.sb([128, D], F32, f"X{i}") for i in range(2)]
    M = [P.sb([128, D], F32, f"M{i}") for i in range(1)] * 2
    C0 = P.sb([128, D], F32, "C0")
    C1 = P.sb([128, D], F32, "C1")
    MB = P.sb([128, D], BF16, "MB")
    mixT = P.sb([128, 8, 128], BF16, "mixT")
    ss = P.sb([128, 1], F32, "ss")
    xnb = P.sb([128, D], BF16, "xnb")
    if moe:
        xn32 = P.sb([128, D], F32, "xn32")
        hT32 = P.sb([128, 8, 128], F32, "hT32")
        lg = P.sb([128, 8], F32, "lg")
        top8 = P.sb([128, 8], F32, "top8")
        msk = P.sb([128, 8], F32, "msk")
        wexp = P.sb([128, 8], F32, "wexp")
        den = P.sb([128, 1], F32, "den")
        negv1 = P.sb([128, 1], F32, "negv1")

    pg = [P.ps([128, 512], F32, f"pg{j}") for j in range(2)]
    pu = [P.ps([128, 512], F32, f"pu{j}") for j in range(2)]
    pd = [P.ps([128, 512], F32, f"pd{j}") for j in range(2)]
    ptr = P.ps([128, 8, 128], BF16, "ptr")
    pm = P.ps([128, 512], F32, "pm")

    for q in range(4):
        st = stage[q % 3]
        P.dma(st[:], wout_d[:, q * 2048:(q + 1) * 2048], writes=[st])
        P.cp("pool", woutb[:].rearrange("p c n -> p (c n)")[:, q * 2048:(q + 1) * 2048], st[:], [st], [woutb])

    gidx = 0
    for half in range(NHALF):
        r0 = half * HT
        for t in range(NT):
            Xt, Mt = X[t % 2], M[t % 2]
            rows = slice(r0 + t * 128, r0 + (t + 1) * 128)
            P.dma(Xt[:], xin[rows, :], writes=[Xt])
            rr = r0 + t * 128
            for p_ in range(2):
                a0 = ctx.gaddr(p_, rr)
                a1 = ctx.gaddr(p_, ntok + rr)
                P.dma(C0[:, p_ * 512:(p_ + 1) * 512], Gd[a0:a0 + 128, :], writes=[C0])
                P.dma(C1[:, p_ * 512:(p_ + 1) * 512], Gd[a1:a1 + 128, :], writes=[C1])
            P.ts("dve", Mt[:], C0[:], sel[:, 0:1], ALU.mult, [C0, sel], [Mt])
            P.stt("dve", Mt[:], C1[:], sel[:, 1:2], Mt[:], ALU.mult, ALU.add, [C1, sel, Mt], [Mt])
            P.cp("act", MB[:], Mt[:], [Mt], [MB])
            for c in range(8):
                P.tr(ptr[:, c, :], MB[:, c * 128:(c + 1) * 128], identb[:], [MB, identb], [ptr])
            P.cp("dve", mixT[:], ptr[:], [ptr], [mixT])
            A = acc[t]
            for hf in range(2):
                po = pd[hf]
                for c in range(8):
                    P.mm(po[:], mixT[:, c, :], woutb[:, c, hf * 512:(hf + 1) * 512], c == 0, c == 7,
                         [mixT, woutb], [po])
                P.tt("dve", A[:, hf * 512:(hf + 1) * 512], Xt[:, hf * 512:(hf + 1) * 512], po[:], ALU.add,
                     [Xt, po], [A])
            P.actv(Mt[:], A[:], AF.Square, [A], [Mt, ss], accum_out=ss[:])
            P.actv(ss[:], ss[:], AF.Ln, [ss], [ss], scale=1.0 / D, bias=EPS)
            P.actv(ss[:], ss[:], AF.Exp, [ss], [ss], scale=-0.5)
            P.ts("dve", xnb[:], A[:], ss[:, 0:1], ALU.mult, [A, ss], [xnb])
            for c in range(8):
                P.tr(ptr[:, c, :], xnb[:, c * 128:(c + 1) * 128], identb[:], [xnb, identb], [ptr])
            H = hT[t // 4]
            tok = slice((t % 4) * 128, (t % 4 + 1) * 128)
            P.tt("dve", H[:, :, tok], ptr[:], gain[:].unsqueeze(2).to_broadcast([128, 8, 128]), ALU.mult,
                 [ptr, gain], [H])
            if moe:
                G = gates[t]
                P.ts("dve", xn32[:], A[:], ss[:, 0:1], ALU.mult, [A, ss], [xn32])
                for rnd in range(2):
                    for c4 in range(4):
                        c = rnd * 4 + c4
                        P.tr(pm[:, c4 * 128:(c4 + 1) * 128], xn32[:, c * 128:(c + 1) * 128], identf[:],
                             [xn32, identf], [pm])
                    P.tt("dve", hT32[:, rnd * 4:(rnd + 1) * 4, :], pm[:].rearrange("p (c t) -> p c t", c=4),
                         gain[:, rnd * 4:(rnd + 1) * 4].unsqueeze(2).to_broadcast([128, 4, 128]), ALU.mult,
                         [pm, gain], [hT32])
                for c in range(8):
                    P.mm(pm[:, 0:8], hT32[:, c, :], router[:, c, :], c == 0, c == 7, [hT32, router], [pm])
                P.cp("dve", lg[:], pm[:, 0:8], [pm], [lg])
                P.op("dve", lambda e: e.max(out=top8[:], in_=lg[:]), [lg], [top8])
                P.ts("dve", msk[:], lg[:], top8[:, 1:2], ALU.is_ge, [lg, top8], [msk])
                P.ts("dve", negv1[:], top8[:, 0:1], -1.0, ALU.mult, [top8], [negv1])
                P.actv(wexp[:], lg[:], AF.Exp, [lg, negv1], [wexp], bias=negv1[:, 0:1], scale=1.0)
                P.tt("dve", wexp[:], wexp[:], msk[:], ALU.mult, [wexp, msk], [wexp])
                P.op("dve", lambda e: e.reduce_sum(out=den[:], in_=wexp[:], axis=AX.X), [wexp], [den])
                P.op("dve", lambda e: e.reciprocal(out=den[:], in_=den[:]), [den], [den])
                P.ts("dve", G[:], wexp[:], den[:, 0:1], ALU.mult, [wexp, den], [G])

        pend = None

        def emit_down(job):
            k, e, tb, AT = job
            for tt_ in range(4):
                t = tb * 4 + tt_
                A = acc[t]
                for hf in range(2):
                    po = pd[hf]
                    for j in range(2):
                        P.mm(po[:], AT[j][:, tt_ * 128:(tt_ + 1) * 128],
                             wdb[k][:, j * 1024 + hf * 512: j * 1024 + (hf + 1) * 512], j == 0, j == 1,
                             [AT[j], wdb[k]], [po])
                    dst = A[:, hf * 512:(hf + 1) * 512]
                    if moe:
                        P.stt("dve", dst, po[:], gates[t][:, e:e + 1], dst, ALU.mult, ALU.add,
                              [po, gates[t], A], [A])
                    else:
                        P.tt("dve", dst, dst, po[:], ALU.add, [po, A], [A])
                    yield

        it = 0
        for e in range(n_exp):
            for fg in range(NFG):
                k = gidx % 2
                g = e * NFG + fg
                for wi, (src, dstb, ceng) in enumerate(((wg_d, wgb[k], "pool"), (wu_d, wub[k], "pool"),
                                                        (wd_d, wdb[k], "act"))):
                    st = stage[(gidx * 3 + wi) % 3]
                    P.dma(st[:], src[g], writes=[st])
                    P.cp(ceng, dstb[:], st[:], [st], [dstb])
                gidx += 1
                for tb in range(NTB):
                    AT = actT[it % 2]
                    it += 1
                    H = hT[tb]
                    dgen = emit_down(pend) if pend is not None else iter(())
                    nmm = 0
                    for j in range(2):
                        for c in range(8):
                            P.mm(pg[j][:], wgb[k][:, c * 256 + j * 128: c * 256 + (j + 1) * 128], H[:, c, :],
                                 c == 0, c == 7, [wgb[k], H], [pg[j]])
                            nmm += 1
                            if nmm % 4 == 0:
                                next(dgen, None)
                        for c in range(8):
                            P.mm(pu[j][:], wub[k][:, c * 256 + j * 128: c * 256 + (j + 1) * 128], H[:, c, :],
                                 c == 0, c == 7, [wub[k], H], [pu[j]])
                            nmm += 1
                            if nmm % 4 == 0:
                                next(dgen, None)
                        P.actv(sil[j][:], pg[j][:], AF.Silu, [pg[j]], [sil[j]])
                        P.tt("dve", AT[j][:], sil[j][:], pu[j][:], ALU.mult, [sil[j], pu[j]], [AT[j]])
                    for _ in dgen:
                        pass
                    pend = (k, e, tb, AT)
        for _ in emit_down(pend):
            pass
        pend = None
        for t in range(NT):
            P.dma(y[r0 + t * 128: r0 + (t + 1) * 128, :], acc[t][:], reads=[acc[t]])
    return


def ffn_layouts(w_out, gain, w_gate, w_up, w_down, router=None):
    E = w_gate.shape[0]
    d = {}
    d["wout_l"] = np.ascontiguousarray(w_out.reshape(8, 128, D).transpose(1, 0, 2).reshape(128, 8 * D))
    d["gain_l"] = np.ascontiguousarray(gain.reshape(8, 128).T)
    d["ident"] = np.eye(128, dtype=np.float32)

    def gu(w):
        return np.ascontiguousarray(
            w.reshape(E, 8, 128, NFG, 256).transpose(0, 3, 2, 1, 4).reshape(E * NFG, 128, 2048))
    d["wg_l"] = gu(w_gate)
    d["wu_l"] = gu(w_up)
    d["wd_l"] = np.ascontiguousarray(
        w_down.reshape(E, NFG, 2, 128, D).transpose(0, 1, 3, 2, 4).reshape(E * NFG, 128, 2048))
    if router is not None:
        d["router_l"] = np.ascontiguousarray(router.reshape(8, 128, 8).transpose(1, 0, 2).reshape(128, 64))
    return d


D = 1024
EPS = 1e-6
SEQ = 8192


class FrontEnd:
    def __init__(self, P, x_d, gain, identb, ptr, nx=2, rowmap=None):
        self.P, self.x_d, self.gain, self.identb, self.ptr = P, x_d, gain, identb, ptr
        self.X = [P.sb([128, D], F32, f"feX{i}") for i in range(nx)]
        self.ss = P.sb([128, 1], F32, "fess")
        self.xnb = P.sb([128, D], BF16, "fexnb")
        self.n = 0
        self.rowmap = rowmap

    def block(self, blk, H):
        P = self.P
        for tt_ in range(4):
            Xt = self.X[self.n % len(self.X)]
            self.n += 1
            r = blk * 512 + tt_ * 128
            if self.rowmap is not None:
                r = self.rowmap(r)
            P.dma(Xt[:], self.x_d[r:r + 128, :], writes=[Xt])
            P.actv(self.xnb[:], Xt[:], AF.Square, [Xt], [self.xnb, self.ss], accum_out=self.ss[:])
            P.actv(self.ss[:], self.ss[:], AF.Ln, [self.ss], [self.ss], scale=1.0 / D, bias=EPS)
            P.actv(self.ss[:], self.ss[:], AF.Exp, [self.ss], [self.ss], scale=-0.5)
            P.ts("dve", self.xnb[:], Xt[:], self.ss[:, 0:1], ALU.mult, [Xt, self.ss], [self.xnb])
            for c in range(8):
                P.tr(self.ptr[:, c, :], self.xnb[:, c * 128:(c + 1) * 128], self.identb[:],
                     [self.xnb, self.identb], [self.ptr])
            P.tt("dve", H[:, :, tt_ * 128:(tt_ + 1) * 128], self.ptr[:],
                 self.gain[:].unsqueeze(2).to_broadcast([128, 8, 128]), ALU.mult, [self.ptr, self.gain], [H])


def load_w_bf16(P, dst, src_d, ncols, stage, eng="pool"):
    tot = 8 * ncols
    flat = dst[:].rearrange("p c n -> p (c n)")
    q = 0
    i = 0
    SW = stage[0].t.shape[1]
    while q < tot:
        w = min(SW, tot - q)
        st = stage[i % len(stage)]
        P.dma(st[:, 0:w], src_d[:, q:q + w], writes=[st])
        P.cp(eng, flat[:, q:q + w], st[:, 0:w], [st], [dst])
        q += w
        i += 1


def emit_ssd(ctx, seq=SEQ):
    nc, P, tag = ctx.nc, ctx.P, ctx.tag
    NBLK = seq // 512
    x_d = ctx.x_d
    gain_d = nc.dram_tensor(tag + "gain_l", [128, 8], F32, kind="ExternalInput").ap()
    ident_d = nc.dram_tensor(tag + "ident", [128, 128], F32, kind="ExternalInput").ap()
    wch_d = nc.dram_tensor(tag + "wch_l", [128, 8 * 512], F32, kind="ExternalInput").ap()
    wzd_d = nc.dram_tensor(tag + "wzd_l", [128, 8 * 260], F32, kind="ExternalInput").ap()
    convw_d = nc.dram_tensor(tag + "convw_l", [128, 4 * 4], F32, kind="ExternalInput").ap()
    convb_d = nc.dram_tensor(tag + "convb_l", [128, 4], F32, kind="ExternalInput").ap()
    hp_d = nc.dram_tensor(tag + "hp_l", [128, 12], F32, kind="ExternalInput").ap()
    ng_d = nc.dram_tensor(tag + "ng_l", [128, 256], F32, kind="ExternalInput").ap()
    tri_d = nc.dram_tensor(tag + "tri", [128, 384], F32, kind="ExternalInput").ap()
    out = ctx.out

    identf = P.sb([128, 128], F32, "identf")
    identb = P.sb([128, 128], BF16, "identb")
    gain = P.sb([128, 8], F32, "gain")
    convw = P.sb([128, 4, 4], F32, "convw")
    convb = P.sb([128, 4], F32, "convb")
    hp = P.sb([128, 12], F32, "hp")
    ng = P.sb([128, 256], F32, "ng")
    trif = P.sb([128, 384], F32, "trif")
    trib = P.sb([128, 384], BF16, "trib")
    P.dma(identf[:], ident_d, writes=[identf])
    P.cp("dve", identb[:], identf[:], [identf], [identb])
    P.dma(gain[:], gain_d, writes=[gain])
    P.dma(convw[:].rearrange("p a b -> p (a b)"), convw_d, writes=[convw])
    P.dma(convb[:], convb_d, writes=[convb])
    P.dma(hp[:], hp_d, writes=[hp])
    P.dma(ng[:], ng_d, writes=[ng])
    P.dma(trif[:], tri_d, writes=[trif])
    P.cp("dve", trib[:], trif[:], [trif], [trib])
    tri = trib[:, 0:128]
    upp = trif[:, 128:256]
    ones = trib[:, 256:384]
    aneg = P.sb([128, 4], F32, "aneg")
    P.actv(aneg[:], hp[:, 4:8], AF.Exp, [hp], [aneg])
    P.ts("dve", aneg[:], aneg[:], -1.0, ALU.mult, [aneg], [aneg])
    cmask = trif[:, 0:128]

    stage = [P.sb([128, 2048], F32, f"stage{i}") for i in range(2)]
    wch = P.sb([128, 8, 512], BF16, "wch")
    wzd = P.sb([128, 8, 260], BF16, "wzd")
    load_w_bf16(P, wch, wch_d, 512, stage)
    load_w_bf16(P, wzd, wzd_d, 260, stage)

    ptr = P.ps([128, 8, 128], BF16, "ptr")
    pYS = P.ps([128, 512], F32, "pYS")
    pch = [pYS, pYS]
    pZA_2 = [P.ps([128, 512], F32, f"pZA{i}") for i in range(2)]
    pD_2 = [P.ps([128, 512], F32, f"pD{i}") for i in range(2)]
    pYd = [P.ps([128, 512], F32, f"pYd{i}") for i in range(2)]

    fe = FrontEnd(P, x_d, gain, identb, ptr, rowmap=getattr(ctx, 'rowmap', None))
    hTb = [P.sb([128, 8, 512], BF16, f"hT{i}") for i in range(2)]
    xpre = P.sb([128, 4, 515], F32, "xpre")
    xpre_ct = [P.sub(xpre[:, ct, :], f"xpre{ct}") for ct in range(4)]
    P.op("pool", lambda e: e.memset(xpre[:], 0.0), [], xpre_ct)
    cacc = [P.sb([128, 512], F32, f"cacc{i}") for i in range(2)]
    xc = [P.sb([128, 4, 512], BF16, f"xc{i}") for i in range(2)]
    S32 = P.sb([128, 256], F32, "S32")
    Sb = P.sb([128, 256], BF16, "Sb")
    P.op("pool", lambda e: e.memset(S32[:], 0.0), [], [S32])
    P.op("pool", lambda e: e.memset(Sb[:], 0.0), [], [Sb])
    xtok_2 = [P.sb([128, 256], BF16, f"xtok{_i}") for _i in range(2)]
    btok_2 = [P.sb([128, 128], BF16, f"btok{_i}") for _i in range(2)]
    zd_2 = [P.sb([128, 260], F32, f"zd{_i}") for _i in range(2)]
    dt_2 = [P.sb([128, 4], F32, f"dt{_i}") for _i in range(2)]
    ad_2 = [P.sb([128, 4], F32, f"ad{_i}") for _i in range(2)]
    adh_2 = [P.sb([128, 4], BF16, f"adh{_i}") for _i in range(2)]
    adl_2 = [P.sb([128, 4], BF16, f"adl{_i}") for _i in range(2)]
    cs_2 = [P.sb([128, 8], F32, f"cs{_i}") for _i in range(2)]
    ec_2 = [P.sb([128, 4], F32, f"ec{_i}") for _i in range(2)]
    ed_2 = [P.sb([128, 4], F32, f"ed{_i}") for _i in range(2)]
    et_2 = [P.sb([128, 4], F32, f"et{_i}") for _i in range(2)]
    wdt_2 = [P.sb([128, 4], F32, f"wdt{_i}") for _i in range(2)]
    Ah_2 = [P.sb([128, 4, 128], BF16, f"Ah{_i}") for _i in range(2)]
    Al_2 = [P.sb([128, 4, 128], BF16, f"Al{_i}") for _i in range(2)]
    Aful_2 = [P.sb([128, 4, 128], F32, f"Aful{_i}") for _i in range(2)]
    E_2 = [P.sb([128, 4, 128], F32, f"E{_i}") for _i in range(2)]
    Gm_2 = [P.sb([128, 128], F32, f"Gm{_i}") for _i in range(2)]
    Mh_2 = [P.sb([128, 4, 128], BF16, f"Mh{_i}") for _i in range(2)]
    xw_2 = [P.sb([128, 256], BF16, f"xw{_i}") for _i in range(2)]
    y1_2 = [P.sb([128, 256], F32, f"y1{_i}") for _i in range(2)]
    y2_2 = [P.sb([128, 256], F32, f"y2{_i}") for _i in range(2)]
    sz_2 = [P.sb([128, 256], F32, f"sz{_i}") for _i in range(2)]
    junk_2 = [P.sb([128, 256], F32, f"junk{_i}") for _i in range(2)]
    ssq_2 = [P.sb([128, 1], F32, f"ssq{_i}") for _i in range(2)]
    yo = [P.sb([128, 256], F32, f"yo{i}") for i in range(2)]

    for blk in range(NBLK):
        H = hTb[blk % 2]
        fe.block(blk, H)
        XC = xc[blk % 2]
        for ct in range(4):
            pc = pch[ct % 2]
            for c in range(8):
                P.mm(pc[:], wch[:, c, ct * 128:(ct + 1) * 128], H[:, c, :], c == 0, c == 7, [wch, H], [pc])
            xp = xpre_ct[ct]
            P.cp("act", xp[:, 3:515], pc[:], [pc], [xp])
            ca = cacc[ct % 2]
            P.ts("dve", ca[:], xp[:, 0:512], convw[:, ct, 0:1], ALU.mult, [xp, convw], [ca])
            for w in range(1, 4):
                P.stt("dve", ca[:], xp[:, w:w + 512], convw[:, ct, w:w + 1], ca[:], ALU.mult, ALU.add,
                      [xp, convw, ca], [ca])
            P.actv(XC[:, ct, :], ca[:], AF.Silu, [ca, convb], [XC], bias=convb[:, ct:ct + 1], scale=1.0)
            P.cp("pool", xp[:, 0:3], xp[:, 512:515], [xp], [xp])
        def do_tile(blk, tt_, H, XC):
            tok = slice(tt_ * 128, (tt_ + 1) * 128)
            r = blk * 512 + tt_ * 128
            pYd_ = pYd[tt_ % 2]
            pz = pZA_2[tt_ % 2]
            pA = pz
            pD = pD_2[tt_ % 2]
            po_ = 4 * (tt_ % 2)
            xtok = xtok_2[tt_ % 2]; btok = btok_2[tt_ % 2]; zd = zd_2[tt_ % 2]; dt = dt_2[tt_ % 2]; ad = ad_2[tt_ % 2]; adh = adh_2[tt_ % 2]; adl = adl_2[tt_ % 2]; cs = cs_2[tt_ % 2]; ec = ec_2[tt_ % 2]; ed = ed_2[tt_ % 2]; et = et_2[tt_ % 2]; wdt = wdt_2[tt_ % 2]; Ah = Ah_2[tt_ % 2]; Al = Al_2[tt_ % 2]; Aful = Aful_2[tt_ % 2]; E = E_2[tt_ % 2]; Gm = Gm_2[tt_ % 2]; Mh = Mh_2[tt_ % 2]; xw = xw_2[tt_ % 2]; y1 = y1_2[tt_ % 2]; y2 = y2_2[tt_ % 2]; sz = sz_2[tt_ % 2]; junk = junk_2[tt_ % 2]; ssq = ssq_2[tt_ % 2]
            yield
            for c in range(8):
                P.mm(pz[:, 0:260], H[:, c, tok], wzd[:, c, :], c == 0, c == 7, [H, wzd], [pz])
            yield
            P.cp("act", zd[:], pz[:, 0:260], [pz], [zd])
            yield
            P.tt("dve", dt[:], zd[:, 256:260], hp[:, 0:4], ALU.add, [zd, hp], [dt])
            yield
            P.actv(dt[:], dt[:], AF.Exp, [dt], [dt])
            yield
            P.actv(dt[:], dt[:], AF.Ln, [dt], [dt], bias=1.0, scale=1.0)
            yield
            P.tt("dve", ad[:], dt[:], aneg[:], ALU.mult, [dt, aneg], [ad])
            yield
            P.cp("dve", adh[:], ad[:], [ad], [adh])
            yield
            P.tt("dve", adl[:], ad[:], adh[:], ALU.subtract, [ad, adh], [adl])
            yield
            P.tr(ptr[:, po_ + 0, :], XC[:, 0, tok], identb[:], [XC, identb], [ptr])
            yield
            P.tr(ptr[:, po_ + 1, :], XC[:, 1, tok], identb[:], [XC, identb], [ptr])
            yield
            P.tr(ptr[:, po_ + 2, :], XC[:, 2, tok], identb[:], [XC, identb], [ptr])
            yield
            P.cp("act", xtok[:].rearrange("p (a b) -> p a b", a=2), ptr[:, po_:po_ + 2, :], [ptr], [xtok])
            yield
            P.cp("act", btok[:], ptr[:, po_ + 2, :], [ptr], [btok])
            yield
            P.mm(pA[:, 260:388], XC[:, 2, tok], XC[:, 3, tok], True, True, [XC], [pA])
            yield
            P.mm(pA[:, 388:392], tri, adh[:], True, False, [trib, adh], [pA])
            yield
            P.mm(pA[:, 388:392], tri, adl[:], False, True, [trib, adl], [pA])
            yield
            P.mm(pA[:, 392:396], ones, adh[:], True, False, [trib, adh], [pA])
            yield
            P.mm(pA[:, 392:396], ones, adl[:], False, True, [trib, adl], [pA])
            yield
            P.cp("dve", cs[:], pA[:, 388:396], [pA], [cs])
            yield
            P.tt("dve", Gm[:], pA[:, 260:388], cmask, ALU.mult, [pA, trif], [Gm])
            yield
            P.actv(ec[:], cs[:, 0:4], AF.Exp, [cs], [ec])
            yield
            P.actv(et[:], cs[:, 4:8], AF.Exp, [cs], [et])
            yield
            P.tt("dve", ed[:], cs[:, 4:8], cs[:, 0:4], ALU.subtract, [cs], [ed])
            yield
            P.actv(ed[:], ed[:], AF.Exp, [ed], [ed])
            yield
            P.tt("dve", wdt[:], ed[:], dt[:], ALU.mult, [ed, dt], [wdt])
            yield
            for h in range(4):
                P.ts("dve", Aful[:, h, :], upp, ad[:, h:h + 1], ALU.mult, [trif, ad], [Aful])
            yield
            P.cp("dve", Ah[:], Aful[:], [Aful], [Ah])
            yield
            P.tt("dve", Al[:], Aful[:], Ah[:], ALU.subtract, [Aful, Ah], [Al])
            yield
            for h in range(4):
                P.mm(pD[:, h * 128:(h + 1) * 128], Ah[:, h, :], tri, True, False, [Ah, trib], [pD])
                P.mm(pD[:, h * 128:(h + 1) * 128], Al[:, h, :], tri, False, True, [Al, trib], [pD])
            yield
            P.actv(E[:].rearrange("p a b -> p (a b)"), pD[:], AF.Exp, [pD], [E])
            yield
            for h in range(4):
                P.stt("dve", Mh[:, h, :], E[:, h, :], dt[:, h:h + 1], Gm[:], ALU.mult, ALU.mult,
                      [E, dt, Gm], [Mh])
            yield
            for h in range(4):
                P.mm(pYd_[:, h * 64:(h + 1) * 64], Mh[:, h, :], xtok[:, h * 64:(h + 1) * 64], True, True,
                     [Mh, xtok], [pYd_])
            yield
            P.tt("dve", xw[:].rearrange("p (h d) -> p h d", h=4), xtok[:].rearrange("p (h d) -> p h d", h=4),
                 wdt[:].unsqueeze(2).to_broadcast([128, 4, 64]), ALU.mult, [xtok, wdt], [xw])

            def stage_b():
                P.mm(pYS[:, 0:256], XC[:, 3, tok], Sb[:], True, True, [XC, Sb], [pYS])
                P.mm(pYS[:, 256:512], btok[:], xw[:], True, True, [btok, xw], [pYS])
                P.tt("dve", y1[:].rearrange("p (h d) -> p h d", h=4), pYS[:, 0:256].rearrange("p (h d) -> p h d", h=4),
                     ec[:].unsqueeze(2).to_broadcast([128, 4, 64]), ALU.mult, [pYS, ec], [y1])
                P.tt("dve", y1[:], y1[:], pYd_[:, 0:256], ALU.add, [y1, pYd_], [y1])
                P.tt("dve", S32[:].rearrange("p (h d) -> p h d", h=4), S32[:].rearrange("p (h d) -> p h d", h=4),
                     et[:].unsqueeze(2).to_broadcast([128, 4, 64]), ALU.mult, [S32, et], [S32])
                P.tt("dve", S32[:], S32[:], pYS[:, 256:512], ALU.add, [S32, pYS], [S32])
                P.cp("dve", Sb[:], S32[:], [S32], [Sb])
                P.tt("pool", y2[:].rearrange("p (h d) -> p h d", h=4), xtok[:].rearrange("p (h d) -> p h d", h=4),
                     hp[:, 8:12].unsqueeze(2).to_broadcast([128, 4, 64]), ALU.mult, [xtok, hp], [y2])
                P.tt("dve", y1[:], y1[:], y2[:], ALU.add, [y1, y2], [y1])
                P.actv(sz[:], zd[:, 0:256], AF.Silu, [zd], [sz])
                P.tt("dve", y1[:], y1[:], sz[:], ALU.mult, [y1, sz], [y1])
                P.actv(junk[:], y1[:], AF.Square, [y1], [junk, ssq], accum_out=ssq[:])
                P.actv(ssq[:], ssq[:], AF.Ln, [ssq], [ssq], scale=1.0 / 256, bias=EPS)
                P.actv(ssq[:], ssq[:], AF.Exp, [ssq], [ssq], scale=-0.5)
                YO = yo[tt_ % 2]
                P.stt("dve", YO[:], y1[:], ssq[:, 0:1], ng[:], ALU.mult, ALU.mult, [y1, ssq, ng], [YO])
                P.dma(out[r:r + 128, :], YO[:], reads=[YO])

            return stage_b

        for pr_ in range(2):
            gens = [do_tile(blk, 2 * pr_ + i_, H, XC) for i_ in range(2)]
            stage_bs = [None, None]
            live = [True, True]
            while any(live):
                for i_ in range(2):
                    if live[i_]:
                        try:
                            next(gens[i_])
                        except StopIteration as fin_:
                            stage_bs[i_] = fin_.value
                            live[i_] = False
            for fb_ in stage_bs:
                fb_()
    return


def ssd_consts():
    m = np.arange(128)
    tri = (m[:, None] <= m[None, :]).astype(np.float32)
    upp = (m[:, None] > m[None, :]).astype(np.float32)
    ones = np.ones((128, 128), np.float32)
    return {"tri": np.concatenate([tri, upp, ones], 1), "ident": np.eye(128, dtype=np.float32)}


def wl(w):
    n = w.shape[1]
    return np.ascontiguousarray(w.reshape(8, 128, n).transpose(1, 0, 2).reshape(128, 8 * n))


def rep(v, n=128):
    v = np.asarray(v, np.float32).reshape(1, -1)
    return np.ascontiguousarray(np.broadcast_to(v, (n, v.shape[1])))


def ssd_inputs(g, x_b, norm_gain, w_in, conv_w, conv_b, dt_bias, a_log, d_skip, ssm_norm_gain):
    o_z = 512 + 512 + 512
    o_xbc = o_z + 512
    o_dt = o_xbc + 1024
    zc = w_in[:, o_z + g * 256: o_z + (g + 1) * 256]
    xcols = w_in[:, o_xbc + g * 256: o_xbc + (g + 1) * 256]
    bcols = w_in[:, o_xbc + 512 + g * 128: o_xbc + 512 + (g + 1) * 128]
    ccols = w_in[:, o_xbc + 768 + g * 128: o_xbc + 768 + (g + 1) * 128]
    dtc = w_in[:, o_dt + g * 4: o_dt + (g + 1) * 4]
    chan = np.concatenate([np.arange(g * 256, (g + 1) * 256), 512 + np.arange(g * 128, (g + 1) * 128),
                           768 + np.arange(g * 128, (g + 1) * 128)])
    cw = conv_w[:, chan]
    cb = conv_b[chan]
    d = dict(ssd_consts())
    d["x"] = np.ascontiguousarray(x_b)
    d["gain_l"] = np.ascontiguousarray(norm_gain.reshape(8, 128).T)
    d["wch_l"] = wl(np.concatenate([xcols, bcols, ccols], 1))
    d["wzd_l"] = wl(np.concatenate([zc, dtc], 1))
    d["convw_l"] = np.ascontiguousarray(cw.reshape(4, 4, 128).transpose(2, 1, 0).reshape(128, 16))
    d["convb_l"] = np.ascontiguousarray(cb.reshape(4, 128).T)
    hs = slice(4 * g, 4 * g + 4)
    d["hp_l"] = rep(np.concatenate([dt_bias[hs], a_log[hs], d_skip[hs]]))
    d["ng_l"] = rep(ssm_norm_gain[g * 256:(g + 1) * 256])
    return d


NEG = -30000.0


def run_attention(P, steps, ST, PT, scale):
    n = len(steps)
    NB = len(ST)

    def do_qk(i):
        s = steps[i]
        st = ST[i % NB]
        m = len(s["qk"])
        for a, (l, r, rd) in enumerate(s["qk"]):
            P.mm(st[:], l, r, a == 0, a == m - 1, rd, [st])

    def do_pv(i):
        s = steps[i]
        st, pt = ST[i % NB], PT[i % NB]
        P.actv(pt[:], st[:], AF.Exp, [st], [pt], scale=scale)
        vr, vrd = s["v"]
        for (ob, oap, cs, first, last) in s["pv"]:
            P.op("pe", lambda e, oap=oap, l_=pt[:, cs], vr=vr, first=first, last=last: e.matmul(oap, lhsT=l_, rhs=vr, start=first, stop=last, skip_group_check=True), [pt] + vrd, [ob])
        if s.get("fin"):
            s["fin"]()

    pending = []
    for i in range(n):
        if any(steps[p].get("sync") for p in pending):
            for p in pending:
                do_pv(p)
            pending = []
        do_qk(i)
        pending.append(i)
        while len(pending) > NB - 1:
            do_pv(pending.pop(0))
    for p in pending:
        do_pv(p)


def causal_steps(nsb, qk_fn, v_fn, O_fn, fin_fn, identb, maskd, extra_fn=None):
    steps = []
    for sb in range(nsb):
        nk = 4 * sb + 4
        for kt in range(nk):
            t = kt - 4 * sb
            qk = list(qk_fn(sb, kt))
            if extra_fn is not None:
                qk += extra_fn(sb, kt)
            if t >= 0:
                qk.append((identb[:], maskd[:, t, :], [identb, maskd]))
            pv = []
            for j in range(4):
                if t > j:
                    continue
                ob, oap = O_fn(sb, j)
                pv.append((ob, oap, slice(j * 128, (j + 1) * 128), kt == 0 and j % 2 == 0, kt == 4 * sb + j))
            steps.append(dict(qk=qk, pv=pv, v=v_fn(kt), fin=(fin_fn(sb) if kt == nk - 1 else None)))
    return steps


def tok_l(a):
    n = a.shape[1]
    return np.ascontiguousarray(a.reshape(-1, 128, n).transpose(1, 0, 2).reshape(128, -1))


def diag_masks():
    k = np.arange(128)[:, None]
    q = np.arange(512)[None, :]
    m = np.stack([np.where(128 * t + k <= q, 0.0, NEG) for t in range(4)], 1)
    return np.ascontiguousarray(m.reshape(128, 4 * 512).astype(np.float32))


def rope_tables(seq, dim):
    inv = 1.0 / (10000.0 ** (np.arange(0, dim, 2, dtype=np.float32) / dim))
    ang = np.arange(seq, dtype=np.float32)[:, None] * inv[None, :].astype(np.float32)
    return np.cos(ang).astype(np.float32), np.sin(ang).astype(np.float32)


def emit_rmsrope(P, src32, nh, dh, gainrep, cos_ap, sin_ap, dst, scr, cs_reads, rope_cols=None):
    sq, ssq, qn, ta, tb = scr["sq"], scr["ssq"], scr["qn"], scr["ta"], scr["tb"]
    W = nh * dh
    hh = dh // 2
    v3 = lambda b: b[:, 0:W].rearrange("p (h d) -> p h d", h=nh)
    P.tt("pool", sq[:, 0:W], src32[:, 0:W], src32[:, 0:W], ALU.mult, [src32], [sq])
    P.op("dve", lambda e: e.reduce_sum(out=ssq[:, 0:nh], in_=v3(sq), axis=AX.X), [sq], [ssq])
    P.actv(ssq[:, 0:nh], ssq[:, 0:nh], AF.Ln, [ssq], [ssq], scale=1.0 / dh, bias=EPS)
    P.actv(ssq[:, 0:nh], ssq[:, 0:nh], AF.Exp, [ssq], [ssq], scale=-0.5)
    P.tt("dve", v3(qn), v3(src32), ssq[:, 0:nh].unsqueeze(2).to_broadcast([128, nh, dh]), ALU.mult,
         [src32, ssq], [qn])
    P.tt("pool", qn[:, 0:W], qn[:, 0:W], gainrep[:, 0:W], ALU.mult, [qn, gainrep], [qn])
    if cos_ap is None:
        P.cp("dve", dst[:, 0:W], qn[:, 0:W], [qn], [dst])
        return
    cb = cos_ap.unsqueeze(1).to_broadcast([128, nh, hh])
    sb_ = sin_ap.unsqueeze(1).to_broadcast([128, nh, hh])
    q3 = v3(qn)
    d3 = v3(dst)
    a3 = ta[:, 0:nh * hh].rearrange("p (h d) -> p h d", h=nh)
    b3 = tb[:, 0:nh * hh].rearrange("p (h d) -> p h d", h=nh)
    P.tt("dve", a3, q3[:, :, 0:hh], cb, ALU.mult, [qn] + cs_reads, [ta])
    P.tt("pool", b3, q3[:, :, hh:dh], sb_, ALU.mult, [qn] + cs_reads, [tb])
    P.tt("dve", d3[:, :, 0:hh], a3, b3, ALU.subtract, [ta, tb], [dst])
    P.tt("dve", a3, q3[:, :, hh:dh], cb, ALU.mult, [qn] + cs_reads, [ta])
    P.tt("pool", b3, q3[:, :, 0:hh], sb_, ALU.mult, [qn] + cs_reads, [tb])
    P.tt("dve", d3[:, :, hh:dh], a3, b3, ALU.add, [ta, tb], [dst])


def emit_diffattn(ctx, seq=SEQ, layer_idx=0):
    import math
    lam_init = 0.8 - 0.6 * math.exp(-0.3 * layer_idx)
    nc, P, tag = ctx.nc, ctx.P, ctx.tag
    NBLK = seq // 512
    NT = seq // 128
    x_d = ctx.x_d
    gain_d = nc.dram_tensor(tag + "gain_l", [128, 8], F32, kind="ExternalInput").ap()
    ident_d = nc.dram_tensor(tag + "ident", [128, 128], F32, kind="ExternalInput").ap()
    wqk_d = nc.dram_tensor(tag + "wqk_l", [128, 8 * 512], F32, kind="ExternalInput").ap()
    wv_d = nc.dram_tensor(tag + "wv_l", [128, 8 * 256], F32, kind="ExternalInput").ap()
    gqk_d = nc.dram_tensor(tag + "gqk_l", [128, 512], F32, kind="ExternalInput").ap()
    cos_d = nc.dram_tensor(tag + "cos", [128, NT * 32], F32, kind="ExternalInput").ap()
    sin_d = nc.dram_tensor(tag + "sin", [128, NT * 32], F32, kind="ExternalInput").ap()
    lam_d = nc.dram_tensor(tag + "lam_l", [128, 256], F32, kind="ExternalInput").ap()
    sg_d = nc.dram_tensor(tag + "sg_l", [128, 128], F32, kind="ExternalInput").ap()
    mask_d = nc.dram_tensor(tag + "maskd_d", [128, 4 * 512], F32, kind="ExternalInput").ap()
    out = ctx.out

    identf = P.sb([128, 128], F32, "identf")
    identb = P.sb([128, 128], BF16, "identb")
    gain = P.sb([128, 8], F32, "gain")
    gqk = P.sb([128, 512], F32, "gqk")
    lam = P.sb([128, 256], F32, "lam")
    sg = P.sb([128, 128], F32, "sg")
    cos_t = P.sb([128, NT, 32], F32, "cos_t")
    sin_t = P.sb([128, NT, 32], F32, "sin_t")
    maskd = P.sb([128, 4, 512], BF16, "maskd")
    stage = [P.sb([128, 2048], F32, f"stage{i}") for i in range(2)]
    P.dma(identf[:], ident_d, writes=[identf])
    P.cp("dve", identb[:], identf[:], [identf], [identb])
    P.dma(gain[:], gain_d, writes=[gain])
    P.dma(gqk[:], gqk_d, writes=[gqk])
    P.dma(lam[:], lam_d, writes=[lam])
    P.dma(sg[:], sg_d, writes=[sg])
    P.dma(cos_t[:].rearrange("p t d -> p (t d)"), cos_d, writes=[cos_t])
    P.dma(sin_t[:].rearrange("p t d -> p (t d)"), sin_d, writes=[sin_t])
    P.dma(stage[0][:], mask_d, writes=[stage[0]])
    P.cp("dve", maskd[:].rearrange("p a b -> p (a b)"), stage[0][:], [stage[0]], [maskd])
    P.ts("dve", sg[:], sg[:], 1.0 - lam_init, ALU.mult, [sg], [sg])
    lp = P.sb([128, 128], F32, "lp")
    ls = P.sb([128, 2], F32, "ls")
    neglam = P.sb([128, 1], F32, "neglam")
    P.tt("dve", lp[:, 0:64], lam[:, 0:64], lam[:, 64:128], ALU.mult, [lam], [lp])
    P.tt("dve", lp[:, 64:128], lam[:, 128:192], lam[:, 192:256], ALU.mult, [lam], [lp])
    P.op("dve", lambda e: e.reduce_sum(out=ls[:], in_=lp[:].rearrange("p (a b) -> p a b", a=2), axis=AX.X), [lp], [ls])
    P.actv(ls[:], ls[:], AF.Exp, [ls], [ls])
    P.tt("dve", neglam[:], ls[:, 1:2], ls[:, 0:1], ALU.subtract, [ls], [neglam])
    P.ts("dve", neglam[:], neglam[:], -lam_init, ALU.add, [neglam], [neglam])

    wqk = P.sb([128, 8, 512], BF16, "wqk")
    wv = P.sb([128, 8, 256], BF16, "wv")
    load_w_bf16(P, wqk, wqk_d, 512, stage)
    load_w_bf16(P, wv, wv_d, 256, stage)

    QT = P.sb([128, 2, seq], BF16, "QT")
    KT = P.sb([128, 2, seq], BF16, "KT")
    Vaug = P.sb([128, NT, 2, 129], BF16, "Vaug")
    P.op("dve", lambda e: e.memset(Vaug[:, :, :, 128:129], 1.0), [], [Vaug])

    ST = [P.ps([128, 512], F32, f"ST{i}") for i in range(3)]
    OA = [P.ps([128, 2, 256], F32, f"OA{i}") for i in range(2)]
    OB = [P.ps([128, 2, 256], F32, f"OB{i}") for i in range(2)]
    ptr = P.ps([128, 8, 128], BF16, "ptr")
    PT = [P.sb([128, 512], BF16, f"PT{i}") for i in range(3)]

    fe = FrontEnd(P, x_d, gain, identb, ptr, rowmap=getattr(ctx, 'rowmap', None))
    hTb = [P.sb([128, 8, 512], BF16, f"hT{i}") for i in range(2)]
    qk32_2 = [P.sb([128, 512], F32, f"qk32{i}") for i in range(2)]
    scr_2 = [dict(sq=P.sb([128, 512], F32, f"sq{i}"), ssq=P.sb([128, 8], F32, f"ssq{i}"), qn=P.sb([128, 512], F32, f"qn{i}"),
                  ta=P.sb([128, 256], F32, f"ta{i}"), tb=P.sb([128, 256], F32, f"tb{i}")) for i in range(2)]
    qkr_2 = [P.sb([128, 512], BF16, f"qkr{i}") for i in range(2)]

    for blk in range(NBLK):
        H = hTb[blk % 2]
        fe.block(blk, H)
        for tt_ in range(4):
            tok = slice(tt_ * 128, (tt_ + 1) * 128)
            ti = blk * 4 + tt_
            qk32, scr, qkr = qk32_2[ti % 2], scr_2[ti % 2], qkr_2[ti % 2]
            pq = OA[ti % 2]
            pv = OB[ti % 2]
            pqf = pq[:].rearrange("p a b -> p (a b)")
            pvf = pv[:].rearrange("p a b -> p (a b)")
            for c in range(8):
                P.mm(pqf, H[:, c, tok], wqk[:, c, :], c == 0, c == 7, [H, wqk], [pq])
            for c in range(8):
                P.mm(pvf[:, 0:256], H[:, c, tok], wv[:, c, :], c == 0, c == 7, [H, wv], [pv])
            P.cp("act", qk32[:], pqf, [pq], [qk32])
            P.cp("dve", Vaug[:, ti, :, 0:128], pvf[:, 0:256].rearrange("p (h d) -> p h d", h=2), [pv], [Vaug])
            emit_rmsrope(P, qk32, 8, 64, gqk, cos_t[:, ti, :], sin_t[:, ti, :], qkr, scr, [cos_t, sin_t])
            for g4 in range(4):
                P.tr(ptr[:, g4, :], qkr[:, g4 * 128:(g4 + 1) * 128], identb[:], [qkr, identb], [ptr])
            gt = slice(ti * 128, (ti + 1) * 128)
            P.cp("act", QT[:, :, gt], ptr[:, 0:2, :], [ptr], [QT])
            P.cp("act", KT[:, :, gt], ptr[:, 2:4, :], [ptr], [KT])

    tmpo = [P.sb([128, 4, 128], F32, f"tmpo{i}") for i in range(2)]
    rs = P.sb([128, 4], F32, "rs")
    o32 = P.sb([128, 128], F32, "o32")
    junk = P.sb([128, 128], F32, "junk")
    s1 = P.sb([128, 1], F32, "s1")
    yo = [P.sb([128, 128], F32, f"yo{i}") for i in range(4)]
    cnt = [0]
    scale = 64 ** -0.5
    for head in range(2):
        steps = []
        for sb in range(NBLK):
            for comp in range(2):
                Oset = OA if comp == 0 else OB
                ps = slice(comp * 64, (comp + 1) * 64)

                def qk_fn(sb_, kt, head=head, ps=ps):
                    return [(KT[ps, head, kt * 128:(kt + 1) * 128], QT[ps, head, sb_ * 512:(sb_ + 1) * 512], [KT, QT])]

                def v_fn(kt, head=head):
                    return (Vaug[:, kt, head, :], [Vaug])

                def O_fn(sb_, j, Oset=Oset):
                    return (Oset[j // 2], Oset[j // 2][:, j % 2, 0:129])

                def fin_fn(sb_, comp=comp, head=head, Oset=Oset):
                    def fin():
                        T = tmpo[sb_ % 2]
                        if comp == 0:
                            for j in range(4):
                                ob = Oset[j // 2]
                                P.op("dve", lambda e, ob=ob, j=j: e.reciprocal(out=rs[:, j:j + 1], in_=ob[:, j % 2, 128:129]),
                                     [ob], [rs])
                                P.ts("dve", T[:, j, :], ob[:, j % 2, 0:128], rs[:, j:j + 1], ALU.mult, [ob, rs], [T])
                        else:
                            for j in range(4):
                                ob = Oset[j // 2]
                                P.op("dve", lambda e, ob=ob, j=j: e.reciprocal(out=rs[:, j:j + 1], in_=ob[:, j % 2, 128:129]),
                                     [ob], [rs])
                                P.tt("dve", rs[:, j:j + 1], rs[:, j:j + 1], neglam[:], ALU.mult, [rs, neglam], [rs])
                                P.stt("dve", o32[:], ob[:, j % 2, 0:128], rs[:, j:j + 1], T[:, j, :], ALU.mult, ALU.add,
                                      [ob, rs, T], [o32])
                                P.actv(junk[:], o32[:], AF.Square, [o32], [junk, s1], accum_out=s1[:])
                                P.actv(s1[:], s1[:], AF.Ln, [s1], [s1], scale=1.0 / 128, bias=EPS)
                                P.actv(s1[:], s1[:], AF.Exp, [s1], [s1], scale=-0.5)
                                Y = yo[cnt[0] % 4]
                                cnt[0] += 1
                                P.stt("dve", Y[:], o32[:], s1[:, 0:1], sg[:], ALU.mult, ALU.mult, [o32, s1, sg], [Y])
                                r = sb_ * 512 + j * 128
                                P.dma(out[r:r + 128, head * 128:(head + 1) * 128], Y[:], reads=[Y])
                    return fin

                nk = 4 * sb + 4
                for kt in range(nk):
                    t = kt - 4 * sb
                    qk = qk_fn(sb, kt)
                    if t >= 0:
                        qk.append((identb[:], maskd[:, t, :], [identb, maskd]))
                    pv = []
                    for j in range(4):
                        if t > j:
                            continue
                        ob, oap = O_fn(sb, j)
                        pv.append((ob, oap, slice(j * 128, (j + 1) * 128), kt == 0 and j % 2 == 0, kt == 4 * sb + j))
                    steps.append(dict(qk=qk, pv=pv, v=v_fn(kt), fin=(fin_fn(sb) if kt == nk - 1 else None)))
        run_attention(P, steps, ST, PT, scale)
    return


def diffattn_inputs(hp, x_b, norm_gain, w_in, q_gain, k_gain, lam, subln_gain, seq):
    q = w_in[:, hp * 256:(hp + 1) * 256]
    k = w_in[:, 512 + hp * 256: 512 + (hp + 1) * 256]
    v = w_in[:, 1024 + hp * 256: 1024 + (hp + 1) * 256]
    cos, sin = rope_tables(seq, 64)
    d = {"ident": np.eye(128, dtype=np.float32)}
    d["x"] = np.ascontiguousarray(x_b)
    d["gain_l"] = np.ascontiguousarray(norm_gain.reshape(8, 128).T)
    d["wqk_l"] = wl(np.concatenate([q, k], 1))
    d["wv_l"] = wl(v)
    d["gqk_l"] = rep(np.concatenate([np.tile(q_gain, 4), np.tile(k_gain, 4)]))
    d["cos"] = tok_l(cos)
    d["sin"] = tok_l(sin)
    d["lam_l"] = rep(lam.reshape(-1))
    d["sg_l"] = rep(subln_gain)
    d["maskd_d"] = diag_masks()
    return d


def rmsrope3(P, src3, src_reads, nh, dh, gain3, gain_reads, cos_ap, sin_ap, cs_reads, dsts, scr):
    sq, ssq, qn, ta, tb = scr["sq"], scr["ssq"], scr["qn"], scr["ta"], scr["tb"]
    W = nh * dh
    hh = dh // 2
    v3 = lambda b, w=dh: b[:, 0:nh * w].rearrange("p (h d) -> p h d", h=nh)
    P.tt("pool", v3(sq), src3, src3, ALU.mult, src_reads, [sq])
    P.op("dve", lambda e: e.reduce_sum(out=ssq[:, 0:nh], in_=v3(sq), axis=AX.X), [sq], [ssq])
    P.actv(ssq[:, 0:nh], ssq[:, 0:nh], AF.Ln, [ssq], [ssq], scale=1.0 / dh, bias=EPS)
    P.actv(ssq[:, 0:nh], ssq[:, 0:nh], AF.Exp, [ssq], [ssq], scale=-0.5)
    P.tt("dve", v3(qn), src3, ssq[:, 0:nh].unsqueeze(2).to_broadcast([128, nh, dh]), ALU.mult,
         src_reads + [ssq], [qn])
    if cos_ap is None:
        for (d3, db) in dsts:
            P.tt("pool", d3, v3(qn), gain3, ALU.mult, [qn] + gain_reads, [db])
        return
    P.tt("pool", v3(qn), v3(qn), gain3, ALU.mult, [qn] + gain_reads, [qn])
    cb = cos_ap.unsqueeze(1).to_broadcast([128, nh, hh])
    sb_ = sin_ap.unsqueeze(1).to_broadcast([128, nh, hh])
    q3 = v3(qn)
    a3 = v3(ta, hh)
    b3 = v3(tb, hh)
    P.tt("dve", a3, q3[:, :, 0:hh], cb, ALU.mult, [qn] + cs_reads, [ta])
    P.tt("pool", b3, q3[:, :, hh:dh], sb_, ALU.mult, [qn] + cs_reads, [tb])
    for (d3, db) in dsts:
        P.tt("dve", d3[:, :, 0:hh], a3, b3, ALU.subtract, [ta, tb], [db])
    P.tt("dve", a3, q3[:, :, hh:dh], cb, ALU.mult, [qn] + cs_reads, [ta])
    P.tt("pool", b3, q3[:, :, 0:hh], sb_, ALU.mult, [qn] + cs_reads, [tb])
    for (d3, db) in dsts:
        P.tt("dve", d3[:, :, hh:dh], a3, b3, ALU.add, [ta, tb], [db])


class BankStart:
    def __init__(self):
        self.started = {}

    def new_round(self, buf):
        self.started[id(buf)] = False

    def flag(self, buf):
        if not self.started.get(id(buf), False):
            self.started[id(buf)] = True
            return True
        return False


def emit_mla(ctx, seq=SEQ):
    nc, P, tag = ctx.nc, ctx.P, ctx.tag
    NBLK = seq // 512
    NT = seq // 128
    x_d = ctx.x_d
    gain_d = nc.dram_tensor(tag + "gain_l", [128, 8], F32, kind="ExternalInput").ap()
    ident_d = nc.dram_tensor(tag + "ident", [128, 128], F32, kind="ExternalInput").ap()
    win_d = nc.dram_tensor(tag + "win_l", [128, 8 * 448], F32, kind="ExternalInput").ap()
    wuq_d = nc.dram_tensor(tag + "wuq_l", [128, 2 * 384], F32, kind="ExternalInput").ap()
    wukv_d = nc.dram_tensor(tag + "wukv_l", [128, 512], F32, kind="ExternalInput").ap()
    g1_d = nc.dram_tensor(tag + "g1_l", [128, 448], F32, kind="ExternalInput").ap()
    g2_d = nc.dram_tensor(tag + "g2_l", [128, 320], F32, kind="ExternalInput").ap()
    cos_d = nc.dram_tensor(tag + "cos", [128, NT * 32], F32, kind="ExternalInput").ap()
    sin_d = nc.dram_tensor(tag + "sin", [128, NT * 32], F32, kind="ExternalInput").ap()
    mask_d = nc.dram_tensor(tag + "maskd_d", [128, 4 * 512], F32, kind="ExternalInput").ap()
    out = ctx.out

    identf = P.sb([128, 128], F32, "identf")
    identb = P.sb([128, 128], BF16, "identb")
    gain = P.sb([128, 8], F32, "gain")
    g1 = P.sb([128, 448], F32, "g1")
    g2 = P.sb([128, 320], F32, "g2")
    cst = [P.sb([128, 32], F32, f"cst{i}") for i in range(2)]
    maskd = P.sb([128, 4, 512], BF16, "maskd")
    stage = [P.sb([128, 2048], F32, f"stage{i}") for i in range(1)] * 2
    P.dma(identf[:], ident_d, writes=[identf])
    P.cp("dve", identb[:], identf[:], [identf], [identb])
    P.dma(gain[:], gain_d, writes=[gain])
    P.dma(g1[:], g1_d, writes=[g1])
    P.dma(g2[:], g2_d, writes=[g2])
    P.dma(stage[0][:], mask_d, writes=[stage[0]])
    P.cp("dve", maskd[:].rearrange("p a b -> p (a b)"), stage[0][:], [stage[0]], [maskd])
    win = P.sb([128, 8, 448], BF16, "win")
    wuq = P.sb([128, 2, 384], BF16, "wuq")
    wukv = P.sb([128, 512], BF16, "wukv")
    load_w_bf16(P, win, win_d, 448, stage)
    P.dma(stage[1][:, 0:768], wuq_d, writes=[stage[1]])
    P.cp("pool", wuq[:].rearrange("p c n -> p (c n)"), stage[1][:, 0:768], [stage[1]], [wuq])
    P.dma(stage[0][:, 0:512], wukv_d, writes=[stage[0]])
    P.cp("pool", wukv[:], stage[0][:, 0:512], [stage[0]], [wukv])

    QnT = P.sb([128, 2, seq], BF16, "QnT")
    KnT = P.sb([128, 2, seq], BF16, "KnT")
    QrT = P.sb([128, seq], BF16, "QrT")
    KrT = P.sb([128, seq], BF16, "KrT")
    Vaug = P.sb([128, NT, 2, 129], BF16, "Vaug")
    P.op("dve", lambda e: e.memset(Vaug[:, :, :, 128:129], 1.0), [], [Vaug])

    ST = [P.ps([128, 512], F32, f"ST{i}") for i in range(3)]
    OA = [P.ps([128, 2, 256], F32, f"OA{i}") for i in range(2)]
    OB = [P.ps([128, 2, 256], F32, f"OB{i}") for i in range(2)]
    ptr = P.ps([128, 8, 128], BF16, "ptr")
    PT = [P.sb([128, 512], BF16, f"PT{i}") for i in range(3)]

    fe = FrontEnd(P, x_d, gain, identb, ptr, rowmap=getattr(ctx, 'rowmap', None))
    hTb = [P.sb([128, 8, 512], BF16, f"hT{i}") for i in range(1)] * 2
    c32_2 = [P.sb([128, 448], F32, f"c32{i}") for i in range(2)]
    cn_2 = [P.sb([128, 512], BF16, f"cn{i}") for i in range(2)]
    cT_2 = [P.sb([128, 3, 128], BF16, f"cT{i}") for i in range(2)]
    q32_2 = [P.sb([128, 384], F32, f"q32{i}") for i in range(2)]
    kv32_2 = [P.sb([128, 512], F32, f"kv32{i}") for i in range(2)]
    qkb_2 = [P.sb([128, 5, 128], BF16, f"qkb{i}") for i in range(2)]
    scr_2 = [dict(sq=P.sb([128, 256], F32, f"sq{i}"), ssq=P.sb([128, 8], F32, f"ssq{i}"), qn=P.sb([128, 256], F32, f"qn{i}"),
                  ta=P.sb([128, 128], F32, f"ta{i}"), tb=P.sb([128, 128], F32, f"tb{i}")) for i in range(2)]

    for blk in range(NBLK):
        H = hTb[blk % 2]
        fe.block(blk, H)
        for tt_ in range(4):
            tok = slice(tt_ * 128, (tt_ + 1) * 128)
            ti = blk * 4 + tt_
            gt = slice(ti * 128, (ti + 1) * 128)
            c32, cn, cT, q32, kv32, qkb, scr = (c32_2[ti % 2], cn_2[ti % 2], cT_2[ti % 2], q32_2[ti % 2],
                                                kv32_2[ti % 2], qkb_2[ti % 2], scr_2[ti % 2])
            cs_, sn_ = cst[0], cst[1]
            P.dma(cs_[:], cos_d[:, ti * 32:(ti + 1) * 32], writes=[cs_])
            P.dma(sn_[:], sin_d[:, ti * 32:(ti + 1) * 32], writes=[sn_])
            pc = OA[0]
            pcf = pc[:].rearrange("p a b -> p (a b)")
            for c in range(8):
                P.mm(pcf[:, 0:448], H[:, c, tok], win[:, c, :], c == 0, c == 7, [H, win], [pc])
            P.cp("act", c32[:], pcf[:, 0:448], [pc], [c32])
            rmsrope3(P, c32[:, 0:256].rearrange("p (h d) -> p h d", h=1), [c32], 1, 256,
                     g1[:, 0:256].rearrange("p (h d) -> p h d", h=1), [g1], None, None, [],
                     [(cn[:, 0:256].rearrange("p (h d) -> p h d", h=1), cn)], scr)
            rmsrope3(P, c32[:, 256:384].rearrange("p (h d) -> p h d", h=1), [c32], 1, 128,
                     g1[:, 256:384].rearrange("p (h d) -> p h d", h=1), [g1], None, None, [],
                     [(cn[:, 256:384].rearrange("p (h d) -> p h d", h=1), cn)], scr)
            rmsrope3(P, c32[:, 384:448].rearrange("p (h d) -> p h d", h=1), [c32], 1, 64,
                     g1[:, 384:448].rearrange("p (h d) -> p h d", h=1), [g1], cs_[:], sn_[:],
                     [cs_, sn_],
                     [(cn[:, 384:448].rearrange("p (h d) -> p h d", h=1), cn),
                      (cn[:, 448:512].rearrange("p (h d) -> p h d", h=1), cn)], scr)
            for c in range(4):
                P.tr(ptr[:, c, :], cn[:, c * 128:(c + 1) * 128], identb[:], [cn, identb], [ptr])
            P.cp("act", cT[:], ptr[:, 0:3, :], [ptr], [cT])
            P.cp("act", KrT[:, gt], ptr[:, 3, :], [ptr], [KrT])
            pq = OA[1]
            pkv = OB[0]
            pqf = pq[:].rearrange("p a b -> p (a b)")
            pkvf = pkv[:].rearrange("p a b -> p (a b)")
            for c in range(2):
                P.mm(pqf[:, 0:384], cT[:, c, :], wuq[:, c, :], c == 0, c == 1, [cT, wuq], [pq])
            P.mm(pkvf, cT[:, 2, :], wukv[:], True, True, [cT, wukv], [pkv])
            P.cp("act", q32[:], pqf[:, 0:384], [pq], [q32])
            P.cp("act", kv32[:], pkvf, [pkv], [kv32])
            q3 = q32[:].rearrange("p (h d) -> p h d", h=2)
            kv3 = kv32[:].rearrange("p (h d) -> p h d", h=2)
            rmsrope3(P, q3[:, :, 0:128], [q32], 2, 128, g2[:, 0:128].unsqueeze(1).to_broadcast([128, 2, 128]), [g2],
                     None, None, [], [(qkb[:, 0:2, :], qkb)], scr)
            rmsrope3(P, q3[:, :, 128:192], [q32], 2, 64, g2[:, 128:192].unsqueeze(1).to_broadcast([128, 2, 64]), [g2],
                     cs_[:], sn_[:], [cs_, sn_],
                     [(qkb[:, 2, :].rearrange("p (h d) -> p h d", h=2), qkb)], scr)
            rmsrope3(P, kv3[:, :, 0:128], [kv32], 2, 128, g2[:, 192:320].unsqueeze(1).to_broadcast([128, 2, 128]), [g2],
                     None, None, [], [(qkb[:, 3:5, :], qkb)], scr)
            P.cp("dve", Vaug[:, ti, :, 0:128], kv3[:, :, 128:256], [kv32], [Vaug])
            for c in range(5):
                P.tr(ptr[:, c, :], qkb[:, c, :], identb[:], [qkb, identb], [ptr])
            P.cp("act", QnT[:, :, gt], ptr[:, 0:2, :], [ptr], [QnT])
            P.cp("act", QrT[:, gt], ptr[:, 2, :], [ptr], [QrT])
            P.cp("act", KnT[:, :, gt], ptr[:, 3:5, :], [ptr], [KnT])

    rs = P.sb([128, 4], F32, "rs")
    yo = [P.sb([128, 128], F32, f"yo{i}") for i in range(4)]
    cnt = [0]
    scale = 192 ** -0.5
    bs = BankStart()
    for head in range(2):
        steps = []
        ps = slice(head * 64, (head + 1) * 64)
        for sb in range(NBLK):
            Oset = OA if sb % 2 == 0 else OB

            def fin_fn(sb_=sb, Oset=Oset, head=head):
                def fin():
                    for j in range(4):
                        ob = Oset[j // 2]
                        P.op("dve", lambda e, ob=ob, j=j: e.reciprocal(out=rs[:, j:j + 1], in_=ob[:, j % 2, 128:129]),
                             [ob], [rs])
                        Y = yo[cnt[0] % 4]
                        cnt[0] += 1
                        P.ts("dve", Y[:], ob[:, j % 2, 0:128], rs[:, j:j + 1], ALU.mult, [ob, rs], [Y])
                        r = sb_ * 512 + j * 128
                        P.dma(out[r:r + 128, head * 128:(head + 1) * 128], Y[:], reads=[Y])
                return fin

            nk = 4 * sb + 4
            for kt in range(nk):
                t = kt - 4 * sb
                ks = slice(kt * 128, (kt + 1) * 128)
                qs = slice(sb * 512, (sb + 1) * 512)
                qk = [(KnT[:, head, ks], QnT[:, head, qs], [KnT, QnT]),
                      (KrT[ps, ks], QrT[ps, qs], [KrT, QrT])]
                if t >= 0:
                    qk.append((identb[:], maskd[:, t, :], [identb, maskd]))
                pv = []
                for j in range(4):
                    if t > j:
                        continue
                    ob = Oset[j // 2]
                    pv.append((ob, ob[:, j % 2, 0:129], slice(j * 128, (j + 1) * 128),
                               kt == 0 and j % 2 == 0, kt == 4 * sb + j))
                steps.append(dict(qk=qk, pv=pv, v=(Vaug[:, kt, head, :], [Vaug]),
                                  fin=(fin_fn() if kt == nk - 1 else None)))
        run_attention(P, steps, ST, PT, scale)
    return


def mla_inputs(hp, x_b, norm_gain, w_in, cq_gain, ckv_gain, w_uq, w_ukv, qn_gain, qr_gain, kn_gain, kr_gain, seq):
    o = 512 + 6 * 128 + 24
    cols = w_in[:, o:o + 448]
    cos, sin = rope_tables(seq, 64)
    d = {"ident": np.eye(128, dtype=np.float32)}
    d["x"] = np.ascontiguousarray(x_b)
    d["gain_l"] = np.ascontiguousarray(norm_gain.reshape(8, 128).T)
    d["win_l"] = wl(cols)
    uq = w_uq[:, hp * 384:(hp + 1) * 384]
    d["wuq_l"] = np.ascontiguousarray(uq.reshape(2, 128, 384).transpose(1, 0, 2).reshape(128, 768))
    d["wukv_l"] = np.ascontiguousarray(w_ukv[:, hp * 512:(hp + 1) * 512])
    d["g1_l"] = rep(np.concatenate([cq_gain, ckv_gain, kr_gain]))
    d["g2_l"] = rep(np.concatenate([qn_gain, qr_gain, kn_gain]))
    d["cos"] = tok_l(cos)
    d["sin"] = tok_l(sin)
    d["maskd_d"] = diag_masks()
    return d


def nsa_consts(seq):
    NT = seq // 128
    n_cmp = seq // 16 - 1
    NCT = (n_cmp + 127) // 128
    n_sel = seq // 64
    k = np.arange(128)[:, None]
    q = np.arange(512)[None, :]
    mw = np.stack([np.where((q < 128 * t + k) & (128 * t + k <= q + 512), 0.0, NEG) for t in range(8)], 1)
    mc = np.stack([np.where(16 * k + 31 <= 512 * m + q, 0.0, NEG) for m in range(5)], 1)
    n = np.arange(NCT * 128)[:, None]
    j = np.arange(128)[None, :]
    ov = ((16 * n < 64 * j + 64) & (16 * n + 32 > 64 * j) & (n < n_cmp) & (j < n_sel)).astype(np.float32)
    ovl = ov.reshape(NCT, 128, 128).transpose(1, 0, 2).reshape(128, NCT * 128)
    kk = np.arange(seq)[None, :]
    esel = (kk // 64 == np.arange(128)[:, None]).astype(np.float32)
    qq = np.arange(seq)[:, None]
    cur = qq // 64
    jj = np.arange(128)[None, :]
    forced = (jj == 0) | (jj == cur) | (jj == cur - 1)
    future = (jj * 64 > qq) | (jj >= n_sel)
    A = np.where(future | forced, 0.0, 1.0)
    B = np.where(future, -1e6, np.where(forced, 1e6, 0.0))
    ab = np.concatenate([A, B], 1).astype(np.float32)
    cosf, sinf = rope_tables(seq, 64)
    ce = np.minimum(np.arange(NCT * 128) * 16 + 31, seq - 1)
    d = dict(maskd_d=diag_masks(),
             maskw_d=np.ascontiguousarray(mw.reshape(128, 8 * 512).astype(np.float32)),
             maskc_d=np.ascontiguousarray(mc.reshape(128, 5 * 512).astype(np.float32)),
             ovl_d=np.ascontiguousarray(ovl), esel_d=esel, ab_d=tok_l(ab),
             cos=tok_l(cosf), sin=tok_l(sinf), cosc=tok_l(cosf[ce]), sinc=tok_l(sinf[ce]),
             ident=np.eye(128, dtype=np.float32))
    return d


def emit_nsa(ctx, seq=SEQ):
    nc, P, tag = ctx.nc, ctx.P, ctx.tag
    NBLK = seq // 512
    NT = seq // 128
    n_cmp = seq // 16 - 1
    NCT = (n_cmp + 127) // 128
    NCP = NCT * 128
    x_d = ctx.x_d
    gain_d = nc.dram_tensor(tag + "gain_l", [128, 8], F32, kind="ExternalInput").ap()
    ident_d = nc.dram_tensor(tag + "ident", [128, 128], F32, kind="ExternalInput").ap()
    win_d = nc.dram_tensor(tag + "win_l", [128, 8 * 652], F32, kind="ExternalInput").ap()
    gq_d = nc.dram_tensor(tag + "gq_l", [128, 448], F32, kind="ExternalInput").ap()
    w1_d = nc.dram_tensor(tag + "w1_l", [128, 2048], F32, kind="ExternalInput").ap()
    w1c_d = nc.dram_tensor(tag + "w1c_l", [128, 2048], F32, kind="ExternalInput").ap()
    pos_d = nc.dram_tensor(tag + "pos_l", [128, 32], F32, kind="ExternalInput").ap()
    w2_d = nc.dram_tensor(tag + "w2_l", [64, 128], F32, kind="ExternalInput").ap()
    cos_d = nc.dram_tensor(tag + "cos", [128, NT * 32], F32, kind="ExternalInput").ap()
    sin_d = nc.dram_tensor(tag + "sin", [128, NT * 32], F32, kind="ExternalInput").ap()
    cosc_d = nc.dram_tensor(tag + "cosc", [128, NCT * 32], F32, kind="ExternalInput").ap()
    sinc_d = nc.dram_tensor(tag + "sinc", [128, NCT * 32], F32, kind="ExternalInput").ap()
    maskd_d = nc.dram_tensor(tag + "maskd_d", [128, 4 * 512], F32, kind="ExternalInput").ap()
    maskw_d = nc.dram_tensor(tag + "maskw_d", [128, 8 * 512], F32, kind="ExternalInput").ap()
    maskc_d = nc.dram_tensor(tag + "maskc_d", [128, 5 * 512], F32, kind="ExternalInput").ap()
    ovl_d = nc.dram_tensor(tag + "ovl_d", [128, NCT * 128], F32, kind="ExternalInput").ap()
    esel_d = nc.dram_tensor(tag + "esel_d", [128, seq], F32, kind="ExternalInput").ap()
    ab_d = nc.dram_tensor(tag + "ab_d", [128, NT * 256], F32, kind="ExternalInput").ap()
    out = ctx.out

    identf = P.sb([128, 128], F32, "identf")
    identb = P.sb([128, 128], BF16, "identb")
    gain = P.sb([128, 8], F32, "gain")
    gq = P.sb([128, 448], F32, "gq")
    stage = [P.sb([128, 512], F32, f"stage{i}") for i in range(2)]
    P.dma(identf[:], ident_d, writes=[identf])
    P.cp("dve", identb[:], identf[:], [identf], [identb])
    P.dma(gain[:], gain_d, writes=[gain])
    P.dma(gq[:], gq_d, writes=[gq])

    def load_const_bf16(dst_flat_ap, dst_buf, src_d, n):
        q = 0
        i = 0
        while q < n:
            w = min(512, n - q)
            st = stage[i % 2]
            P.dma(st[:, 0:w], src_d[:, q:q + w], writes=[st])
            P.cp("pool", dst_flat_ap[:, q:q + w], st[:, 0:w], [st], [dst_buf])
            q += w
            i += 1

    maskd = P.sb([128, 4, 512], BF16, "maskd")
    maskw = P.sb([128, 8, 512], BF16, "maskw")
    maskc = P.sb([128, 5, 512], BF16, "maskc")
    esel = P.sb([128, seq], BF16, "esel")
    load_const_bf16(maskd[:].rearrange("p a b -> p (a b)"), maskd, maskd_d, 2048)
    load_const_bf16(maskw[:].rearrange("p a b -> p (a b)"), maskw, maskw_d, 4096)
    load_const_bf16(maskc[:].rearrange("p a b -> p (a b)"), maskc, maskc_d, 2560)
    load_const_bf16(esel[:], esel, esel_d, seq)
    win = P.sb([128, 8, 652], BF16, "win")
    load_w_bf16(P, win, win_d, 652, stage)
    w1 = P.sb([128, 32, 64], BF16, "w1")
    load_const_bf16(w1[:].rearrange("p a b -> p (a b)"), w1, w1_d, 2048)
    w1c = P.sb([128, 2, 16, 64], BF16, "w1c")
    load_const_bf16(w1c[:].rearrange("p a b c -> p (a b c)"), w1c, w1c_d, 2048)
    posb = P.sb([128, 2, 16], BF16, "posb")
    w2 = P.sb([64, 2, 64], BF16, "w2")
    cosc = P.sb([128, NCT, 32], F32, "cosc_t")
    sinc = P.sb([128, NCT, 32], F32, "sinc_t")
    P.dma(stage[0][:, 0:32], pos_d, writes=[stage[0]])
    P.cp("pool", posb[:].rearrange("p a b -> p (a b)"), stage[0][:, 0:32], [stage[0]], [posb])
    P.dma(stage[1][0:64, 0:128], w2_d, writes=[stage[1]])
    P.cp("pool", w2[:].rearrange("p a b -> p (a b)"), stage[1][0:64, 0:128], [stage[1]], [w2])
    P.dma(cosc[:].rearrange("p t d -> p (t d)"), cosc_d, writes=[cosc])
    P.dma(sinc[:].rearrange("p t d -> p (t d)"), sinc_d, writes=[sinc])

    import os
    if os.environ.get("NSA_PAD"):
        pad_ = P.sb([128, int(os.environ["NSA_PAD"])], F32, "pad_")
    QT2 = P.sb([128, 2, seq], BF16, "QT2")
    KsT2 = P.sb([128, seq], BF16, "KsT2")
    KwT2 = P.sb([128, seq], BF16, "KwT2")
    kcvT = P.sb([128, seq], BF16, "kcvT")
    Vs = P.sb([128, NT, 65], BF16, "Vs")
    Vw = P.sb([128, NT, 65], BF16, "Vw")
    gts = P.sb([128, NT, 12], F32, "gts")
    CkT2 = P.sb([128, NCP], BF16, "CkT2")
    Cv = P.sb([128, NCT, 193], BF16, "Cv")
    P.op("dve", lambda e: e.memset(Vs[:, :, 64:65], 1.0), [], [Vs])
    P.op("dve", lambda e: e.memset(Vw[:, :, 64:65], 1.0), [], [Vw])
    P.op("dve", lambda e: e.memset(Cv[:, :, 64:65], 1.0), [], [Cv])
    st = stage[0]
    P.dma(st[:, 0:NCT * 128], ovl_d, writes=[st])
    P.cp("dve", Cv[:, :, 65:193], st[:, 0:NCT * 128].rearrange("p (a b) -> p a b", a=NCT), [st], [Cv])

    ST = [P.ps([128, 512], F32, f"ST{i}") for i in range(3)]
    Oc = [P.ps([128, 2, 256], F32, f"Oc{i}") for i in range(2)]
    OX = P.ps([128, 4, 128], F32, "OX")
    OY = P.ps([128, 4, 128], F32, "OY")
    ptr = P.ps([128, 8, 128], BF16, "ptr")
    PT = [P.sb([128, 512], BF16, f"PT{i}") for i in range(3)]

    fe = FrontEnd(P, x_d, gain, identb, ptr, nx=1, rowmap=getattr(ctx, 'rowmap', None))
    hTb = [P.sb([128, 8, 512], BF16, f"hT{i}") for i in range(1)]
    p32 = P.sb([128, 652], F32, "p32")
    qb = P.sb([128, 256], BF16, "qb")
    kb = P.sb([128, 384], BF16, "kb")
    cst = [P.sb([128, 64], F32, f"cst{i}") for i in range(2)]
    scr = dict(sq=P.sb([128, 256], F32, "sq"), ssq=P.sb([128, 8], F32, "ssq"), qn=P.sb([128, 256], F32, "qn"),
               ta=P.sb([128, 128], F32, "ta"), tb=P.sb([128, 128], F32, "tb"))
    g1 = P.sb([128, 12], F32, "g1")

    for blk in range(NBLK):
        H = hTb[0]
        fe.block(blk, H)
        for tt_ in range(4):
            tok = slice(tt_ * 128, (tt_ + 1) * 128)
            ti = blk * 4 + tt_
            gt = slice(ti * 128, (ti + 1) * 128)
            pA = Oc[0]
            pB = Oc[1]
            pAf = pA[:].rearrange("p a b -> p (a b)")
            pBf = pB[:].rearrange("p a b -> p (a b)")
            for c in range(8):
                P.mm(pAf, H[:, c, tok], win[:, c, 0:512], c == 0, c == 7, [H, win], [pA])
            for c in range(8):
                P.mm(pBf[:, 0:140], H[:, c, tok], win[:, c, 512:652], c == 0, c == 7, [H, win], [pB])
            P.cp("act", p32[:, 0:512], pAf, [pA], [p32])
            P.cp("act", p32[:, 512:652], pBf[:, 0:140], [pB], [p32])
            cs_, sn_ = cst[0], cst[1]
            P.dma(cs_[:, 0:32], cos_d[:, ti * 32:(ti + 1) * 32], writes=[cs_])
            P.dma(sn_[:, 0:32], sin_d[:, ti * 32:(ti + 1) * 32], writes=[sn_])
            one = lambda ap: ap.rearrange("p (h d) -> p h d", h=1)
            rmsrope3(P, p32[:, 0:256].rearrange("p (h d) -> p h d", h=4), [p32], 4, 64,
                     gq[:, 0:256].rearrange("p (h d) -> p h d", h=4), [gq], cs_[:, 0:32], sn_[:, 0:32], [cs_, sn_],
                     [(qb[:].rearrange("p (h d) -> p h d", h=4), qb)], scr)
            rmsrope3(P, one(p32[:, 384:448]), [p32], 1, 64, one(gq[:, 256:320]), [gq], cs_[:, 0:32], sn_[:, 0:32],
                     [cs_, sn_], [(one(kb[:, 0:64]), kb), (one(kb[:, 64:128]), kb)], scr)
            rmsrope3(P, one(p32[:, 512:576]), [p32], 1, 64, one(gq[:, 320:384]), [gq], cs_[:, 0:32], sn_[:, 0:32],
                     [cs_, sn_], [(one(kb[:, 128:192]), kb), (one(kb[:, 192:256]), kb)], scr)
            P.cp("dve", kb[:, 256:384], p32[:, 256:384], [p32], [kb])
            P.cp("dve", Vs[:, ti, 0:64], p32[:, 448:512], [p32], [Vs])
            P.cp("dve", Vw[:, ti, 0:64], p32[:, 576:640], [p32], [Vw])
            P.actv(g1[:], p32[:, 640:652], AF.Exp, [p32], [g1], scale=-1.0)
            P.ts("dve", g1[:], g1[:], 1.0, ALU.add, [g1], [g1])
            P.op("dve", lambda e, ti=ti: e.reciprocal(out=gts[:, ti, :], in_=g1[:]), [g1], [gts])
            P.tr(ptr[:, 0, :], qb[:, 0:128], identb[:], [qb, identb], [ptr])
            P.tr(ptr[:, 1, :], qb[:, 128:256], identb[:], [qb, identb], [ptr])
            for c in range(3):
                P.tr(ptr[:, 2 + c, :], kb[:, c * 128:(c + 1) * 128], identb[:], [kb, identb], [ptr])
            P.cp("act", QT2[:, :, gt], ptr[:, 0:2, :], [ptr], [QT2])
            P.cp("act", KsT2[:, gt], ptr[:, 2, :], [ptr], [KsT2])
            P.cp("act", KwT2[:, gt], ptr[:, 3, :], [ptr], [KwT2])
            P.cp("act", kcvT[:, gt], ptr[:, 4, :], [ptr], [kcvT])

    hmid = P.sb([64, 512], BF16, "hmid")
    cb16 = P.sb([64, NCP], BF16, "cb16")
    cbias = P.sb([64, 2], F32, "cbias")
    ctok = P.sb([128, 64], F32, "ctok")
    ckn = P.sb([128, 128], BF16, "ckn")
    P.op("dve", lambda e: e.memset(cb16[:], 0.0), [], [cb16])
    for kv in range(2):
        ps = slice(kv * 64, (kv + 1) * 64)
        pc = Oc[kv]
        pcf = pc[:].rearrange("p a b -> p (a b)")
        for c in range(16):
            P.mm(pcf[0:64, 511:512], w1c[:, kv, c, :], posb[:, kv, c:c + 1], c == 0, c == 15, [w1c, posb], [pc])
        P.cp("dve", cbias[:, kv:kv + 1], pcf[0:64, 511:512], [pc], [cbias])
        for pos in range(32):
            rhs = kcvT[ps, pos: pos + 16 * (n_cmp - 1) + 1: 16]
            P.mm(pcf[0:64, 0:n_cmp], w1[ps, pos, :], rhs, pos == 0, pos == 31, [w1, kcvT], [pc])
        P.actv(hmid[:, 0:n_cmp], pcf[0:64, 0:n_cmp], AF.Silu, [pc, cbias], [hmid], bias=cbias[:, kv:kv + 1], scale=1.0)
        P.mm(pcf[0:64, 0:n_cmp], w2[:, kv, :], hmid[:, 0:n_cmp], True, True, [w2, hmid], [pc])
        P.cp("act", cb16[:, 0:n_cmp], pcf[0:64, 0:n_cmp], [pc], [cb16])
        for nt in range(NCT):
            P.tr(ptr[:, nt, 0:64], cb16[:, nt * 128:(nt + 1) * 128], identb[0:64, 0:64], [cb16, identb], [ptr])
        if kv == 0:
            for nt in range(NCT):
                P.cp("act", ctok[:], ptr[:, nt, 0:64], [ptr], [ctok])
                one = lambda ap: ap.rearrange("p (h d) -> p h d", h=1)
                rmsrope3(P, one(ctok[:]), [ctok], 1, 64, one(gq[:, 384:448]), [gq], cosc[:, nt, :], sinc[:, nt, :],
                         [cosc, sinc], [(one(ckn[:, 0:64]), ckn), (one(ckn[:, 64:128]), ckn)], scr)
                P.tr(ptr[:, 4 + nt % 4, :], ckn[:], identb[:], [ckn, identb], [ptr])
                P.cp("act", CkT2[:, nt * 128:(nt + 1) * 128], ptr[:, 4 + nt % 4, :], [ptr], [CkT2])
        else:
            P.cp("dve", Cv[:, :, 0:64], ptr[:, 0:NCT, 0:64], [ptr], [Cv])

    bs = BankStart()
    scale = 64 ** -0.5
    oc = [P.sb([128, 4, 4, 64], F32, f"oc{i}") for i in range(1)]
    imp = [P.sb([128, 4, 128], F32, f"imp{i}") for i in range(1)] * 2
    negsel = [P.sb([128, 512], BF16, f"negsel{i}") for i in range(2)]
    ABt = [P.sb([128, 256], F32, f"ABt{i}") for i in range(2)]
    impf = P.sb([128, 128], F32, "impf")
    imp2 = P.sb([128, 128], F32, "imp2")
    m8a = P.sb([128, 8], F32, "m8a")
    m8b = P.sb([128, 8], F32, "m8b")
    selb = P.sb([128, 128], BF16, "selb")
    rs = P.sb([128, 4], F32, "rs")
    acc = [P.sb([128, 4, 64], F32, f"acc{i}") for i in range(2)]
    yout = [P.sb([128, 4, 256], F32, f"yout{i}") for i in range(1)]
    steps = []
    for sb in range(NBLK):
        qs = slice(sb * 512, (sb + 1) * 512)
        OC, IMP, NS, YO = oc[0], imp[sb % 2], negsel[sb % 2], yout[0]
        nts = [nt for nt in range(NCT) if sb - 4 * nt >= 0]
        for h in range(4):
            pair, hh = h // 2, h % 2
            ps = slice(hh * 64, (hh + 1) * 64)

            def fin_c(h=h, OC=OC, IMP=IMP, sb=sb, NS=NS):
                for j in range(4):
                    ob = Oc[j // 2]
                    P.ts("dve", rs[:, j:j + 1], ob[:, j % 2, 64:65], 1e-30, ALU.max, [ob], [rs])
                    P.op("dve", lambda e, j=j: e.reciprocal(out=rs[:, j:j + 1], in_=rs[:, j:j + 1]), [rs], [rs])
                    P.ts("dve", OC[:, j, h, :], ob[:, j % 2, 0:64], rs[:, j:j + 1], ALU.mult, [ob, rs], [OC])
                    if h == 0:
                        P.ts("dve", IMP[:, j, :], ob[:, j % 2, 65:193], rs[:, j:j + 1], ALU.mult, [ob, rs], [IMP])
                    else:
                        P.stt("dve", IMP[:, j, :], ob[:, j % 2, 65:193], rs[:, j:j + 1], IMP[:, j, :], ALU.mult, ALU.add,
                              [ob, rs, IMP], [IMP])
                if h == 3:
                    for j in range(4):
                        ti = sb * 4 + j
                        AB = ABt[j % 2]
                        P.dma(AB[:], ab_d[:, ti * 256:(ti + 1) * 256], writes=[AB])
                        P.tt("dve", impf[:], IMP[:, j, :], AB[:, 0:128], ALU.mult, [IMP, AB], [impf])
                        P.tt("dve", impf[:], impf[:], AB[:, 128:256], ALU.add, [impf, AB], [impf])
                        P.op("dve", lambda e: e.max(out=m8a[:], in_=impf[:]), [impf], [m8a])
                        P.op("dve", lambda e: e.match_replace(out=imp2[:], in_to_replace=m8a[:], in_values=impf[:],
                                                              imm_value=-3.0e38), [m8a, impf], [imp2])
                        P.op("dve", lambda e: e.max(out=m8b[:], in_=imp2[:]), [imp2], [m8b])
                        P.ts("dve", selb[:], impf[:], m8b[:, 7:8], ALU.is_ge, [impf, m8b], [selb])
                        P.tr(ptr[:, j, :], selb[:], identb[:], [selb, identb], [ptr])
                        P.ts("dve", NS[:, j * 128:(j + 1) * 128], ptr[:, j, :], -1.0, ALU.add, [ptr], [NS],
                             s2=-NEG, op1=ALU.mult)

            for a, nt in enumerate(nts):
                m = sb - 4 * nt
                qk = [(CkT2[ps, nt * 128:(nt + 1) * 128], QT2[ps, pair, qs], [CkT2, QT2])]
                if m <= 4:
                    qk.append((identb[:], maskc[:, m, :], [identb, maskc]))
                pv = []
                for j in range(4):
                    ob = Oc[j // 2]
                    pv.append((ob, ob[:, j % 2, 0:193], slice(j * 128, (j + 1) * 128),
                               a == 0 and j % 2 == 0, a == len(nts) - 1))
                steps.append(dict(qk=qk, pv=pv, v=(Cv[:, nt, :], [Cv]),
                                  fin=(fin_c if a == len(nts) - 1 else None),
                                  sync=(h == 3 and a == len(nts) - 1)))
        for h in range(4):
            pair, hh = h // 2, h % 2
            ps = slice(hh * 64, (hh + 1) * 64)
            ACC = acc[h % 2]

            def fin_s(h=h, ACC=ACC, OC=OC, sb=sb):
                for j in range(4):
                    ti = sb * 4 + j
                    P.op("dve", lambda e, j=j: e.reciprocal(out=rs[:, j:j + 1], in_=OX[:, j, 64:65]), [OX], [rs])
                    P.tt("dve", rs[:, j:j + 1], rs[:, j:j + 1], gts[:, ti, h * 3 + 1:h * 3 + 2], ALU.mult, [rs, gts], [rs])
                    P.ts("dve", ACC[:, j, :], OX[:, j, 0:64], rs[:, j:j + 1], ALU.mult, [OX, rs], [ACC])
                    import os
                    _m = os.environ.get("NSA_DBG", "")
                    if _m in ("w", "c"):
                        P.ts("dve", ACC[:, j, :], ACC[:, j, :], 0.0, ALU.mult, [ACC], [ACC])
                    if _m in ("", "c"):
                        P.stt("dve", ACC[:, j, :], OC[:, j, h, :], gts[:, ti, h * 3:h * 3 + 1], ACC[:, j, :], ALU.mult, ALU.add,
                              [OC, gts, ACC], [ACC])

            def fin_w(h=h, ACC=ACC, YO=YO, sb=sb):
                for j in range(4):
                    ti = sb * 4 + j
                    P.op("dve", lambda e, j=j: e.reciprocal(out=rs[:, j:j + 1], in_=OY[:, j, 64:65]), [OY], [rs])
                    P.tt("dve", rs[:, j:j + 1], rs[:, j:j + 1], gts[:, ti, h * 3 + 2:h * 3 + 3], ALU.mult, [rs, gts], [rs])
                    import os
                    if os.environ.get("NSA_DBG", "") in ("s", "c"):
                        P.ts("dve", rs[:, j:j + 1], rs[:, j:j + 1], 0.0, ALU.mult, [rs], [rs])
                    P.stt("dve", YO[:, j, h * 64:(h + 1) * 64], OY[:, j, 0:64], rs[:, j:j + 1], ACC[:, j, :],
                          ALU.mult, ALU.add, [OY, rs, ACC], [YO])
                if h == 3:
                    for j in range(4):
                        r = sb * 512 + j * 128
                        P.dma(out[r:r + 128, :], YO[:, j, :], reads=[YO])

            nk = 4 * sb + 4
            for kt in range(nk):
                t = kt - 4 * sb
                ks = slice(kt * 128, (kt + 1) * 128)
                qk = [(KsT2[ps, ks], QT2[ps, pair, qs], [KsT2, QT2]),
                      (esel[:, ks], NS[:], [esel, NS])]
                if t >= 0:
                    qk.append((identb[:], maskd[:, t, :], [identb, maskd]))
                pv = []
                for j in range(4):
                    if t > j:
                        continue
                    pv.append((OX, OX[:, j, 0:65], slice(j * 128, (j + 1) * 128), kt == 0 and j == 0, kt == 4 * sb + j))
                steps.append(dict(qk=qk, pv=pv, v=(Vs[:, kt, :], [Vs]), fin=(fin_s if kt == nk - 1 else None)))
            kts = [kt for kt in range(4 * sb - 4, 4 * sb + 4) if kt >= 0]
            first_done = False
            for kt in kts:
                t8 = kt - (4 * sb - 4)
                ks = slice(kt * 128, (kt + 1) * 128)
                qk = [(KwT2[ps, ks], QT2[ps, pair, qs], [KwT2, QT2]),
                      (identb[:], maskw[:, t8, :], [identb, maskw])]
                pv = []
                for j in range(4):
                    if not (j <= t8 <= j + 4):
                        continue
                    pv.append((OY, OY[:, j, 0:65], slice(j * 128, (j + 1) * 128), not first_done, t8 == j + 4))
                    first_done = True
                steps.append(dict(qk=qk, pv=pv, v=(Vw[:, kt, :], [Vw]), fin=(fin_w if kt == kts[-1] else None)))
    run_attention(P, steps, ST, PT, scale)
    if os.environ.get("NSA_DUMP"):
        dbg = nc.dram_tensor(tag + "dbg", [128, 4 * 1024], F32, kind="ExternalOutput").ap()
        for i, (src, srcb) in enumerate(((QT2[:, 0, 0:1024], QT2), (KsT2[:, 0:1024], KsT2), (KwT2[:, 0:1024], KwT2),
                                         (CkT2[:, 0:512], CkT2))):
            w = 1024 if i < 3 else 512
            for hf in range(w // 256):
                P.cp("dve", impf[:].rearrange("p a -> p a")[:, 0:128], impf[:, 0:128], [impf], [impf]) if False else None
                t_ = yout[0]
                P.cp("dve", t_[:, 0, 0:256], src[:, hf * 256:(hf + 1) * 256], [srcb], [t_])
                P.dma(dbg[:, i * 1024 + hf * 256: i * 1024 + (hf + 1) * 256], t_[:, 0, 0:256], reads=[t_])
    return


def nsa_inputs(g, x_b, norm_gain, w_in, q_gain, k_gain, cmp_pos, cmp_w1, cmp_w2, seq):
    q = w_in[:, g * 256:(g + 1) * 256]
    kvs = [w_in[:, 512 + i * 128 + g * 64: 512 + i * 128 + (g + 1) * 64] for i in range(6)]
    gl = w_in[:, 512 + 768 + g * 12: 512 + 768 + (g + 1) * 12]
    d = nsa_consts(seq)
    d["x"] = np.ascontiguousarray(x_b)
    d["gain_l"] = np.ascontiguousarray(norm_gain.reshape(8, 128).T)
    d["win_l"] = wl(np.concatenate([q] + kvs + [gl], 1))
    d["gq_l"] = rep(np.concatenate([np.tile(q_gain, 4), k_gain[1], k_gain[2], k_gain[0]]))
    w1 = cmp_w1.reshape(2, 32, 64, 64).transpose(0, 2, 1, 3).reshape(128, 2048)
    d["w1_l"] = np.ascontiguousarray(w1)
    d["w1c_l"] = np.ascontiguousarray(cmp_w1.reshape(2, 16, 128, 64).transpose(2, 0, 1, 3).reshape(128, 2048))
    d["pos_l"] = np.ascontiguousarray(cmp_pos.reshape(2, 16, 128).transpose(2, 0, 1).reshape(128, 32))
    d["w2_l"] = np.ascontiguousarray(cmp_w2.transpose(1, 0, 2).reshape(64, 128))
    return d


PAIRS = [[0, 1], [2, 3], [4, 5], [6, 7]]
_CACHE = {}


class Ctx:
    pass


def build_fused(S=SEQ):
    nc = bass.Bass("TRN2", target_bir_lowering=False)
    P = Prog(nc)
    NTK = S // 2
    x_full = nc.dram_tensor("x_full", [S, D], F32, kind="ExternalInput").ap()
    x_rows = nc.dram_tensor("x_rows", [NTK, D], F32, kind="ExternalInput").ap()
    sel_d = nc.dram_tensor("sel", [128, 2], F32, kind="ExternalInput").ap()
    y = nc.dram_tensor("y", [NTK, D], F32, kind="ExternalOutput").ap()
    mixA = nc.dram_tensor("mixA_my", [S, 512], F32, kind="Internal").ap()
    GA = nc.dram_tensor("GA_all", [2 * S, 512], F32, kind="Internal").ap()
    x1_my = nc.dram_tensor("x1_my", [NTK, D], F32, kind="Internal").ap()
    x1_full = nc.dram_tensor("x1_full", [S, D], F32, kind="Internal").ap()
    mixC = nc.dram_tensor("mixC_my", [S, 512], F32, kind="Internal").ap()
    GC = nc.dram_tensor("GC_all", [2 * S, 512], F32, kind="Internal").ap()

    def phase(tag, fn, *args, **kw):
        ctx = Ctx()
        ctx.nc, ctx.P, ctx.tag = nc, P, tag + "_"
        ctx.S, ctx.sel_d = S, sel_d
        for k, v in kw.items():
            setattr(ctx, k, v)
        P.begin_phase(tag)
        fn(ctx, *args)
        P.end_phase()

    def gather(tag, src, dst):
        rows, width = src.shape
        R = (2 * 1024 * 1024) // (width * 4)
        P.begin_phase(tag)
        for k in range(rows // R):
            P.op("pool", lambda e, k=k: e.collective_compute(
                "AllGather", ALU.bypass, replica_groups=PAIRS,
                ins=[src[k * R:(k + 1) * R, :]], outs=[dst[2 * k * R:2 * (k + 1) * R, :]]), cc=True)
        P.end_phase()

    RM = (2 * 1024 * 1024) // (512 * 4)
    RX = (2 * 1024 * 1024) // (D * 4)
    gaddr = lambda p, t: (t // RM) * 2 * RM + p * RM + (t % RM)
    x1map = lambda T: ((T % NTK) // RX) * 2 * RX + (T // NTK) * RX + (T % RX)

    phase("ssd", emit_ssd, S, x_d=x_full, out=mixA[:, 256:512])
    phase("da", emit_diffattn, S, 0, x_d=x_full, out=mixA[:, 0:256])
    gather("e1", mixA, GA)
    phase("ffn", emit_ffn, 1, NTK, xin=x_rows, G=GA, out=x1_my, gaddr=gaddr)
    gather("e2", x1_my, x1_full)
    phase("nsa", emit_nsa, S, x_d=x1_full, out=mixC[:, 0:256], rowmap=x1map)
    phase("mla", emit_mla, S, x_d=x1_full, out=mixC[:, 256:512], rowmap=x1map)
    gather("e3", mixC, GC)
    phase("moe", emit_ffn, 8, NTK, xin=x1_my, G=GC, out=y, gaddr=gaddr)
    return nc


_PERM = np.concatenate([np.arange(0, 256), np.arange(512, 768), np.arange(256, 512), np.arange(768, 1024)])


def kernel(**inp):
    inp = {k: np.asarray(v) for k, v in inp.items()}
    x = np.ascontiguousarray(inp["x"], dtype=np.float32)
    B, S, _ = x.shape
    NTK = S // 2
    f = lambda k: inp[k][0]
    if "nc" not in _CACHE:
        _CACHE["nc"] = build_fused(S)
    nc = _CACHE["nc"]
    lay_ffn = ffn_layouts(f("ev_w_out")[_PERM], f("ev_norm_ffn"), inp["ffn_w_gate"], inp["ffn_w_up"], inp["ffn_w_down"])
    lay_moe = ffn_layouts(f("od_w_out")[_PERM], f("od_norm_ffn"), f("moe_w_gate"), f("moe_w_up"), f("moe_w_down"),
                          f("moe_router"))
    per_half = []
    for h in range(2):
        d = {}

        def add(tag, dd):
            for k, v in dd.items():
                if k != "x":
                    d[tag + "_" + k] = v
        add("ssd", ssd_inputs(h, x[0], f("ev_norm_mix"), f("ev_w_in"), f("ssm_conv_w"), f("ssm_conv_b"),
                              f("ssm_dt_bias"), f("ssm_a_log"), f("ssm_d"), f("ssm_norm_gain")))
        add("da", diffattn_inputs(h, x[0], f("ev_norm_mix"), f("ev_w_in"), f("da_q_gain"), f("da_k_gain"),
                                  f("da_lambda"), f("da_subln_gain"), S))
        add("nsa", nsa_inputs(h, x[0], f("od_norm_mix"), f("od_w_in"), f("nsa_q_gain"), f("nsa_k_gain"),
                              f("nsa_cmp_pos"), f("nsa_cmp_w1"), f("nsa_cmp_w2"), S))
        add("mla", mla_inputs(h, x[0], f("od_norm_mix"), f("od_w_in"), f("mla_cq_gain"), f("mla_ckv_gain"),
                              f("mla_w_uq"), f("mla_w_ukv"), f("mla_qn_gain"), f("mla_qr_gain"), f("mla_kn_gain"),
                              f("mla_kr_gain"), S))
        add("ffn", lay_ffn)
        add("moe", lay_moe)
        d["sel"] = np.ascontiguousarray(np.broadcast_to(np.array([[1.0 - h, float(h)]], np.float32), (128, 2)))
        per_half.append(d)
    ins = []
    for c in range(8):
        b, h = c // 2, c % 2
        d = dict(per_half[h])
        d["x_full"] = x[b]
        d["x_rows"] = np.ascontiguousarray(x[b, h * NTK:(h + 1) * NTK])
        ins.append(d)
    res = run_bass_kernel_spmd(nc, ins, core_ids=list(range(8)))
    out = np.empty((B, S, D), np.float32)
    for c in range(8):
        b, h = c // 2, c % 2
        out[b, h * NTK:(h + 1) * NTK] = res.results[c]["y"]
    return out
```

```python
from concourse.bass_utils import run_bass_kernel_spmd

from contextlib import ExitStack
import numpy as np
import concourse.bass as bass
import concourse.mybir as mybir

F32 = mybir.dt.float32
BF16 = mybir.dt.bfloat16
AF = mybir.ActivationFunctionType
ALU = mybir.AluOpType
AX = mybir.AxisListType

ENGS = ("pe", "dve", "act", "pool", "sp")
NRING = 8


class Buf:
    __slots__ = ("t", "lw", "rd", "name")

    def __init__(self, t, name=""):
        self.t = t
        self.lw = None
        self.rd = []
        self.name = name

    def __getitem__(self, k):
        return self.t[k]


class Op:
    __slots__ = ("eng", "fn", "deps", "sig", "idx", "dma", "ring", "ruse", "cc")

    def __init__(self, eng, fn, dma=False):
        self.eng = eng
        self.fn = fn
        self.deps = []
        self.sig = False
        self.idx = None
        self.dma = dma
        self.ring = None
        self.ruse = None
        self.cc = False


class Prog:
    def __init__(self, nc, same_engine_sync=True):
        self.nc = nc
        self.es = ExitStack()
        self.pes = None
        self.ops = {e: [] for e in ENGS}
        self.same = same_engine_sync
        self.nbuf = 0
        self.tag = "p0"
        self.persist = []
        self.sem = {e: self.es.enter_context(nc.semaphore(f"s_{e}")) for e in ENGS}
        self.ring = {"sp": [self.es.enter_context(nc.semaphore(f"r_sp{i}")) for i in range(NRING)]}
        self.ccsem = self.es.enter_context(nc.semaphore("s_cc"))
        self.cnt = {e: 0 for e in ENGS}
        self.nd = {e: 0 for e in ENGS}
        self.ncc = 0
        self.waited = {e: {} for e in ENGS}
        self.engh = {"pe": nc.tensor, "dve": nc.vector, "act": nc.scalar, "pool": nc.gpsimd, "sp": nc.sync}

    def begin_phase(self, tag):
        self.tag = tag
        self.pes = ExitStack()

    def end_phase(self):
        self._emit_ops()
        self._barrier()
        self.pes.close()
        self.pes = None
        for b in self.persist:
            b.lw = None
            b.rd = []

    def sb(self, shape, dt=F32, name=None):
        self.nbuf += 1
        name = f"{self.tag}_{name or 'sb'}_{self.nbuf}"
        t = self.pes.enter_context(self.nc.sbuf_tensor(name, list(shape), dt))
        return Buf(t, name)

    def ps(self, shape, dt=F32, name=None):
        self.nbuf += 1
        name = f"{self.tag}_{name or 'ps'}_{self.nbuf}"
        t = self.pes.enter_context(self.nc.psum_tensor(name, list(shape), dt))
        return Buf(t, name)

    def sub(self, ap, name=""):
        return Buf(ap, name)

    def dram(self, ap, name=""):
        b = Buf(ap, name)
        self.persist.append(b)
        return b

    def op(self, eng, fn, reads=(), writes=(), dma=False, cc=False):
        o = Op(eng, fn, dma)
        o.cc = cc
        deps = []
        for b in reads:
            if b.lw is not None:
                deps.append(b.lw)
        for b in writes:
            if b.lw is not None:
                deps.append(b.lw)
            deps.extend(b.rd)
        seen = set()
        for d in deps:
            if id(d) in seen or d is o:
                continue
            seen.add(id(d))
            if d.eng == eng and not d.dma and not d.cc:
                if eng == "pe" or not self.same:
                    continue
            d.sig = True
            o.deps.append(d)
        for b in reads:
            b.rd.append(o)
        for b in writes:
            b.lw = o
            b.rd = []
        if dma or cc:
            o.sig = True
        self.ops[eng].append(o)
        return o

    def mm(self, out, lhsT, rhs, start, stop, reads, writes):
        return self.op("pe", lambda e: e.matmul(out, lhsT=lhsT, rhs=rhs, start=start, stop=stop), reads, writes)

    def tr(self, out, in_, ident, reads, writes):
        return self.op("pe", lambda e: e.transpose(out=out, in_=in_, identity=ident), reads, writes)

    def actv(self, out, in_, func, reads, writes, **kw):
        return self.op("act", lambda e: e.activation(out=out, in_=in_, func=func, **kw), reads, writes)

    def cp(self, eng, out, in_, reads, writes):
        if eng == "act":
            return self.op("act", lambda e: e.copy(out=out, in_=in_), reads, writes)
        return self.op(eng, lambda e: e.tensor_copy(out=out, in_=in_), reads, writes)

    def tt(self, eng, out, in0, in1, op, reads, writes):
        return self.op(eng, lambda e: e.tensor_tensor(out=out, in0=in0, in1=in1, op=op), reads, writes)

    def ts(self, eng, out, in0, s1, op0, reads, writes, s2=None, op1=None, **kw):
        if op1 is None:
            return self.op(eng, lambda e: e.tensor_scalar(out=out, in0=in0, scalar1=s1, scalar2=None, op0=op0, **kw), reads, writes)
        return self.op(eng, lambda e: e.tensor_scalar(out=out, in0=in0, scalar1=s1, scalar2=s2, op0=op0, op1=op1, **kw), reads, writes)

    def stt(self, eng, out, in0, scalar, in1, op0, op1, reads, writes):
        return self.op(eng, lambda e: e.scalar_tensor_tensor(out=out, in0=in0, scalar=scalar, in1=in1, op0=op0, op1=op1), reads, writes)

    def dma(self, out, in_, reads=(), writes=(), eng="sp", **kw):
        return self.op(eng, lambda e: e.dma_start(out=out, in_=in_, **kw), reads, writes, dma=True)

    def _emit_ops(self):
        for e in ENGS:
            ops = self.ops[e]
            if ops and not ops[-1].dma and not ops[-1].cc:
                ops[-1].sig = True
            for o in ops:
                if o.dma:
                    o.ring = self.nd[e] % NRING
                    o.ruse = self.nd[e] // NRING
                    self.nd[e] += 1
                elif o.cc:
                    self.ncc += 1
                    o.idx = self.ncc
                elif o.sig:
                    self.cnt[e] += 1
                    o.idx = self.cnt[e]
        for e in ENGS:
            h = self.engh[e]
            waited = self.waited[e]
            for o in self.ops[e]:
                need = {}
                for d in o.deps:
                    if d.dma:
                        key = ("r", d.eng, d.ring)
                        val = 16 * (d.ruse + 1)
                    elif d.cc:
                        key = ("c",)
                        val = d.idx
                    else:
                        key = ("s", d.eng)
                        val = d.idx
                    if need.get(key, 0) < val:
                        need[key] = val
                if o.dma and o.ruse > 0:
                    key = ("r", e, o.ring)
                    val = 16 * o.ruse
                    if need.get(key, 0) < val:
                        need[key] = val
                for key, val in need.items():
                    if waited.get(key, 0) >= val:
                        continue
                    waited[key] = val
                    h.wait_ge(self._semof(key), val)
                ins = o.fn(h)
                if o.dma:
                    ins.then_inc(self.ring[e][o.ring], 16)
                elif o.cc:
                    ins.then_inc(self.ccsem)
                elif o.sig:
                    ins.then_inc(self.sem[e], 1)
            self.ops[e] = []

    def _semof(self, key):
        if key[0] == "s":
            return self.sem[key[1]]
        if key[0] == "c":
            return self.ccsem
        return self.ring[key[1]][key[2]]

    def _barrier(self):
        targets = {}
        for e in ENGS:
            if self.cnt[e] > 0:
                targets[("s", e)] = self.cnt[e]
        for q in self.ring:
            for r in range(NRING):
                n = len(range(r, self.nd[q], NRING))
                if n:
                    targets[("r", q, r)] = 16 * n
        if self.ncc:
            targets[("c",)] = self.ncc
        for e in ENGS:
            h = self.engh[e]
            waited = self.waited[e]
            for key, val in targets.items():
                if key == ("s", e) or waited.get(key, 0) >= val:
                    continue
                waited[key] = val
                h.wait_ge(self._semof(key), val)


D = 1024
DFF = 2816
NFG = 11
EPS = 1e-6


def emit_ffn(ctx, n_exp, ntok=4096):
    moe = n_exp > 1
    nc, P, tag = ctx.nc, ctx.P, ctx.tag
    NHALF = 2
    HT = ntok // NHALF
    NT = HT // 128
    NTB = HT // 512
    xin = ctx.xin
    Gd = ctx.G
    S_ = ctx.S
    sel_d = ctx.sel_d
    wout_d = nc.dram_tensor(tag + "wout_l", [128, 8 * D], F32, kind="ExternalInput").ap()
    gain_d = nc.dram_tensor(tag + "gain_l", [128, 8], F32, kind="ExternalInput").ap()
    ident_d = nc.dram_tensor(tag + "ident", [128, 128], F32, kind="ExternalInput").ap()
    wg_d = nc.dram_tensor(tag + "wg_l", [n_exp * NFG, 128, 2048], F32, kind="ExternalInput").ap()
    wu_d = nc.dram_tensor(tag + "wu_l", [n_exp * NFG, 128, 2048], F32, kind="ExternalInput").ap()
    wd_d = nc.dram_tensor(tag + "wd_l", [n_exp * NFG, 128, 2048], F32, kind="ExternalInput").ap()
    if moe:
        router_d = nc.dram_tensor(tag + "router_l", [128, 8 * 8], F32, kind="ExternalInput").ap()
    y = ctx.out

    identf = P.sb([128, 128], F32, "identf")
    identb = P.sb([128, 128], BF16, "identb")
    gain = P.sb([128, 8], F32, "gain")
    P.dma(identf[:], ident_d, writes=[identf])
    P.cp("dve", identb[:], identf[:], [identf], [identb])
    P.dma(gain[:], gain_d, writes=[gain])
    if moe:
        router = P.sb([128, 8, 8], F32, "router")
        P.dma(router[:].rearrange("p c e -> p (c e)"), router_d, writes=[router])

    sel = P.sb([128, 2], F32, "sel")
    P.dma(sel[:], sel_d, writes=[sel])
    acc_t = P.sb([128, NT, D], F32, "acc")
    acc = [P.sub(acc_t[:, t, :], f"acc{t}") for t in range(NT)]
    hT_t = P.sb([128, 8, HT], BF16, "hT")
    hT = [P.sub(hT_t[:, :, tb * 512:(tb + 1) * 512], f"hT{tb}") for tb in range(NTB)]
    gates_t = P.sb([128, NT, 8], F32, "gates")
    gates = [P.sub(gates_t[:, t, :], f"gates{t}") for t in range(NT)]
    wbuf = [P.sb([128, 6144], BF16, f"wbuf{i}") for i in range(2)]
    wgb = [P.sub(wbuf[i][:, 0:2048], f"wg{i}") for i in range(2)]
    wub = [P.sub(wbuf[i][:, 2048:4096], f"wu{i}") for i in range(2)]
    wdb = [P.sub(wbuf[i][:, 4096:6144], f"wd{i}") for i in range(2)]
    stage = [P.sb([128, 2048], F32, f"stage{i}") for i in range(3)]
    woutb = P.sb([128, 8, D], BF16, "woutb")
    actT = [[P.sb([128, 512], BF16, f"actT{i}{j}") for j in range(2)] for i in range(2)]
    sil = [P.sb([128, 512], BF16, f"sil{j}") for j in range(2)]
    X = [P.sb([128, D], F32, f"X{i}") for i in range(2)]
    M = [P.sb([128, D], F32, f"M{i}") for i in range(1)] * 2
    C0 = P.sb([128, D], F32, "C0")
    C1 = P.sb([128, D], F32, "C1")
    MB = P.sb([128, D], BF16, "MB")
    mixT = P.sb([128, 8, 128], BF16, "mixT")
    ss = P.sb([128, 1], F32, "ss")
    xnb = P.sb([128, D], BF16, "xnb")
    if moe:
        xn32 = P.sb([128, D], F32, "xn32")
        hT32 = P.sb([128, 8, 128], F32, "hT32")
        lg = P.sb([128, 8], F32, "lg")
        top8 = P.sb([128, 8], F32, "top8")
        msk = P.sb([128, 8], F32, "msk")
        wexp = P.sb([128, 8], F32, "wexp")
        den = P.sb([128, 1], F32, "den")
        negv1 = P.sb([128, 1], F32, "negv1")

    pg = [P.ps([128, 512], F32, f"pg{j}") for j in range(2)]
    pu = [P.ps([128, 512], F32, f"pu{j}") for j in range(2)]
    pd = [P.ps([128, 512], F32, f"pd{j}") for j in range(2)]
    ptr = P.ps([128, 8, 128], BF16, "ptr")
    pm = P.ps([128, 512], F32, "pm")

    for q in range(4):
        st = stage[q % 3]
        P.dma(st[:], wout_d[:, q * 2048:(q + 1) * 2048], writes=[st])
        P.cp("pool", woutb[:].rearrange("p c n -> p (c n)")[:, q * 2048:(q + 1) * 2048], st[:], [st], [woutb])

    gidx = 0
    for half in range(NHALF):
        r0 = half * HT
        for t in range(NT):
            Xt, Mt = X[t % 2], M[t % 2]
            rows = slice(r0 + t * 128, r0 + (t + 1) * 128)
            P.dma(Xt[:], xin[rows, :], writes=[Xt])
            rr = r0 + t * 128
            for p_ in range(2):
                a0 = ctx.gaddr(p_, rr)
                a1 = ctx.gaddr(p_, ntok + rr)
                P.dma(C0[:, p_ * 512:(p_ + 1) * 512], Gd[a0:a0 + 128, :], writes=[C0])
                P.dma(C1[:, p_ * 512:(p_ + 1) * 512], Gd[a1:a1 + 128, :], writes=[C1])
            P.ts("dve", Mt[:], C0[:], sel[:, 0:1], ALU.mult, [C0, sel], [Mt])
            P.stt("dve", Mt[:], C1[:], sel[:, 1:2], Mt[:], ALU.mult, ALU.add, [C1, sel, Mt], [Mt])
            P.cp("act", MB[:], Mt[:], [Mt], [MB])
            for c in range(8):
                P.tr(ptr[:, c, :], MB[:, c * 128:(c + 1) * 128], identb[:], [MB, identb], [ptr])
            P.cp("dve", mixT[:], ptr[:], [ptr], [mixT])
            A = acc[t]
            for hf in range(2):
                po = pd[hf]
                for c in range(8):
                    P.mm(po[:], mixT[:, c, :], woutb[:, c, hf * 512:(hf + 1) * 512], c == 0, c == 7,
                         [mixT, woutb], [po])
                P.tt("dve", A[:, hf * 512:(hf + 1) * 512], Xt[:, hf * 512:(hf + 1) * 512], po[:], ALU.add,
                     [Xt, po], [A])
            P.actv(Mt[:], A[:], AF.Square, [A], [Mt, ss], accum_out=ss[:])
            P.actv(ss[:], ss[:], AF.Ln, [ss], [ss], scale=1.0 / D, bias=EPS)
            P.actv(ss[:], ss[:], AF.Exp, [ss], [ss], scale=-0.5)
            P.ts("dve", xnb[:], A[:], ss[:, 0:1], ALU.mult, [A, ss], [xnb])
            for c in range(8):
                P.tr(ptr[:, c, :], xnb[:, c * 128:(c + 1) * 128], identb[:], [xnb, identb], [ptr])
            H = hT[t // 4]
            tok = slice((t % 4) * 128, (t % 4 + 1) * 128)
            P.tt("dve", H[:, :, tok], ptr[:], gain[:].unsqueeze(2).to_broadcast([128, 8, 128]), ALU.mult,
                 [ptr, gain], [H])
            if moe:
                G = gates[t]
                P.ts("dve", xn32[:], A[:], ss[:, 0:1], ALU.mult, [A, ss], [xn32])
                for rnd in range(2):
                    for c4 in range(4):
                        c = rnd * 4 + c4
                        P.tr(pm[:, c4 * 128:(c4 + 1) * 128], xn32[:, c * 128:(c + 1) * 128], identf[:],
                             [xn32, identf], [pm])
                    P.tt("dve", hT32[:, rnd * 4:(rnd + 1) * 4, :], pm[:].rearrange("p (c t) -> p c t", c=4),
                         gain[:, rnd * 4:(rnd + 1) * 4].unsqueeze(2).to_broadcast([128, 4, 128]), ALU.mult,
                         [pm, gain], [hT32])
                for c in range(8):
                    P.mm(pm[:, 0:8], hT32[:, c, :], router[:, c, :], c == 0, c == 7, [hT32, router], [pm])
                P.cp("dve", lg[:], pm[:, 0:8], [pm], [lg])
                P.op("dve", lambda e: e.max(out=top8[:], in_=lg[:]), [lg], [top8])
                P.ts("dve", msk[:], lg[:], top8[:, 1:2], ALU.is_ge, [lg, top8], [msk])
                P.ts("dve", negv1[:], top8[:, 0:1], -1.0, ALU.mult, [top8], [negv1])
                P.actv(wexp[:], lg[:], AF.Exp, [lg, negv1], [wexp], bias=negv1[:, 0:1], scale=1.0)
                P.tt("dve", wexp[:], wexp[:], msk[:], ALU.mult, [wexp, msk], [wexp])
                P.op("dve", lambda e: e.reduce_sum(out=den[:], in_=wexp[:], axis=AX.X), [wexp], [den])
                P.op("dve", lambda e: e.reciprocal(out=den[:], in_=den[:]), [den], [den])
                P.ts("dve", G[:], wexp[:], den[:, 0:1], ALU.mult, [wexp, den], [G])

        pend = None

        def emit_down(job):
            k, e, tb, AT = job
            for tt_ in range(4):
                t = tb * 4 + tt_
                A = acc[t]
                for hf in range(2):
                    po = pd[hf]
                    for j in range(2):
                        P.mm(po[:], AT[j][:, tt_ * 128:(tt_ + 1) * 128],
                             wdb[k][:, j * 1024 + hf * 512: j * 1024 + (hf + 1) * 512], j == 0, j == 1,
                             [AT[j], wdb[k]], [po])
                    dst = A[:, hf * 512:(hf + 1) * 512]
                    if moe:
                        P.stt("dve", dst, po[:], gates[t][:, e:e + 1], dst, ALU.mult, ALU.add,
                              [po, gates[t], A], [A])
                    else:
                        P.tt("dve", dst, dst, po[:], ALU.add, [po, A], [A])
                    yield

        it = 0
        for e in range(n_exp):
            for fg in range(NFG):
                k = gidx % 2
                g = e * NFG + fg
                for wi, (src, dstb, ceng) in enumerate(((wg_d, wgb[k], "pool"), (wu_d, wub[k], "pool"),
                                                        (wd_d, wdb[k], "act"))):
                    st = stage[(gidx * 3 + wi) % 3]
                    P.dma(st[:], src[g], writes=[st])
                    P.cp(ceng, dstb[:], st[:], [st], [dstb])
                gidx += 1
                for tb in range(NTB):
                    AT = actT[it % 2]
                    it += 1
                    H = hT[tb]
                    dgen = emit_down(pend) if pend is not None else iter(())
                    nmm = 0
                    for j in range(2):
                        for c in range(8):
                            P.mm(pg[j][:], wgb[k][:, c * 256 + j * 128: c * 256 + (j + 1) * 128], H[:, c, :],
                                 c == 0, c == 7, [wgb[k], H], [pg[j]])
                            nmm += 1
                            if nmm % 4 == 0:
                                next(dgen, None)
                        for c in range(8):
                            P.mm(pu[j][:], wub[k][:, c * 256 + j * 128: c * 256 + (j + 1) * 128], H[:, c, :],
                                 c == 0, c == 7, [wub[k], H], [pu[j]])
                            nmm += 1
                            if nmm % 4 == 0:
                                next(dgen, None)
                        P.actv(sil[j][:], pg[j][:], AF.Silu, [pg[j]], [sil[j]])
                        P.tt("dve", AT[j][:], sil[j][:], pu[j][:], ALU.mult, [sil[j], pu[j]], [AT[j]])
                    for _ in dgen:
                        pass
                    pend = (k, e, tb, AT)
        for _ in emit_down(pend):
            pass
        pend = None
        for t in range(NT):
            P.dma(y[r0 + t * 128: r0 + (t + 1) * 128, :], acc[t][:], reads=[acc[t]])
    return


def ffn_layouts(w_out, gain, w_gate, w_up, w_down, router=None):
    E = w_gate.shape[0]
    d = {}
    d["wout_l"] = np.ascontiguousarray(w_out.reshape(8, 128, D).transpose(1, 0, 2).reshape(128, 8 * D))
    d["gain_l"] = np.ascontiguousarray(gain.reshape(8, 128).T)
    d["ident"] = np.eye(128, dtype=np.float32)

    def gu(w):
        return np.ascontiguousarray(
            w.reshape(E, 8, 128, NFG, 256).transpose(0, 3, 2, 1, 4).reshape(E * NFG, 128, 2048))
    d["wg_l"] = gu(w_gate)
    d["wu_l"] = gu(w_up)
    d["wd_l"] = np.ascontiguousarray(
        w_down.reshape(E, NFG, 2, 128, D).transpose(0, 1, 3, 2, 4).reshape(E * NFG, 128, 2048))
    if router is not None:
        d["router_l"] = np.ascontiguousarray(router.reshape(8, 128, 8).transpose(1, 0, 2).reshape(128, 64))
    return d


D = 1024
EPS = 1e-6
SEQ = 8192


class FrontEnd:
    def __init__(self, P, x_d, gain, identb, ptr, nx=2, rowmap=None):
        self.P, self.x_d, self.gain, self.identb, self.ptr = P, x_d, gain, identb, ptr
        self.X = [P.sb([128, D], F32, f"feX{i}") for i in range(nx)]
        self.ss = P.sb([128, 1], F32, "fess")
        self.xnb = P.sb([128, D], BF16, "fexnb")
        self.n = 0
        self.rowmap = rowmap

    def block(self, blk, H):
        P = self.P
        for tt_ in range(4):
            Xt = self.X[self.n % len(self.X)]
            self.n += 1
            r = blk * 512 + tt_ * 128
            if self.rowmap is not None:
                r = self.rowmap(r)
            P.dma(Xt[:], self.x_d[r:r + 128, :], writes=[Xt])
            P.actv(self.xnb[:], Xt[:], AF.Square, [Xt], [self.xnb, self.ss], accum_out=self.ss[:])
            P.actv(self.ss[:], self.ss[:], AF.Ln, [self.ss], [self.ss], scale=1.0 / D, bias=EPS)
            P.actv(self.ss[:], self.ss[:], AF.Exp, [self.ss], [self.ss], scale=-0.5)
            P.ts("dve", self.xnb[:], Xt[:], self.ss[:, 0:1], ALU.mult, [Xt, self.ss], [self.xnb])
            for c in range(8):
                P.tr(self.ptr[:, c, :], self.xnb[:, c * 128:(c + 1) * 128], self.identb[:],
                     [self.xnb, self.identb], [self.ptr])
            P.tt("dve", H[:, :, tt_ * 128:(tt_ + 1) * 128], self.ptr[:],
                 self.gain[:].unsqueeze(2).to_broadcast([128, 8, 128]), ALU.mult, [self.ptr, self.gain], [H])


def load_w_bf16(P, dst, src_d, ncols, stage, eng="pool"):
    tot = 8 * ncols
    flat = dst[:].rearrange("p c n -> p (c n)")
    q = 0
    i = 0
    SW = stage[0].t.shape[1]
    while q < tot:
        w = min(SW, tot - q)
        st = stage[i % len(stage)]
        P.dma(st[:, 0:w], src_d[:, q:q + w], writes=[st])
        P.cp(eng, flat[:, q:q + w], st[:, 0:w], [st], [dst])
        q += w
        i += 1


def emit_ssd(ctx, seq=SEQ):
    nc, P, tag = ctx.nc, ctx.P, ctx.tag
    NBLK = seq // 512
    x_d = ctx.x_d
    gain_d = nc.dram_tensor(tag + "gain_l", [128, 8], F32, kind="ExternalInput").ap()
    ident_d = nc.dram_tensor(tag + "ident", [128, 128], F32, kind="ExternalInput").ap()
    wch_d = nc.dram_tensor(tag + "wch_l", [128, 8 * 512], F32, kind="ExternalInput").ap()
    wzd_d = nc.dram_tensor(tag + "wzd_l", [128, 8 * 260], F32, kind="ExternalInput").ap()
    convw_d = nc.dram_tensor(tag + "convw_l", [128, 4 * 4], F32, kind="ExternalInput").ap()
    convb_d = nc.dram_tensor(tag + "convb_l", [128, 4], F32, kind="ExternalInput").ap()
    hp_d = nc.dram_tensor(tag + "hp_l", [128, 12], F32, kind="ExternalInput").ap()
    ng_d = nc.dram_tensor(tag + "ng_l", [128, 256], F32, kind="ExternalInput").ap()
    tri_d = nc.dram_tensor(tag + "tri", [128, 384], F32, kind="ExternalInput").ap()
    out = ctx.out

    identf = P.sb([128, 128], F32, "identf")
    identb = P.sb([128, 128], BF16, "identb")
    gain = P.sb([128, 8], F32, "gain")
    convw = P.sb([128, 4, 4], F32, "convw")
    convb = P.sb([128, 4], F32, "convb")
    hp = P.sb([128, 12], F32, "hp")
    ng = P.sb([128, 256], F32, "ng")
    trif = P.sb([128, 384], F32, "trif")
    trib = P.sb([128, 384], BF16, "trib")
    P.dma(identf[:], ident_d, writes=[identf])
    P.cp("dve", identb[:], identf[:], [identf], [identb])
    P.dma(gain[:], gain_d, writes=[gain])
    P.dma(convw[:].rearrange("p a b -> p (a b)"), convw_d, writes=[convw])
    P.dma(convb[:], convb_d, writes=[convb])
    P.dma(hp[:], hp_d, writes=[hp])
    P.dma(ng[:], ng_d, writes=[ng])
    P.dma(trif[:], tri_d, writes=[trif])
    P.cp("dve", trib[:], trif[:], [trif], [trib])
    tri = trib[:, 0:128]
    upp = trif[:, 128:256]
    ones = trib[:, 256:384]
    aneg = P.sb([128, 4], F32, "aneg")
    P.actv(aneg[:], hp[:, 4:8], AF.Exp, [hp], [aneg])
    P.ts("dve", aneg[:], aneg[:], -1.0, ALU.mult, [aneg], [aneg])
    cmask = trif[:, 0:128]

    stage = [P.sb([128, 2048], F32, f"stage{i}") for i in range(2)]
    wch = P.sb([128, 8, 512], BF16, "wch")
    wzd = P.sb([128, 8, 260], BF16, "wzd")
    load_w_bf16(P, wch, wch_d, 512, stage)
    load_w_bf16(P, wzd, wzd_d, 260, stage)

    ptr = P.ps([128, 8, 128], BF16, "ptr")
    pYS = P.ps([128, 512], F32, "pYS")
    pch = [pYS, pYS]
    pZA_2 = [P.ps([128, 512], F32, f"pZA{i}") for i in range(2)]
    pD_2 = [P.ps([128, 512], F32, f"pD{i}") for i in range(2)]
    pYd = [P.ps([128, 512], F32, f"pYd{i}") for i in range(2)]

    fe = FrontEnd(P, x_d, gain, identb, ptr, rowmap=getattr(ctx, 'rowmap', None))
    hTb = [P.sb([128, 8, 512], BF16, f"hT{i}") for i in range(2)]
    xpre = P.sb([128, 4, 515], F32, "xpre")
    xpre_ct = [P.sub(xpre[:, ct, :], f"xpre{ct}") for ct in range(4)]
    P.op("pool", lambda e: e.memset(xpre[:], 0.0), [], xpre_ct)
    cacc = [P.sb([128, 512], F32, f"cacc{i}") for i in range(2)]
    xc = [P.sb([128, 4, 512], BF16, f"xc{i}") for i in range(2)]
    S32 = P.sb([128, 256], F32, "S32")
    Sb = P.sb([128, 256], BF16, "Sb")
    P.op("pool", lambda e: e.memset(S32[:], 0.0), [], [S32])
    P.op("pool", lambda e: e.memset(Sb[:], 0.0), [], [Sb])
    xtok_2 = [P.sb([128, 256], BF16, f"xtok{_i}") for _i in range(2)]
    btok_2 = [P.sb([128, 128], BF16, f"btok{_i}") for _i in range(2)]
    zd_2 = [P.sb([128, 260], F32, f"zd{_i}") for _i in range(2)]
    dt_2 = [P.sb([128, 4], F32, f"dt{_i}") for _i in range(2)]
    ad_2 = [P.sb([128, 4], F32, f"ad{_i}") for _i in range(2)]
    adh_2 = [P.sb([128, 4], BF16, f"adh{_i}") for _i in range(2)]
    adl_2 = [P.sb([128, 4], BF16, f"adl{_i}") for _i in range(2)]
    cs_2 = [P.sb([128, 8], F32, f"cs{_i}") for _i in range(2)]
    ec_2 = [P.sb([128, 4], F32, f"ec{_i}") for _i in range(2)]
    ed_2 = [P.sb([128, 4], F32, f"ed{_i}") for _i in range(2)]
    et_2 = [P.sb([128, 4], F32, f"et{_i}") for _i in range(2)]
    wdt_2 = [P.sb([128, 4], F32, f"wdt{_i}") for _i in range(2)]
    Ah_2 = [P.sb([128, 4, 128], BF16, f"Ah{_i}") for _i in range(2)]
    Al_2 = [P.sb([128, 4, 128], BF16, f"Al{_i}") for _i in range(2)]
    Aful_2 = [P.sb([128, 4, 128], F32, f"Aful{_i}") for _i in range(2)]
    E_2 = [P.sb([128, 4, 128], F32, f"E{_i}") for _i in range(2)]
    Gm_2 = [P.sb([128, 128], F32, f"Gm{_i}") for _i in range(2)]
    Mh_2 = [P.sb([128, 4, 128], BF16, f"Mh{_i}") for _i in range(2)]
    xw_2 = [P.sb([128, 256], BF16, f"xw{_i}") for _i in range(2)]
    y1_2 = [P.sb([128, 256], F32, f"y1{_i}") for _i in range(2)]
    y2_2 = [P.sb([128, 256], F32, f"y2{_i}") for _i in range(2)]
    sz_2 = [P.sb([128, 256], F32, f"sz{_i}") for _i in range(2)]
    junk_2 = [P.sb([128, 256], F32, f"junk{_i}") for _i in range(2)]
    ssq_2 = [P.sb([128, 1], F32, f"ssq{_i}") for _i in range(2)]
    yo = [P.sb([128, 256], F32, f"yo{i}") for i in range(2)]

    for blk in range(NBLK):
        H = hTb[blk % 2]
        fe.block(blk, H)
        XC = xc[blk % 2]
        for ct in range(4):
            pc = pch[ct % 2]
            for c in range(8):
                P.mm(pc[:], wch[:, c, ct * 128:(ct + 1) * 128], H[:, c, :], c == 0, c == 7, [wch, H], [pc])
            xp = xpre_ct[ct]
            P.cp("act", xp[:, 3:515], pc[:], [pc], [xp])
            ca = cacc[ct % 2]
            P.ts("dve", ca[:], xp[:, 0:512], convw[:, ct, 0:1], ALU.mult, [xp, convw], [ca])
            for w in range(1, 4):
                P.stt("dve", ca[:], xp[:, w:w + 512], convw[:, ct, w:w + 1], ca[:], ALU.mult, ALU.add,
                      [xp, convw, ca], [ca])
            P.actv(XC[:, ct, :], ca[:], AF.Silu, [ca, convb], [XC], bias=convb[:, ct:ct + 1], scale=1.0)
            P.cp("pool", xp[:, 0:3], xp[:, 512:515], [xp], [xp])
        def do_tile(blk, tt_, H, XC):
            tok = slice(tt_ * 128, (tt_ + 1) * 128)
            r = blk * 512 + tt_ * 128
            pYd_ = pYd[tt_ % 2]
            pz = pZA_2[tt_ % 2]
            pA = pz
            pD = pD_2[tt_ % 2]
            po_ = 4 * (tt_ % 2)
            xtok = xtok_2[tt_ % 2]; btok = btok_2[tt_ % 2]; zd = zd_2[tt_ % 2]; dt = dt_2[tt_ % 2]; ad = ad_2[tt_ % 2]; adh = adh_2[tt_ % 2]; adl = adl_2[tt_ % 2]; cs = cs_2[tt_ % 2]; ec = ec_2[tt_ % 2]; ed = ed_2[tt_ % 2]; et = et_2[tt_ % 2]; wdt = wdt_2[tt_ % 2]; Ah = Ah_2[tt_ % 2]; Al = Al_2[tt_ % 2]; Aful = Aful_2[tt_ % 2]; E = E_2[tt_ % 2]; Gm = Gm_2[tt_ % 2]; Mh = Mh_2[tt_ % 2]; xw = xw_2[tt_ % 2]; y1 = y1_2[tt_ % 2]; y2 = y2_2[tt_ % 2]; sz = sz_2[tt_ % 2]; junk = junk_2[tt_ % 2]; ssq = ssq_2[tt_ % 2]
            yield
            for c in range(8):
                P.mm(pz[:, 0:260], H[:, c, tok], wzd[:, c, :], c == 0, c == 7, [H, wzd], [pz])
            yield
            P.cp("act", zd[:], pz[:, 0:260], [pz], [zd])
            yield
            P.tt("dve", dt[:], zd[:, 256:260], hp[:, 0:4], ALU.add, [zd, hp], [dt])
            yield
            P.actv(dt[:], dt[:], AF.Exp, [dt], [dt])
            yield
            P.actv(dt[:], dt[:], AF.Ln, [dt], [dt], bias=1.0, scale=1.0)
            yield
            P.tt("dve", ad[:], dt[:], aneg[:], ALU.mult, [dt, aneg], [ad])
            yield
            P.cp("dve", adh[:], ad[:], [ad], [adh])
            yield
            P.tt("dve", adl[:], ad[:], adh[:], ALU.subtract, [ad, adh], [adl])
            yield
            P.tr(ptr[:, po_ + 0, :], XC[:, 0, tok], identb[:], [XC, identb], [ptr])
            yield
            P.tr(ptr[:, po_ + 1, :], XC[:, 1, tok], identb[:], [XC, identb], [ptr])
            yield
            P.tr(ptr[:, po_ + 2, :], XC[:, 2, tok], identb[:], [XC, identb], [ptr])
            yield
            P.cp("act", xtok[:].rearrange("p (a b) -> p a b", a=2), ptr[:, po_:po_ + 2, :], [ptr], [xtok])
            yield
            P.cp("act", btok[:], ptr[:, po_ + 2, :], [ptr], [btok])
            yield
            P.mm(pA[:, 260:388], XC[:, 2, tok], XC[:, 3, tok], True, True, [XC], [pA])
            yield
            P.mm(pA[:, 388:392], tri, adh[:], True, False, [trib, adh], [pA])
            yield
            P.mm(pA[:, 388:392], tri, adl[:], False, True, [trib, adl], [pA])
            yield
            P.mm(pA[:, 392:396], ones, adh[:], True, False, [trib, adh], [pA])
            yield
            P.mm(pA[:, 392:396], ones, adl[:], False, True, [trib, adl], [pA])
            yield
            P.cp("dve", cs[:], pA[:, 388:396], [pA], [cs])
            yield
            P.tt("dve", Gm[:], pA[:, 260:388], cmask, ALU.mult, [pA, trif], [Gm])
            yield
            P.actv(ec[:], cs[:, 0:4], AF.Exp, [cs], [ec])
            yield
            P.actv(et[:], cs[:, 4:8], AF.Exp, [cs], [et])
            yield
            P.tt("dve", ed[:], cs[:, 4:8], cs[:, 0:4], ALU.subtract, [cs], [ed])
            yield
            P.actv(ed[:], ed[:], AF.Exp, [ed], [ed])
            yield
            P.tt("dve", wdt[:], ed[:], dt[:], ALU.mult, [ed, dt], [wdt])
            yield
            for h in range(4):
                P.ts("dve", Aful[:, h, :], upp, ad[:, h:h + 1], ALU.mult, [trif, ad], [Aful])
            yield
            P.cp("dve", Ah[:], Aful[:], [Aful], [Ah])
            yield
            P.tt("dve", Al[:], Aful[:], Ah[:], ALU.subtract, [Aful, Ah], [Al])
            yield
            for h in range(4):
                P.mm(pD[:, h * 128:(h + 1) * 128], Ah[:, h, :], tri, True, False, [Ah, trib], [pD])
                P.mm(pD[:, h * 128:(h + 1) * 128], Al[:, h, :], tri, False, True, [Al, trib], [pD])
            yield
            P.actv(E[:].rearrange("p a b -> p (a b)"), pD[:], AF.Exp, [pD], [E])
            yield
            for h in range(4):
                P.stt("dve", Mh[:, h, :], E[:, h, :], dt[:, h:h + 1], Gm[:], ALU.mult, ALU.mult,
                      [E, dt, Gm], [Mh])
            yield
            for h in range(4):
                P.mm(pYd_[:, h * 64:(h + 1) * 64], Mh[:, h, :], xtok[:, h * 64:(h + 1) * 64], True, True,
                     [Mh, xtok], [pYd_])
            yield
            P.tt("dve", xw[:].rearrange("p (h d) -> p h d", h=4), xtok[:].rearrange("p (h d) -> p h d", h=4),
                 wdt[:].unsqueeze(2).to_broadcast([128, 4, 64]), ALU.mult, [xtok, wdt], [xw])

            def stage_b():
                P.mm(pYS[:, 0:256], XC[:, 3, tok], Sb[:], True, True, [XC, Sb], [pYS])
                P.mm(pYS[:, 256:512], btok[:], xw[:], True, True, [btok, xw], [pYS])
                P.tt("dve", y1[:].rearrange("p (h d) -> p h d", h=4), pYS[:, 0:256].rearrange("p (h d) -> p h d", h=4),
                     ec[:].unsqueeze(2).to_broadcast([128, 4, 64]), ALU.mult, [pYS, ec], [y1])
                P.tt("dve", y1[:], y1[:], pYd_[:, 0:256], ALU.add, [y1, pYd_], [y1])
                P.tt("dve", S32[:].rearrange("p (h d) -> p h d", h=4), S32[:].rearrange("p (h d) -> p h d", h=4),
                     et[:].unsqueeze(2).to_broadcast([128, 4, 64]), ALU.mult, [S32, et], [S32])
                P.tt("dve", S32[:], S32[:], pYS[:, 256:512], ALU.add, [S32, pYS], [S32])
                P.cp("dve", Sb[:], S32[:], [S32], [Sb])
                P.tt("pool", y2[:].rearrange("p (h d) -> p h d", h=4), xtok[:].rearrange("p (h d) -> p h d", h=4),
                     hp[:, 8:12].unsqueeze(2).to_broadcast([128, 4, 64]), ALU.mult, [xtok, hp], [y2])
                P.tt("dve", y1[:], y1[:], y2[:], ALU.add, [y1, y2], [y1])
                P.actv(sz[:], zd[:, 0:256], AF.Silu, [zd], [sz])
                P.tt("dve", y1[:], y1[:], sz[:], ALU.mult, [y1, sz], [y1])
                P.actv(junk[:], y1[:], AF.Square, [y1], [junk, ssq], accum_out=ssq[:])
                P.actv(ssq[:], ssq[:], AF.Ln, [ssq], [ssq], scale=1.0 / 256, bias=EPS)
                P.actv(ssq[:], ssq[:], AF.Exp, [ssq], [ssq], scale=-0.5)
                YO = yo[tt_ % 2]
                P.stt("dve", YO[:], y1[:], ssq[:, 0:1], ng[:], ALU.mult, ALU.mult, [y1, ssq, ng], [YO])
                P.dma(out[r:r + 128, :], YO[:], reads=[YO])

            return stage_b

        for pr_ in range(2):
            gens = [do_tile(blk, 2 * pr_ + i_, H, XC) for i_ in range(2)]
            stage_bs = [None, None]
            live = [True, True]
            while any(live):
                for i_ in range(2):
                    if live[i_]:
                        try:
                            next(gens[i_])
                        except StopIteration as fin_:
                            stage_bs[i_] = fin_.value
                            live[i_] = False
            for fb_ in stage_bs:
                fb_()
    return


def ssd_consts():
    m = np.arange(128)
    tri = (m[:, None] <= m[None, :]).astype(np.float32)
    upp = (m[:, None] > m[None, :]).astype(np.float32)
    ones = np.ones((128, 128), np.float32)
    return {"tri": np.concatenate([tri, upp, ones], 1), "ident": np.eye(128, dtype=np.float32)}


def wl(w):
    n = w.shape[1]
    return np.ascontiguousarray(w.reshape(8, 128, n).transpose(1, 0, 2).reshape(128, 8 * n))


def rep(v, n=128):
    v = np.asarray(v, np.float32).reshape(1, -1)
    return np.ascontiguousarray(np.broadcast_to(v, (n, v.shape[1])))


def ssd_inputs(g, x_b, norm_gain, w_in, conv_w, conv_b, dt_bias, a_log, d_skip, ssm_norm_gain):
    o_z = 512 + 512 + 512
    o_xbc = o_z + 512
    o_dt = o_xbc + 1024
    zc = w_in[:, o_z + g * 256: o_z + (g + 1) * 256]
    xcols = w_in[:, o_xbc + g * 256: o_xbc + (g + 1) * 256]
    bcols = w_in[:, o_xbc + 512 + g * 128: o_xbc + 512 + (g + 1) * 128]
    ccols = w_in[:, o_xbc + 768 + g * 128: o_xbc + 768 + (g + 1) * 128]
    dtc = w_in[:, o_dt + g * 4: o_dt + (g + 1) * 4]
    chan = np.concatenate([np.arange(g * 256, (g + 1) * 256), 512 + np.arange(g * 128, (g + 1) * 128),
                           768 + np.arange(g * 128, (g + 1) * 128)])
    cw = conv_w[:, chan]
    cb = conv_b[chan]
    d = dict(ssd_consts())
    d["x"] = np.ascontiguousarray(x_b)
    d["gain_l"] = np.ascontiguousarray(norm_gain.reshape(8, 128).T)
    d["wch_l"] = wl(np.concatenate([xcols, bcols, ccols], 1))
    d["wzd_l"] = wl(np.concatenate([zc, dtc], 1))
    d["convw_l"] = np.ascontiguousarray(cw.reshape(4, 4, 128).transpose(2, 1, 0).reshape(128, 16))
    d["convb_l"] = np.ascontiguousarray(cb.reshape(4, 128).T)
    hs = slice(4 * g, 4 * g + 4)
    d["hp_l"] = rep(np.concatenate([dt_bias[hs], a_log[hs], d_skip[hs]]))
    d["ng_l"] = rep(ssm_norm_gain[g * 256:(g + 1) * 256])
    return d


NEG = -30000.0


def run_attention(P, steps, ST, PT, scale):
    n = len(steps)

    def do_qk(i):
        s = steps[i]
        st = ST[i % 2]
        m = len(s["qk"])
        for a, (l, r, rd) in enumerate(s["qk"]):
            P.mm(st[:], l, r, a == 0, a == m - 1, rd, [st])

    def do_pv(i):
        s = steps[i]
        st, pt = ST[i % 2], PT[i % 2]
        P.actv(pt[:], st[:], AF.Exp, [st], [pt], scale=scale)
        vr, vrd = s["v"]
        for (ob, oap, cs, first, last) in s["pv"]:
            P.op("pe", lambda e, oap=oap, l_=pt[:, cs], vr=vr, first=first, last=last: e.matmul(oap, lhsT=l_, rhs=vr, start=first, stop=last, skip_group_check=True), [pt] + vrd, [ob])
        if s.get("fin"):
            s["fin"]()

    pending = None
    for i in range(n):
        if pending is not None and steps[pending].get("sync"):
            do_pv(pending)
            pending = None
        do_qk(i)
        if pending is not None:
            do_pv(pending)
        pending = i
    do_pv(pending)


def causal_steps(nsb, qk_fn, v_fn, O_fn, fin_fn, identb, maskd, extra_fn=None):
    steps = []
    for sb in range(nsb):
        nk = 4 * sb + 4
        for kt in range(nk):
            t = kt - 4 * sb
            qk = list(qk_fn(sb, kt))
            if extra_fn is not None:
                qk += extra_fn(sb, kt)
            if t >= 0:
                qk.append((identb[:], maskd[:, t, :], [identb, maskd]))
            pv = []
            for j in range(4):
                if t > j:
                    continue
                ob, oap = O_fn(sb, j)
                pv.append((ob, oap, slice(j * 128, (j + 1) * 128), kt == 0 and j % 2 == 0, kt == 4 * sb + j))
            steps.append(dict(qk=qk, pv=pv, v=v_fn(kt), fin=(fin_fn(sb) if kt == nk - 1 else None)))
    return steps


def tok_l(a):
    n = a.shape[1]
    return np.ascontiguousarray(a.reshape(-1, 128, n).transpose(1, 0, 2).reshape(128, -1))


def diag_masks():
    k = np.arange(128)[:, None]
    q = np.arange(512)[None, :]
    m = np.stack([np.where(128 * t + k <= q, 0.0, NEG) for t in range(4)], 1)
    return np.ascontiguousarray(m.reshape(128, 4 * 512).astype(np.float32))


def rope_tables(seq, dim):
    inv = 1.0 / (10000.0 ** (np.arange(0, dim, 2, dtype=np.float32) / dim))
    ang = np.arange(seq, dtype=np.float32)[:, None] * inv[None, :].astype(np.float32)
    return np.cos(ang).astype(np.float32), np.sin(ang).astype(np.float32)


def emit_rmsrope(P, src32, nh, dh, gainrep, cos_ap, sin_ap, dst, scr, cs_reads, rope_cols=None):
    sq, ssq, qn, ta, tb = scr["sq"], scr["ssq"], scr["qn"], scr["ta"], scr["tb"]
    W = nh * dh
    hh = dh // 2
    v3 = lambda b: b[:, 0:W].rearrange("p (h d) -> p h d", h=nh)
    P.tt("pool", sq[:, 0:W], src32[:, 0:W], src32[:, 0:W], ALU.mult, [src32], [sq])
    P.op("dve", lambda e: e.reduce_sum(out=ssq[:, 0:nh], in_=v3(sq), axis=AX.X), [sq], [ssq])
    P.actv(ssq[:, 0:nh], ssq[:, 0:nh], AF.Ln, [ssq], [ssq], scale=1.0 / dh, bias=EPS)
    P.actv(ssq[:, 0:nh], ssq[:, 0:nh], AF.Exp, [ssq], [ssq], scale=-0.5)
    P.tt("dve", v3(qn), v3(src32), ssq[:, 0:nh].unsqueeze(2).to_broadcast([128, nh, dh]), ALU.mult,
         [src32, ssq], [qn])
    P.tt("pool", qn[:, 0:W], qn[:, 0:W], gainrep[:, 0:W], ALU.mult, [qn, gainrep], [qn])
    if cos_ap is None:
        P.cp("dve", dst[:, 0:W], qn[:, 0:W], [qn], [dst])
        return
    cb = cos_ap.unsqueeze(1).to_broadcast([128, nh, hh])
    sb_ = sin_ap.unsqueeze(1).to_broadcast([128, nh, hh])
    q3 = v3(qn)
    d3 = v3(dst)
    a3 = ta[:, 0:nh * hh].rearrange("p (h d) -> p h d", h=nh)
    b3 = tb[:, 0:nh * hh].rearrange("p (h d) -> p h d", h=nh)
    P.tt("dve", a3, q3[:, :, 0:hh], cb, ALU.mult, [qn] + cs_reads, [ta])
    P.tt("pool", b3, q3[:, :, hh:dh], sb_, ALU.mult, [qn] + cs_reads, [tb])
    P.tt("dve", d3[:, :, 0:hh], a3, b3, ALU.subtract, [ta, tb], [dst])
    P.tt("dve", a3, q3[:, :, hh:dh], cb, ALU.mult, [qn] + cs_reads, [ta])
    P.tt("pool", b3, q3[:, :, 0:hh], sb_, ALU.mult, [qn] + cs_reads, [tb])
    P.tt("dve", d3[:, :, hh:dh], a3, b3, ALU.add, [ta, tb], [dst])


def emit_diffattn(ctx, seq=SEQ, layer_idx=0):
    import math
    lam_init = 0.8 - 0.6 * math.exp(-0.3 * layer_idx)
    nc, P, tag = ctx.nc, ctx.P, ctx.tag
    NBLK = seq // 512
    NT = seq // 128
    x_d = ctx.x_d
    gain_d = nc.dram_tensor(tag + "gain_l", [128, 8], F32, kind="ExternalInput").ap()
    ident_d = nc.dram_tensor(tag + "ident", [128, 128], F32, kind="ExternalInput").ap()
    wqk_d = nc.dram_tensor(tag + "wqk_l", [128, 8 * 512], F32, kind="ExternalInput").ap()
    wv_d = nc.dram_tensor(tag + "wv_l", [128, 8 * 256], F32, kind="ExternalInput").ap()
    gqk_d = nc.dram_tensor(tag + "gqk_l", [128, 512], F32, kind="ExternalInput").ap()
    cos_d = nc.dram_tensor(tag + "cos", [128, NT * 32], F32, kind="ExternalInput").ap()
    sin_d = nc.dram_tensor(tag + "sin", [128, NT * 32], F32, kind="ExternalInput").ap()
    lam_d = nc.dram_tensor(tag + "lam_l", [128, 256], F32, kind="ExternalInput").ap()
    sg_d = nc.dram_tensor(tag + "sg_l", [128, 128], F32, kind="ExternalInput").ap()
    mask_d = nc.dram_tensor(tag + "maskd_d", [128, 4 * 512], F32, kind="ExternalInput").ap()
    out = ctx.out

    identf = P.sb([128, 128], F32, "identf")
    identb = P.sb([128, 128], BF16, "identb")
    gain = P.sb([128, 8], F32, "gain")
    gqk = P.sb([128, 512], F32, "gqk")
    lam = P.sb([128, 256], F32, "lam")
    sg = P.sb([128, 128], F32, "sg")
    cos_t = P.sb([128, NT, 32], F32, "cos_t")
    sin_t = P.sb([128, NT, 32], F32, "sin_t")
    maskd = P.sb([128, 4, 512], BF16, "maskd")
    stage = [P.sb([128, 2048], F32, f"stage{i}") for i in range(2)]
    P.dma(identf[:], ident_d, writes=[identf])
    P.cp("dve", identb[:], identf[:], [identf], [identb])
    P.dma(gain[:], gain_d, writes=[gain])
    P.dma(gqk[:], gqk_d, writes=[gqk])
    P.dma(lam[:], lam_d, writes=[lam])
    P.dma(sg[:], sg_d, writes=[sg])
    P.dma(cos_t[:].rearrange("p t d -> p (t d)"), cos_d, writes=[cos_t])
    P.dma(sin_t[:].rearrange("p t d -> p (t d)"), sin_d, writes=[sin_t])
    P.dma(stage[0][:], mask_d, writes=[stage[0]])
    P.cp("dve", maskd[:].rearrange("p a b -> p (a b)"), stage[0][:], [stage[0]], [maskd])
    P.ts("dve", sg[:], sg[:], 1.0 - lam_init, ALU.mult, [sg], [sg])
    lp = P.sb([128, 128], F32, "lp")
    ls = P.sb([128, 2], F32, "ls")
    neglam = P.sb([128, 1], F32, "neglam")
    P.tt("dve", lp[:, 0:64], lam[:, 0:64], lam[:, 64:128], ALU.mult, [lam], [lp])
    P.tt("dve", lp[:, 64:128], lam[:, 128:192], lam[:, 192:256], ALU.mult, [lam], [lp])
    P.op("dve", lambda e: e.reduce_sum(out=ls[:], in_=lp[:].rearrange("p (a b) -> p a b", a=2), axis=AX.X), [lp], [ls])
    P.actv(ls[:], ls[:], AF.Exp, [ls], [ls])
    P.tt("dve", neglam[:], ls[:, 1:2], ls[:, 0:1], ALU.subtract, [ls], [neglam])
    P.ts("dve", neglam[:], neglam[:], -lam_init, ALU.add, [neglam], [neglam])

    wqk = P.sb([128, 8, 512], BF16, "wqk")
    wv = P.sb([128, 8, 256], BF16, "wv")
    load_w_bf16(P, wqk, wqk_d, 512, stage)
    load_w_bf16(P, wv, wv_d, 256, stage)

    QT = P.sb([128, 2, seq], BF16, "QT")
    KT = P.sb([128, 2, seq], BF16, "KT")
    Vaug = P.sb([128, NT, 2, 129], BF16, "Vaug")
    P.op("dve", lambda e: e.memset(Vaug[:, :, :, 128:129], 1.0), [], [Vaug])

    ST = [P.ps([128, 512], F32, f"ST{i}") for i in range(2)]
    OA = [P.ps([128, 2, 256], F32, f"OA{i}") for i in range(2)]
    OB = [P.ps([128, 2, 256], F32, f"OB{i}") for i in range(2)]
    ptr = P.ps([128, 8, 128], BF16, "ptr")
    PT = [P.sb([128, 512], BF16, f"PT{i}") for i in range(2)]

    fe = FrontEnd(P, x_d, gain, identb, ptr, rowmap=getattr(ctx, 'rowmap', None))
    hTb = [P.sb([128, 8, 512], BF16, f"hT{i}") for i in range(2)]
    qk32_2 = [P.sb([128, 512], F32, f"qk32{i}") for i in range(2)]
    scr_2 = [dict(sq=P.sb([128, 512], F32, f"sq{i}"), ssq=P.sb([128, 8], F32, f"ssq{i}"), qn=P.sb([128, 512], F32, f"qn{i}"),
                  ta=P.sb([128, 256], F32, f"ta{i}"), tb=P.sb([128, 256], F32, f"tb{i}")) for i in range(2)]
    qkr_2 = [P.sb([128, 512], BF16, f"qkr{i}") for i in range(2)]

    for blk in range(NBLK):
        H = hTb[blk % 2]
        fe.block(blk, H)
        for tt_ in range(4):
            tok = slice(tt_ * 128, (tt_ + 1) * 128)
            ti = blk * 4 + tt_
            qk32, scr, qkr = qk32_2[ti % 2], scr_2[ti % 2], qkr_2[ti % 2]
            pq = OA[ti % 2]
            pv = OB[ti % 2]
            pqf = pq[:].rearrange("p a b -> p (a b)")
            pvf = pv[:].rearrange("p a b -> p (a b)")
            for c in range(8):
                P.mm(pqf, H[:, c, tok], wqk[:, c, :], c == 0, c == 7, [H, wqk], [pq])
            for c in range(8):
                P.mm(pvf[:, 0:256], H[:, c, tok], wv[:, c, :], c == 0, c == 7, [H, wv], [pv])
            P.cp("act", qk32[:], pqf, [pq], [qk32])
            P.cp("dve", Vaug[:, ti, :, 0:128], pvf[:, 0:256].rearrange("p (h d) -> p h d", h=2), [pv], [Vaug])
            emit_rmsrope(P, qk32, 8, 64, gqk, cos_t[:, ti, :], sin_t[:, ti, :], qkr, scr, [cos_t, sin_t])
            for g4 in range(4):
                P.tr(ptr[:, g4, :], qkr[:, g4 * 128:(g4 + 1) * 128], identb[:], [qkr, identb], [ptr])
            gt = slice(ti * 128, (ti + 1) * 128)
            P.cp("act", QT[:, :, gt], ptr[:, 0:2, :], [ptr], [QT])
            P.cp("act", KT[:, :, gt], ptr[:, 2:4, :], [ptr], [KT])

    tmpo = [P.sb([128, 4, 128], F32, f"tmpo{i}") for i in range(2)]
    rs = P.sb([128, 4], F32, "rs")
    o32 = P.sb([128, 128], F32, "o32")
    junk = P.sb([128, 128], F32, "junk")
    s1 = P.sb([128, 1], F32, "s1")
    yo = [P.sb([128, 128], F32, f"yo{i}") for i in range(4)]
    cnt = [0]
    scale = 64 ** -0.5
    for head in range(2):
        steps = []
        for sb in range(NBLK):
            for comp in range(2):
                Oset = OA if comp == 0 else OB
                ps = slice(comp * 64, (comp + 1) * 64)

                def qk_fn(sb_, kt, head=head, ps=ps):
                    return [(KT[ps, head, kt * 128:(kt + 1) * 128], QT[ps, head, sb_ * 512:(sb_ + 1) * 512], [KT, QT])]

                def v_fn(kt, head=head):
                    return (Vaug[:, kt, head, :], [Vaug])

                def O_fn(sb_, j, Oset=Oset):
                    return (Oset[j // 2], Oset[j // 2][:, j % 2, 0:129])

                def fin_fn(sb_, comp=comp, head=head, Oset=Oset):
                    def fin():
                        T = tmpo[sb_ % 2]
                        if comp == 0:
                            for j in range(4):
                                ob = Oset[j // 2]
                                P.op("dve", lambda e, ob=ob, j=j: e.reciprocal(out=rs[:, j:j + 1], in_=ob[:, j % 2, 128:129]),
                                     [ob], [rs])
                                P.ts("dve", T[:, j, :], ob[:, j % 2, 0:128], rs[:, j:j + 1], ALU.mult, [ob, rs], [T])
                        else:
                            for j in range(4):
                                ob = Oset[j // 2]
                                P.op("dve", lambda e, ob=ob, j=j: e.reciprocal(out=rs[:, j:j + 1], in_=ob[:, j % 2, 128:129]),
                                     [ob], [rs])
                                P.tt("dve", rs[:, j:j + 1], rs[:, j:j + 1], neglam[:], ALU.mult, [rs, neglam], [rs])
                                P.stt("dve", o32[:], ob[:, j % 2, 0:128], rs[:, j:j + 1], T[:, j, :], ALU.mult, ALU.add,
                                      [ob, rs, T], [o32])
                                P.actv(junk[:], o32[:], AF.Square, [o32], [junk, s1], accum_out=s1[:])
                                P.actv(s1[:], s1[:], AF.Ln, [s1], [s1], scale=1.0 / 128, bias=EPS)
                                P.actv(s1[:], s1[:], AF.Exp, [s1], [s1], scale=-0.5)
                                Y = yo[cnt[0] % 4]
                                cnt[0] += 1
                                P.stt("dve", Y[:], o32[:], s1[:, 0:1], sg[:], ALU.mult, ALU.mult, [o32, s1, sg], [Y])
                                r = sb_ * 512 + j * 128
                                P.dma(out[r:r + 128, head * 128:(head + 1) * 128], Y[:], reads=[Y])
                    return fin

                nk = 4 * sb + 4
                for kt in range(nk):
                    t = kt - 4 * sb
                    qk = qk_fn(sb, kt)
                    if t >= 0:
                        qk.append((identb[:], maskd[:, t, :], [identb, maskd]))
                    pv = []
                    for j in range(4):
                        if t > j:
                            continue
                        ob, oap = O_fn(sb, j)
                        pv.append((ob, oap, slice(j * 128, (j + 1) * 128), kt == 0 and j % 2 == 0, kt == 4 * sb + j))
                    steps.append(dict(qk=qk, pv=pv, v=v_fn(kt), fin=(fin_fn(sb) if kt == nk - 1 else None)))
        run_attention(P, steps, ST, PT, scale)
    return


def diffattn_inputs(hp, x_b, norm_gain, w_in, q_gain, k_gain, lam, subln_gain, seq):
    q = w_in[:, hp * 256:(hp + 1) * 256]
    k = w_in[:, 512 + hp * 256: 512 + (hp + 1) * 256]
    v = w_in[:, 1024 + hp * 256: 1024 + (hp + 1) * 256]
    cos, sin = rope_tables(seq, 64)
    d = {"ident": np.eye(128, dtype=np.float32)}
    d["x"] = np.ascontiguousarray(x_b)
    d["gain_l"] = np.ascontiguousarray(norm_gain.reshape(8, 128).T)
    d["wqk_l"] = wl(np.concatenate([q, k], 1))
    d["wv_l"] = wl(v)
    d["gqk_l"] = rep(np.concatenate([np.tile(q_gain, 4), np.tile(k_gain, 4)]))
    d["cos"] = tok_l(cos)
    d["sin"] = tok_l(sin)
    d["lam_l"] = rep(lam.reshape(-1))
    d["sg_l"] = rep(subln_gain)
    d["maskd_d"] = diag_masks()
    return d


def rmsrope3(P, src3, src_reads, nh, dh, gain3, gain_reads, cos_ap, sin_ap, cs_reads, dsts, scr):
    sq, ssq, qn, ta, tb = scr["sq"], scr["ssq"], scr["qn"], scr["ta"], scr["tb"]
    W = nh * dh
    hh = dh // 2
    v3 = lambda b, w=dh: b[:, 0:nh * w].rearrange("p (h d) -> p h d", h=nh)
    P.tt("pool", v3(sq), src3, src3, ALU.mult, src_reads, [sq])
    P.op("dve", lambda e: e.reduce_sum(out=ssq[:, 0:nh], in_=v3(sq), axis=AX.X), [sq], [ssq])
    P.actv(ssq[:, 0:nh], ssq[:, 0:nh], AF.Ln, [ssq], [ssq], scale=1.0 / dh, bias=EPS)
    P.actv(ssq[:, 0:nh], ssq[:, 0:nh], AF.Exp, [ssq], [ssq], scale=-0.5)
    P.tt("dve", v3(qn), src3, ssq[:, 0:nh].unsqueeze(2).to_broadcast([128, nh, dh]), ALU.mult,
         src_reads + [ssq], [qn])
    if cos_ap is None:
        for (d3, db) in dsts:
            P.tt("pool", d3, v3(qn), gain3, ALU.mult, [qn] + gain_reads, [db])
        return
    P.tt("pool", v3(qn), v3(qn), gain3, ALU.mult, [qn] + gain_reads, [qn])
    cb = cos_ap.unsqueeze(1).to_broadcast([128, nh, hh])
    sb_ = sin_ap.unsqueeze(1).to_broadcast([128, nh, hh])
    q3 = v3(qn)
    a3 = v3(ta, hh)
    b3 = v3(tb, hh)
    P.tt("dve", a3, q3[:, :, 0:hh], cb, ALU.mult, [qn] + cs_reads, [ta])
    P.tt("pool", b3, q3[:, :, hh:dh], sb_, ALU.mult, [qn] + cs_reads, [tb])
    for (d3, db) in dsts:
        P.tt("dve", d3[:, :, 0:hh], a3, b3, ALU.subtract, [ta, tb], [db])
    P.tt("dve", a3, q3[:, :, hh:dh], cb, ALU.mult, [qn] + cs_reads, [ta])
    P.tt("pool", b3, q3[:, :, 0:hh], sb_, ALU.mult, [qn] + cs_reads, [tb])
    for (d3, db) in dsts:
        P.tt("dve", d3[:, :, hh:dh], a3, b3, ALU.add, [ta, tb], [db])


class BankStart:
    def __init__(self):
        self.started = {}

    def new_round(self, buf):
        self.started[id(buf)] = False

    def flag(self, buf):
        if not self.started.get(id(buf), False):
            self.started[id(buf)] = True
            return True
        return False


def emit_mla(ctx, seq=SEQ):
    nc, P, tag = ctx.nc, ctx.P, ctx.tag
    NBLK = seq // 512
    NT = seq // 128
    x_d = ctx.x_d
    gain_d = nc.dram_tensor(tag + "gain_l", [128, 8], F32, kind="ExternalInput").ap()
    ident_d = nc.dram_tensor(tag + "ident", [128, 128], F32, kind="ExternalInput").ap()
    win_d = nc.dram_tensor(tag + "win_l", [128, 8 * 448], F32, kind="ExternalInput").ap()
    wuq_d = nc.dram_tensor(tag + "wuq_l", [128, 2 * 384], F32, kind="ExternalInput").ap()
    wukv_d = nc.dram_tensor(tag + "wukv_l", [128, 512], F32, kind="ExternalInput").ap()
    g1_d = nc.dram_tensor(tag + "g1_l", [128, 448], F32, kind="ExternalInput").ap()
    g2_d = nc.dram_tensor(tag + "g2_l", [128, 320], F32, kind="ExternalInput").ap()
    cos_d = nc.dram_tensor(tag + "cos", [128, NT * 32], F32, kind="ExternalInput").ap()
    sin_d = nc.dram_tensor(tag + "sin", [128, NT * 32], F32, kind="ExternalInput").ap()
    mask_d = nc.dram_tensor(tag + "maskd_d", [128, 4 * 512], F32, kind="ExternalInput").ap()
    out = ctx.out

    identf = P.sb([128, 128], F32, "identf")
    identb = P.sb([128, 128], BF16, "identb")
    gain = P.sb([128, 8], F32, "gain")
    g1 = P.sb([128, 448], F32, "g1")
    g2 = P.sb([128, 320], F32, "g2")
    cst = [P.sb([128, 32], F32, f"cst{i}") for i in range(2)]
    maskd = P.sb([128, 4, 512], BF16, "maskd")
    stage = [P.sb([128, 2048], F32, f"stage{i}") for i in range(1)] * 2
    P.dma(identf[:], ident_d, writes=[identf])
    P.cp("dve", identb[:], identf[:], [identf], [identb])
    P.dma(gain[:], gain_d, writes=[gain])
    P.dma(g1[:], g1_d, writes=[g1])
    P.dma(g2[:], g2_d, writes=[g2])
    P.dma(stage[0][:], mask_d, writes=[stage[0]])
    P.cp("dve", maskd[:].rearrange("p a b -> p (a b)"), stage[0][:], [stage[0]], [maskd])
    win = P.sb([128, 8, 448], BF16, "win")
    wuq = P.sb([128, 2, 384], BF16, "wuq")
    wukv = P.sb([128, 512], BF16, "wukv")
    load_w_bf16(P, win, win_d, 448, stage)
    P.dma(stage[1][:, 0:768], wuq_d, writes=[stage[1]])
    P.cp("pool", wuq[:].rearrange("p c n -> p (c n)"), stage[1][:, 0:768], [stage[1]], [wuq])
    P.dma(stage[0][:, 0:512], wukv_d, writes=[stage[0]])
    P.cp("pool", wukv[:], stage[0][:, 0:512], [stage[0]], [wukv])

    QnT = P.sb([128, 2, seq], BF16, "QnT")
    KnT = P.sb([128, 2, seq], BF16, "KnT")
    QrT = P.sb([128, seq], BF16, "QrT")
    KrT = P.sb([128, seq], BF16, "KrT")
    Vaug = P.sb([128, NT, 2, 129], BF16, "Vaug")
    P.op("dve", lambda e: e.memset(Vaug[:, :, :, 128:129], 1.0), [], [Vaug])

    ST = [P.ps([128, 512], F32, f"ST{i}") for i in range(2)]
    OA = [P.ps([128, 2, 256], F32, f"OA{i}") for i in range(2)]
    OB = [P.ps([128, 2, 256], F32, f"OB{i}") for i in range(2)]
    ptr = P.ps([128, 8, 128], BF16, "ptr")
    PT = [P.sb([128, 512], BF16, f"PT{i}") for i in range(2)]

    fe = FrontEnd(P, x_d, gain, identb, ptr, rowmap=getattr(ctx, 'rowmap', None))
    hTb = [P.sb([128, 8, 512], BF16, f"hT{i}") for i in range(1)] * 2
    c32_2 = [P.sb([128, 448], F32, f"c32{i}") for i in range(2)]
    cn_2 = [P.sb([128, 512], BF16, f"cn{i}") for i in range(2)]
    cT_2 = [P.sb([128, 3, 128], BF16, f"cT{i}") for i in range(2)]
    q32_2 = [P.sb([128, 384], F32, f"q32{i}") for i in range(2)]
    kv32_2 = [P.sb([128, 512], F32, f"kv32{i}") for i in range(2)]
    qkb_2 = [P.sb([128, 5, 128], BF16, f"qkb{i}") for i in range(2)]
    scr_2 = [dict(sq=P.sb([128, 256], F32, f"sq{i}"), ssq=P.sb([128, 8], F32, f"ssq{i}"), qn=P.sb([128, 256], F32, f"qn{i}"),
                  ta=P.sb([128, 128], F32, f"ta{i}"), tb=P.sb([128, 128], F32, f"tb{i}")) for i in range(2)]

    for blk in range(NBLK):
        H = hTb[blk % 2]
        fe.block(blk, H)
        for tt_ in range(4):
            tok = slice(tt_ * 128, (tt_ + 1) * 128)
            ti = blk * 4 + tt_
            gt = slice(ti * 128, (ti + 1) * 128)
            c32, cn, cT, q32, kv32, qkb, scr = (c32_2[ti % 2], cn_2[ti % 2], cT_2[ti % 2], q32_2[ti % 2],
                                                kv32_2[ti % 2], qkb_2[ti % 2], scr_2[ti % 2])
            cs_, sn_ = cst[0], cst[1]
            P.dma(cs_[:], cos_d[:, ti * 32:(ti + 1) * 32], writes=[cs_])
            P.dma(sn_[:], sin_d[:, ti * 32:(ti + 1) * 32], writes=[sn_])
            pc = OA[0]
            pcf = pc[:].rearrange("p a b -> p (a b)")
            for c in range(8):
                P.mm(pcf[:, 0:448], H[:, c, tok], win[:, c, :], c == 0, c == 7, [H, win], [pc])
            P.cp("act", c32[:], pcf[:, 0:448], [pc], [c32])
            rmsrope3(P, c32[:, 0:256].rearrange("p (h d) -> p h d", h=1), [c32], 1, 256,
                     g1[:, 0:256].rearrange("p (h d) -> p h d", h=1), [g1], None, None, [],
                     [(cn[:, 0:256].rearrange("p (h d) -> p h d", h=1), cn)], scr)
            rmsrope3(P, c32[:, 256:384].rearrange("p (h d) -> p h d", h=1), [c32], 1, 128,
                     g1[:, 256:384].rearrange("p (h d) -> p h d", h=1), [g1], None, None, [],
                     [(cn[:, 256:384].rearrange("p (h d) -> p h d", h=1), cn)], scr)
            rmsrope3(P, c32[:, 384:448].rearrange("p (h d) -> p h d", h=1), [c32], 1, 64,
                     g1[:, 384:448].rearrange("p (h d) -> p h d", h=1), [g1], cs_[:], sn_[:],
                     [cs_, sn_],
                     [(cn[:, 384:448].rearrange("p (h d) -> p h d", h=1), cn),
                      (cn[:, 448:512].rearrange("p (h d) -> p h d", h=1), cn)], scr)
            for c in range(4):
                P.tr(ptr[:, c, :], cn[:, c * 128:(c + 1) * 128], identb[:], [cn, identb], [ptr])
            P.cp("act", cT[:], ptr[:, 0:3, :], [ptr], [cT])
            P.cp("act", KrT[:, gt], ptr[:, 3, :], [ptr], [KrT])
            pq = OA[1]
            pkv = OB[0]
            pqf = pq[:].rearrange("p a b -> p (a b)")
            pkvf = pkv[:].rearrange("p a b -> p (a b)")
            for c in range(2):
                P.mm(pqf[:, 0:384], cT[:, c, :], wuq[:, c, :], c == 0, c == 1, [cT, wuq], [pq])
            P.mm(pkvf, cT[:, 2, :], wukv[:], True, True, [cT, wukv], [pkv])
            P.cp("act", q32[:], pqf[:, 0:384], [pq], [q32])
            P.cp("act", kv32[:], pkvf, [pkv], [kv32])
            q3 = q32[:].rearrange("p (h d) -> p h d", h=2)
            kv3 = kv32[:].rearrange("p (h d) -> p h d", h=2)
            rmsrope3(P, q3[:, :, 0:128], [q32], 2, 128, g2[:, 0:128].unsqueeze(1).to_broadcast([128, 2, 128]), [g2],
                     None, None, [], [(qkb[:, 0:2, :], qkb)], scr)
            rmsrope3(P, q3[:, :, 128:192], [q32], 2, 64, g2[:, 128:192].unsqueeze(1).to_broadcast([128, 2, 64]), [g2],
                     cs_[:], sn_[:], [cs_, sn_],
                     [(qkb[:, 2, :].rearrange("p (h d) -> p h d", h=2), qkb)], scr)
            rmsrope3(P, kv3[:, :, 0:128], [kv32], 2, 128, g2[:, 192:320].unsqueeze(1).to_broadcast([128, 2, 128]), [g2],
                     None, None, [], [(qkb[:, 3:5, :], qkb)], scr)
            P.cp("dve", Vaug[:, ti, :, 0:128], kv3[:, :, 128:256], [kv32], [Vaug])
            for c in range(5):
                P.tr(ptr[:, c, :], qkb[:, c, :], identb[:], [qkb, identb], [ptr])
            P.cp("act", QnT[:, :, gt], ptr[:, 0:2, :], [ptr], [QnT])
            P.cp("act", QrT[:, gt], ptr[:, 2, :], [ptr], [QrT])
            P.cp("act", KnT[:, :, gt], ptr[:, 3:5, :], [ptr], [KnT])

    rs = P.sb([128, 4], F32, "rs")
    yo = [P.sb([128, 128], F32, f"yo{i}") for i in range(4)]
    cnt = [0]
    scale = 192 ** -0.5
    bs = BankStart()
    for head in range(2):
        steps = []
        ps = slice(head * 64, (head + 1) * 64)
        for sb in range(NBLK):
            Oset = OA if sb % 2 == 0 else OB

            def fin_fn(sb_=sb, Oset=Oset, head=head):
                def fin():
                    for j in range(4):
                        ob = Oset[j // 2]
                        P.op("dve", lambda e, ob=ob, j=j: e.reciprocal(out=rs[:, j:j + 1], in_=ob[:, j % 2, 128:129]),
                             [ob], [rs])
                        Y = yo[cnt[0] % 4]
                        cnt[0] += 1
                        P.ts("dve", Y[:], ob[:, j % 2, 0:128], rs[:, j:j + 1], ALU.mult, [ob, rs], [Y])
                        r = sb_ * 512 + j * 128
                        P.dma(out[r:r + 128, head * 128:(head + 1) * 128], Y[:], reads=[Y])
                return fin

            nk = 4 * sb + 4
            for kt in range(nk):
                t = kt - 4 * sb
                ks = slice(kt * 128, (kt + 1) * 128)
                qs = slice(sb * 512, (sb + 1) * 512)
                qk = [(KnT[:, head, ks], QnT[:, head, qs], [KnT, QnT]),
                      (KrT[ps, ks], QrT[ps, qs], [KrT, QrT])]
                if t >= 0:
                    qk.append((identb[:], maskd[:, t, :], [identb, maskd]))
                pv = []
                for j in range(4):
                    if t > j:
                        continue
                    ob = Oset[j // 2]
                    pv.append((ob, ob[:, j % 2, 0:129], slice(j * 128, (j + 1) * 128),
                               kt == 0 and j % 2 == 0, kt == 4 * sb + j))
                steps.append(dict(qk=qk, pv=pv, v=(Vaug[:, kt, head, :], [Vaug]),
                                  fin=(fin_fn() if kt == nk - 1 else None)))
        run_attention(P, steps, ST, PT, scale)
    return


def mla_inputs(hp, x_b, norm_gain, w_in, cq_gain, ckv_gain, w_uq, w_ukv, qn_gain, qr_gain, kn_gain, kr_gain, seq):
    o = 512 + 6 * 128 + 24
    cols = w_in[:, o:o + 448]
    cos, sin = rope_tables(seq, 64)
    d = {"ident": np.eye(128, dtype=np.float32)}
    d["x"] = np.ascontiguousarray(x_b)
    d["gain_l"] = np.ascontiguousarray(norm_gain.reshape(8, 128).T)
    d["win_l"] = wl(cols)
    uq = w_uq[:, hp * 384:(hp + 1) * 384]
    d["wuq_l"] = np.ascontiguousarray(uq.reshape(2, 128, 384).transpose(1, 0, 2).reshape(128, 768))
    d["wukv_l"] = np.ascontiguousarray(w_ukv[:, hp * 512:(hp + 1) * 512])
    d["g1_l"] = rep(np.concatenate([cq_gain, ckv_gain, kr_gain]))
    d["g2_l"] = rep(np.concatenate([qn_gain, qr_gain, kn_gain]))
    d["cos"] = tok_l(cos)
    d["sin"] = tok_l(sin)
    d["maskd_d"] = diag_masks()
    return d


def nsa_consts(seq):
    NT = seq // 128
    n_cmp = seq // 16 - 1
    NCT = (n_cmp + 127) // 128
    n_sel = seq // 64
    k = np.arange(128)[:, None]
    q = np.arange(512)[None, :]
    mw = np.stack([np.where((q < 128 * t + k) & (128 * t + k <= q + 512), 0.0, NEG) for t in range(8)], 1)
    mc = np.stack([np.where(16 * k + 31 <= 512 * m + q, 0.0, NEG) for m in range(5)], 1)
    n = np.arange(NCT * 128)[:, None]
    j = np.arange(128)[None, :]
    ov = ((16 * n < 64 * j + 64) & (16 * n + 32 > 64 * j) & (n < n_cmp) & (j < n_sel)).astype(np.float32)
    ovl = ov.reshape(NCT, 128, 128).transpose(1, 0, 2).reshape(128, NCT * 128)
    kk = np.arange(seq)[None, :]
    esel = (kk // 64 == np.arange(128)[:, None]).astype(np.float32)
    qq = np.arange(seq)[:, None]
    cur = qq // 64
    jj = np.arange(128)[None, :]
    forced = (jj == 0) | (jj == cur) | (jj == cur - 1)
    future = (jj * 64 > qq) | (jj >= n_sel)
    A = np.where(future | forced, 0.0, 1.0)
    B = np.where(future, -1e6, np.where(forced, 1e6, 0.0))
    ab = np.concatenate([A, B], 1).astype(np.float32)
    cosf, sinf = rope_tables(seq, 64)
    ce = np.minimum(np.arange(NCT * 128) * 16 + 31, seq - 1)
    d = dict(maskd_d=diag_masks(),
             maskw_d=np.ascontiguousarray(mw.reshape(128, 8 * 512).astype(np.float32)),
             maskc_d=np.ascontiguousarray(mc.reshape(128, 5 * 512).astype(np.float32)),
             ovl_d=np.ascontiguousarray(ovl), esel_d=esel, ab_d=tok_l(ab),
             cos=tok_l(cosf), sin=tok_l(sinf), cosc=tok_l(cosf[ce]), sinc=tok_l(sinf[ce]),
             ident=np.eye(128, dtype=np.float32))
    return d


def emit_nsa(ctx, seq=SEQ):
    nc, P, tag = ctx.nc, ctx.P, ctx.tag
    NBLK = seq // 512
    NT = seq // 128
    n_cmp = seq // 16 - 1
    NCT = (n_cmp + 127) // 128
    NCP = NCT * 128
    x_d = ctx.x_d
    gain_d = nc.dram_tensor(tag + "gain_l", [128, 8], F32, kind="ExternalInput").ap()
    ident_d = nc.dram_tensor(tag + "ident", [128, 128], F32, kind="ExternalInput").ap()
    win_d = nc.dram_tensor(tag + "win_l", [128, 8 * 652], F32, kind="ExternalInput").ap()
    gq_d = nc.dram_tensor(tag + "gq_l", [128, 448], F32, kind="ExternalInput").ap()
    w1_d = nc.dram_tensor(tag + "w1_l", [128, 2048], F32, kind="ExternalInput").ap()
    w1c_d = nc.dram_tensor(tag + "w1c_l", [128, 2048], F32, kind="ExternalInput").ap()
    pos_d = nc.dram_tensor(tag + "pos_l", [128, 32], F32, kind="ExternalInput").ap()
    w2_d = nc.dram_tensor(tag + "w2_l", [64, 128], F32, kind="ExternalInput").ap()
    cos_d = nc.dram_tensor(tag + "cos", [128, NT * 32], F32, kind="ExternalInput").ap()
    sin_d = nc.dram_tensor(tag + "sin", [128, NT * 32], F32, kind="ExternalInput").ap()
    cosc_d = nc.dram_tensor(tag + "cosc", [128, NCT * 32], F32, kind="ExternalInput").ap()
    sinc_d = nc.dram_tensor(tag + "sinc", [128, NCT * 32], F32, kind="ExternalInput").ap()
    maskd_d = nc.dram_tensor(tag + "maskd_d", [128, 4 * 512], F32, kind="ExternalInput").ap()
    maskw_d = nc.dram_tensor(tag + "maskw_d", [128, 8 * 512], F32, kind="ExternalInput").ap()
    maskc_d = nc.dram_tensor(tag + "maskc_d", [128, 5 * 512], F32, kind="ExternalInput").ap()
    ovl_d = nc.dram_tensor(tag + "ovl_d", [128, NCT * 128], F32, kind="ExternalInput").ap()
    esel_d = nc.dram_tensor(tag + "esel_d", [128, seq], F32, kind="ExternalInput").ap()
    ab_d = nc.dram_tensor(tag + "ab_d", [128, NT * 256], F32, kind="ExternalInput").ap()
    out = ctx.out

    identf = P.sb([128, 128], F32, "identf")
    identb = P.sb([128, 128], BF16, "identb")
    gain = P.sb([128, 8], F32, "gain")
    gq = P.sb([128, 448], F32, "gq")
    stage = [P.sb([128, 512], F32, f"stage{i}") for i in range(2)]
    P.dma(identf[:], ident_d, writes=[identf])
    P.cp("dve", identb[:], identf[:], [identf], [identb])
    P.dma(gain[:], gain_d, writes=[gain])
    P.dma(gq[:], gq_d, writes=[gq])

    def load_const_bf16(dst_flat_ap, dst_buf, src_d, n):
        q = 0
        i = 0
        while q < n:
            w = min(512, n - q)
            st = stage[i % 2]
            P.dma(st[:, 0:w], src_d[:, q:q + w], writes=[st])
            P.cp("pool", dst_flat_ap[:, q:q + w], st[:, 0:w], [st], [dst_buf])
            q += w
            i += 1

    maskd = P.sb([128, 4, 512], BF16, "maskd")
    maskw = P.sb([128, 8, 512], BF16, "maskw")
    maskc = P.sb([128, 5, 512], BF16, "maskc")
    esel = P.sb([128, seq], BF16, "esel")
    load_const_bf16(maskd[:].rearrange("p a b -> p (a b)"), maskd, maskd_d, 2048)
    load_const_bf16(maskw[:].rearrange("p a b -> p (a b)"), maskw, maskw_d, 4096)
    load_const_bf16(maskc[:].rearrange("p a b -> p (a b)"), maskc, maskc_d, 2560)
    load_const_bf16(esel[:], esel, esel_d, seq)
    win = P.sb([128, 8, 652], BF16, "win")
    load_w_bf16(P, win, win_d, 652, stage)
    w1 = P.sb([128, 32, 64], BF16, "w1")
    load_const_bf16(w1[:].rearrange("p a b -> p (a b)"), w1, w1_d, 2048)
    w1c = P.sb([128, 2, 16, 64], BF16, "w1c")
    load_const_bf16(w1c[:].rearrange("p a b c -> p (a b c)"), w1c, w1c_d, 2048)
    posb = P.sb([128, 2, 16], BF16, "posb")
    w2 = P.sb([64, 2, 64], BF16, "w2")
    cosc = P.sb([128, NCT, 32], F32, "cosc_t")
    sinc = P.sb([128, NCT, 32], F32, "sinc_t")
    P.dma(stage[0][:, 0:32], pos_d, writes=[stage[0]])
    P.cp("pool", posb[:].rearrange("p a b -> p (a b)"), stage[0][:, 0:32], [stage[0]], [posb])
    P.dma(stage[1][0:64, 0:128], w2_d, writes=[stage[1]])
    P.cp("pool", w2[:].rearrange("p a b -> p (a b)"), stage[1][0:64, 0:128], [stage[1]], [w2])
    P.dma(cosc[:].rearrange("p t d -> p (t d)"), cosc_d, writes=[cosc])
    P.dma(sinc[:].rearrange("p t d -> p (t d)"), sinc_d, writes=[sinc])

    import os
    if os.environ.get("NSA_PAD"):
        pad_ = P.sb([128, int(os.environ["NSA_PAD"])], F32, "pad_")
    QT2 = P.sb([128, 2, seq], BF16, "QT2")
    KsT2 = P.sb([128, seq], BF16, "KsT2")
    KwT2 = P.sb([128, seq], BF16, "KwT2")
    kcvT = P.sb([128, seq], BF16, "kcvT")
    Vs = P.sb([128, NT, 65], BF16, "Vs")
    Vw = P.sb([128, NT, 65], BF16, "Vw")
    gts = P.sb([128, NT, 12], F32, "gts")
    CkT2 = P.sb([128, NCP], BF16, "CkT2")
    Cv = P.sb([128, NCT, 193], BF16, "Cv")
    P.op("dve", lambda e: e.memset(Vs[:, :, 64:65], 1.0), [], [Vs])
    P.op("dve", lambda e: e.memset(Vw[:, :, 64:65], 1.0), [], [Vw])
    P.op("dve", lambda e: e.memset(Cv[:, :, 64:65], 1.0), [], [Cv])
    st = stage[0]
    P.dma(st[:, 0:NCT * 128], ovl_d, writes=[st])
    P.cp("dve", Cv[:, :, 65:193], st[:, 0:NCT * 128].rearrange("p (a b) -> p a b", a=NCT), [st], [Cv])

    ST = [P.ps([128, 512], F32, f"ST{i}") for i in range(2)]
    Oc = [P.ps([128, 2, 256], F32, f"Oc{i}") for i in range(2)]
    OX = P.ps([128, 4, 128], F32, "OX")
    OY = P.ps([128, 4, 128], F32, "OY")
    ptr = P.ps([128, 8, 128], BF16, "ptr")
    PT = [P.sb([128, 512], BF16, f"PT{i}") for i in range(2)]

    fe = FrontEnd(P, x_d, gain, identb, ptr, nx=1, rowmap=getattr(ctx, 'rowmap', None))
    hTb = [P.sb([128, 8, 512], BF16, f"hT{i}") for i in range(1)]
    p32 = P.sb([128, 652], F32, "p32")
    qb = P.sb([128, 384], BF16, "qb")
    kb = P.sb([128, 384], BF16, "kb")
    cst = [P.sb([128, 64], F32, f"cst{i}") for i in range(2)]
    scr = dict(sq=P.sb([128, 384], F32, "sq"), ssq=P.sb([128, 8], F32, "ssq"), qn=P.sb([128, 384], F32, "qn"),
               ta=P.sb([128, 192], F32, "ta"), tb=P.sb([128, 192], F32, "tb"))
    g1 = P.sb([128, 12], F32, "g1")

    for blk in range(NBLK):
        H = hTb[0]
        fe.block(blk, H)
        for tt_ in range(4):
            tok = slice(tt_ * 128, (tt_ + 1) * 128)
            ti = blk * 4 + tt_
            gt = slice(ti * 128, (ti + 1) * 128)
            pA = Oc[0]
            pB = Oc[1]
            pAf = pA[:].rearrange("p a b -> p (a b)")
            pBf = pB[:].rearrange("p a b -> p (a b)")
            for c in range(8):
                P.mm(pAf, H[:, c, tok], win[:, c, 0:512], c == 0, c == 7, [H, win], [pA])
            for c in range(8):
                P.mm(pBf[:, 0:140], H[:, c, tok], win[:, c, 512:652], c == 0, c == 7, [H, win], [pB])
            P.cp("act", p32[:, 0:512], pAf, [pA], [p32])
            P.cp("act", p32[:, 512:652], pBf[:, 0:140], [pB], [p32])
            cs_, sn_ = cst[0], cst[1]
            P.dma(cs_[:, 0:32], cos_d[:, ti * 32:(ti + 1) * 32], writes=[cs_])
            P.dma(sn_[:, 0:32], sin_d[:, ti * 32:(ti + 1) * 32], writes=[sn_])
            rmsrope3(P, p32[:, 0:384].rearrange("p (h d) -> p h d", h=6), [p32], 6, 64,
                     gq[:, 0:384].rearrange("p (h d) -> p h d", h=6), [gq], cs_[:, 0:32], sn_[:, 0:32], [cs_, sn_],
                     [(qb[:].rearrange("p (h d) -> p h d", h=6), qb)], scr)
            kb4 = kb[:, 0:256].rearrange("p (a b d) -> p a b d", a=2, b=2)
            ksw = qb[:, 256:384].rearrange("p (a d) -> p a d", a=2)
            P.cp("dve", kb4[:, :, 0, :], ksw, [qb], [kb])
            P.cp("pool", kb4[:, :, 1, :], ksw, [qb], [kb])
            P.cp("dve", kb[:, 256:384], p32[:, 384:512], [p32], [kb])
            P.cp("dve", Vs[:, ti, 0:64], p32[:, 512:576], [p32], [Vs])
            P.cp("dve", Vw[:, ti, 0:64], p32[:, 576:640], [p32], [Vw])
            P.actv(g1[:], p32[:, 640:652], AF.Exp, [p32], [g1], scale=-1.0)
            P.ts("dve", g1[:], g1[:], 1.0, ALU.add, [g1], [g1])
            P.op("dve", lambda e, ti=ti: e.reciprocal(out=gts[:, ti, :], in_=g1[:]), [g1], [gts])
            P.tr(ptr[:, 0, :], qb[:, 0:128], identb[:], [qb, identb], [ptr])
            P.tr(ptr[:, 1, :], qb[:, 128:256], identb[:], [qb, identb], [ptr])
            for c in range(3):
                P.tr(ptr[:, 2 + c, :], kb[:, c * 128:(c + 1) * 128], identb[:], [kb, identb], [ptr])
            P.cp("act", QT2[:, :, gt], ptr[:, 0:2, :], [ptr], [QT2])
            P.cp("act", KsT2[:, gt], ptr[:, 2, :], [ptr], [KsT2])
            P.cp("act", KwT2[:, gt], ptr[:, 3, :], [ptr], [KwT2])
            P.cp("act", kcvT[:, gt], ptr[:, 4, :], [ptr], [kcvT])

    hmid = P.sb([64, 512], BF16, "hmid")
    cb16 = P.sb([64, NCP], BF16, "cb16")
    cbias = P.sb([64, 2], F32, "cbias")
    ctok = P.sb([128, 64], F32, "ctok")
    ckn = P.sb([128, 128], BF16, "ckn")
    P.op("dve", lambda e: e.memset(cb16[:], 0.0), [], [cb16])
    for kv in range(2):
        ps = slice(kv * 64, (kv + 1) * 64)
        pc = Oc[kv]
        pcf = pc[:].rearrange("p a b -> p (a b)")
        for c in range(16):
            P.mm(pcf[0:64, 511:512], w1c[:, kv, c, :], posb[:, kv, c:c + 1], c == 0, c == 15, [w1c, posb], [pc])
        P.cp("dve", cbias[:, kv:kv + 1], pcf[0:64, 511:512], [pc], [cbias])
        for pos in range(32):
            rhs = kcvT[ps, pos: pos + 16 * (n_cmp - 1) + 1: 16]
            P.mm(pcf[0:64, 0:n_cmp], w1[ps, pos, :], rhs, pos == 0, pos == 31, [w1, kcvT], [pc])
        P.actv(hmid[:, 0:n_cmp], pcf[0:64, 0:n_cmp], AF.Silu, [pc, cbias], [hmid], bias=cbias[:, kv:kv + 1], scale=1.0)
        P.mm(pcf[0:64, 0:n_cmp], w2[:, kv, :], hmid[:, 0:n_cmp], True, True, [w2, hmid], [pc])
        P.cp("act", cb16[:, 0:n_cmp], pcf[0:64, 0:n_cmp], [pc], [cb16])
        for nt in range(NCT):
            P.tr(ptr[:, nt, 0:64], cb16[:, nt * 128:(nt + 1) * 128], identb[0:64, 0:64], [cb16, identb], [ptr])
        if kv == 0:
            for nt in range(NCT):
                P.cp("act", ctok[:], ptr[:, nt, 0:64], [ptr], [ctok])
                one = lambda ap: ap.rearrange("p (h d) -> p h d", h=1)
                rmsrope3(P, one(ctok[:]), [ctok], 1, 64, one(gq[:, 384:448]), [gq], cosc[:, nt, :], sinc[:, nt, :],
                         [cosc, sinc], [(one(ckn[:, 0:64]), ckn), (one(ckn[:, 64:128]), ckn)], scr)
                P.tr(ptr[:, 4 + nt % 4, :], ckn[:], identb[:], [ckn, identb], [ptr])
                P.cp("act", CkT2[:, nt * 128:(nt + 1) * 128], ptr[:, 4 + nt % 4, :], [ptr], [CkT2])
        else:
            P.cp("dve", Cv[:, :, 0:64], ptr[:, 0:NCT, 0:64], [ptr], [Cv])

    bs = BankStart()
    scale = 64 ** -0.5
    oc = [P.sb([128, 4, 4, 64], F32, f"oc{i}") for i in range(1)]
    imp = [P.sb([128, 4, 128], F32, f"imp{i}") for i in range(1)] * 2
    negsel = [P.sb([128, 512], BF16, f"negsel{i}") for i in range(2)]
    ABt = [P.sb([128, 256], F32, f"ABt{i}") for i in range(2)]
    impf = P.sb([128, 128], F32, "impf")
    imp2 = P.sb([128, 128], F32, "imp2")
    m8a = P.sb([128, 8], F32, "m8a")
    m8b = P.sb([128, 8], F32, "m8b")
    selb = P.sb([128, 128], BF16, "selb")
    rs = P.sb([128, 4], F32, "rs")
    acc = [P.sb([128, 4, 64], F32, f"acc{i}") for i in range(2)]
    yout = [P.sb([128, 4, 256], F32, f"yout{i}") for i in range(1)]
    steps = []
    for sb in range(NBLK):
        qs = slice(sb * 512, (sb + 1) * 512)
        OC, IMP, NS, YO = oc[0], imp[sb % 2], negsel[sb % 2], yout[0]
        nts = [nt for nt in range(NCT) if sb - 4 * nt >= 0]
        for h in range(4):
            pair, hh = h // 2, h % 2
            ps = slice(hh * 64, (hh + 1) * 64)

            def fin_c(h=h, OC=OC, IMP=IMP, sb=sb, NS=NS):
                for j in range(4):
                    ob = Oc[j // 2]
                    P.ts("dve", rs[:, j:j + 1], ob[:, j % 2, 64:65], 1e-30, ALU.max, [ob], [rs])
                    P.op("dve", lambda e, j=j: e.reciprocal(out=rs[:, j:j + 1], in_=rs[:, j:j + 1]), [rs], [rs])
                    P.ts("dve", OC[:, j, h, :], ob[:, j % 2, 0:64], rs[:, j:j + 1], ALU.mult, [ob, rs], [OC])
                    if h == 0:
                        P.ts("dve", IMP[:, j, :], ob[:, j % 2, 65:193], rs[:, j:j + 1], ALU.mult, [ob, rs], [IMP])
                    else:
                        P.stt("dve", IMP[:, j, :], ob[:, j % 2, 65:193], rs[:, j:j + 1], IMP[:, j, :], ALU.mult, ALU.add,
                              [ob, rs, IMP], [IMP])
                if h == 3:
                    for j in range(4):
                        ti = sb * 4 + j
                        AB = ABt[j % 2]
                        P.dma(AB[:], ab_d[:, ti * 256:(ti + 1) * 256], writes=[AB])
                        P.tt("dve", impf[:], IMP[:, j, :], AB[:, 0:128], ALU.mult, [IMP, AB], [impf])
                        P.tt("dve", impf[:], impf[:], AB[:, 128:256], ALU.add, [impf, AB], [impf])
                        P.op("dve", lambda e: e.max(out=m8a[:], in_=impf[:]), [impf], [m8a])
                        P.op("dve", lambda e: e.match_replace(out=imp2[:], in_to_replace=m8a[:], in_values=impf[:],
                                                              imm_value=-3.0e38), [m8a, impf], [imp2])
                        P.op("dve", lambda e: e.max(out=m8b[:], in_=imp2[:]), [imp2], [m8b])
                        P.ts("dve", selb[:], impf[:], m8b[:, 7:8], ALU.is_ge, [impf, m8b], [selb])
                        P.tr(ptr[:, j, :], selb[:], identb[:], [selb, identb], [ptr])
                        P.ts("dve", NS[:, j * 128:(j + 1) * 128], ptr[:, j, :], -1.0, ALU.add, [ptr], [NS],
                             s2=-NEG, op1=ALU.mult)

            for a, nt in enumerate(nts):
                m = sb - 4 * nt
                qk = [(CkT2[ps, nt * 128:(nt + 1) * 128], QT2[ps, pair, qs], [CkT2, QT2])]
                if m <= 4:
                    qk.append((identb[:], maskc[:, m, :], [identb, maskc]))
                pv = []
                for j in range(4):
                    ob = Oc[j // 2]
                    pv.append((ob, ob[:, j % 2, 0:193], slice(j * 128, (j + 1) * 128),
                               a == 0 and j % 2 == 0, a == len(nts) - 1))
                steps.append(dict(qk=qk, pv=pv, v=(Cv[:, nt, :], [Cv]),
                                  fin=(fin_c if a == len(nts) - 1 else None),
                                  sync=(h == 3 and a == len(nts) - 1)))
        for h in range(4):
            pair, hh = h // 2, h % 2
            ps = slice(hh * 64, (hh + 1) * 64)
            ACC = acc[h % 2]

            def fin_s(h=h, ACC=ACC, OC=OC, sb=sb):
                for j in range(4):
                    ti = sb * 4 + j
                    P.op("dve", lambda e, j=j: e.reciprocal(out=rs[:, j:j + 1], in_=OX[:, j, 64:65]), [OX], [rs])
                    P.tt("dve", rs[:, j:j + 1], rs[:, j:j + 1], gts[:, ti, h * 3 + 1:h * 3 + 2], ALU.mult, [rs, gts], [rs])
                    P.ts("dve", ACC[:, j, :], OX[:, j, 0:64], rs[:, j:j + 1], ALU.mult, [OX, rs], [ACC])
                    import os
                    _m = os.environ.get("NSA_DBG", "")
                    if _m in ("w", "c"):
                        P.ts("dve", ACC[:, j, :], ACC[:, j, :], 0.0, ALU.mult, [ACC], [ACC])
                    if _m in ("", "c"):
                        P.stt("dve", ACC[:, j, :], OC[:, j, h, :], gts[:, ti, h * 3:h * 3 + 1], ACC[:, j, :], ALU.mult, ALU.add,
                              [OC, gts, ACC], [ACC])

            def fin_w(h=h, ACC=ACC, YO=YO, sb=sb):
                for j in range(4):
                    ti = sb * 4 + j
                    P.op("dve", lambda e, j=j: e.reciprocal(out=rs[:, j:j + 1], in_=OY[:, j, 64:65]), [OY], [rs])
                    P.tt("dve", rs[:, j:j + 1], rs[:, j:j + 1], gts[:, ti, h * 3 + 2:h * 3 + 3], ALU.mult, [rs, gts], [rs])
                    import os
                    if os.environ.get("NSA_DBG", "") in ("s", "c"):
                        P.ts("dve", rs[:, j:j + 1], rs[:, j:j + 1], 0.0, ALU.mult, [rs], [rs])
                    P.stt("dve", YO[:, j, h * 64:(h + 1) * 64], OY[:, j, 0:64], rs[:, j:j + 1], ACC[:, j, :],
                          ALU.mult, ALU.add, [OY, rs, ACC], [YO])
                if h == 3:
                    for j in range(4):
                        r = sb * 512 + j * 128
                        P.dma(out[r:r + 128, :], YO[:, j, :], reads=[YO])

            nk = 4 * sb + 4
            for kt in range(nk):
                t = kt - 4 * sb
                ks = slice(kt * 128, (kt + 1) * 128)
                qk = [(KsT2[ps, ks], QT2[ps, pair, qs], [KsT2, QT2]),
                      (esel[:, ks], NS[:], [esel, NS])]
                if t >= 0:
                    qk.append((identb[:], maskd[:, t, :], [identb, maskd]))
                pv = []
                for j in range(4):
                    if t > j:
                        continue
                    pv.append((OX, OX[:, j, 0:65], slice(j * 128, (j + 1) * 128), kt == 0 and j == 0, kt == 4 * sb + j))
                steps.append(dict(qk=qk, pv=pv, v=(Vs[:, kt, :], [Vs]), fin=(fin_s if kt == nk - 1 else None)))
            kts = [kt for kt in range(4 * sb - 4, 4 * sb + 4) if kt >= 0]
            first_done = False
            for kt in kts:
                t8 = kt - (4 * sb - 4)
                ks = slice(kt * 128, (kt + 1) * 128)
                qk = [(KwT2[ps, ks], QT2[ps, pair, qs], [KwT2, QT2]),
                      (identb[:], maskw[:, t8, :], [identb, maskw])]
                pv = []
                for j in range(4):
                    if not (j <= t8 <= j + 4):
                        continue
                    pv.append((OY, OY[:, j, 0:65], slice(j * 128, (j + 1) * 128), not first_done, t8 == j + 4))
                    first_done = True
                steps.append(dict(qk=qk, pv=pv, v=(Vw[:, kt, :], [Vw]), fin=(fin_w if kt == kts[-1] else None)))
    run_attention(P, steps, ST, PT, scale)
    if os.environ.get("NSA_DUMP"):
        dbg = nc.dram_tensor(tag + "dbg", [128, 4 * 1024], F32, kind="ExternalOutput").ap()
        for i, (src, srcb) in enumerate(((QT2[:, 0, 0:1024], QT2), (KsT2[:, 0:1024], KsT2), (KwT2[:, 0:1024], KwT2),
                                         (CkT2[:, 0:512], CkT2))):
            w = 1024 if i < 3 else 512
            for hf in range(w // 256):
                P.cp("dve", impf[:].rearrange("p a -> p a")[:, 0:128], impf[:, 0:128], [impf], [impf]) if False else None
                t_ = yout[0]
                P.cp("dve", t_[:, 0, 0:256], src[:, hf * 256:(hf + 1) * 256], [srcb], [t_])
                P.dma(dbg[:, i * 1024 + hf * 256: i * 1024 + (hf + 1) * 256], t_[:, 0, 0:256], reads=[t_])
    return


def nsa_inputs(g, x_b, norm_gain, w_in, q_gain, k_gain, cmp_pos, cmp_w1, cmp_w2, seq):
    q = w_in[:, g * 256:(g + 1) * 256]
    kvs = [w_in[:, 512 + i * 128 + g * 64: 512 + i * 128 + (g + 1) * 64] for i in range(6)]
    gl = w_in[:, 512 + 768 + g * 12: 512 + 768 + (g + 1) * 12]
    d = nsa_consts(seq)
    d["x"] = np.ascontiguousarray(x_b)
    d["gain_l"] = np.ascontiguousarray(norm_gain.reshape(8, 128).T)
    d["win_l"] = wl(np.concatenate([q, kvs[2], kvs[4], kvs[0], kvs[1], kvs[3], kvs[5], gl], 1))
    d["gq_l"] = rep(np.concatenate([np.tile(q_gain, 4), k_gain[1], k_gain[2], k_gain[0]]))
    w1 = cmp_w1.reshape(2, 32, 64, 64).transpose(0, 2, 1, 3).reshape(128, 2048)
    d["w1_l"] = np.ascontiguousarray(w1)
    d["w1c_l"] = np.ascontiguousarray(cmp_w1.reshape(2, 16, 128, 64).transpose(2, 0, 1, 3).reshape(128, 2048))
    d["pos_l"] = np.ascontiguousarray(cmp_pos.reshape(2, 16, 128).transpose(2, 0, 1).reshape(128, 32))
    d["w2_l"] = np.ascontiguousarray(cmp_w2.transpose(1, 0, 2).reshape(64, 128))
    return d


PAIRS = [[0, 1], [2, 3], [4, 5], [6, 7]]
_CACHE = {}


class Ctx:
    pass


def build_fused(S=SEQ):
    nc = bass.Bass("TRN2", target_bir_lowering=False)
    P = Prog(nc)
    NTK = S // 2
    x_full = nc.dram_tensor("x_full", [S, D], F32, kind="ExternalInput").ap()
    x_rows = nc.dram_tensor("x_rows", [NTK, D], F32, kind="ExternalInput").ap()
    sel_d = nc.dram_tensor("sel", [128, 2], F32, kind="ExternalInput").ap()
    y = nc.dram_tensor("y", [NTK, D], F32, kind="ExternalOutput").ap()
    mixA = nc.dram_tensor("mixA_my", [S, 512], F32, kind="Internal").ap()
    GA = nc.dram_tensor("GA_all", [2 * S, 512], F32, kind="Internal").ap()
    x1_my = nc.dram_tensor("x1_my", [NTK, D], F32, kind="Internal").ap()
    x1_full = nc.dram_tensor("x1_full", [S, D], F32, kind="Internal").ap()
    mixC = nc.dram_tensor("mixC_my", [S, 512], F32, kind="Internal").ap()
    GC = nc.dram_tensor("GC_all", [2 * S, 512], F32, kind="Internal").ap()

    def phase(tag, fn, *args, **kw):
        ctx = Ctx()
        ctx.nc, ctx.P, ctx.tag = nc, P, tag + "_"
        ctx.S, ctx.sel_d = S, sel_d
        for k, v in kw.items():
            setattr(ctx, k, v)
        P.begin_phase(tag)
        fn(ctx, *args)
        P.end_phase()

    def gather(tag, src, dst):
        rows, width = src.shape
        R = (2 * 1024 * 1024) // (width * 4)
        P.begin_phase(tag)
        for k in range(rows // R):
            P.op("pool", lambda e, k=k: e.collective_compute(
                "AllGather", ALU.bypass, replica_groups=PAIRS,
                ins=[src[k * R:(k + 1) * R, :]], outs=[dst[2 * k * R:2 * (k + 1) * R, :]]), cc=True)
        P.end_phase()

    RM = (2 * 1024 * 1024) // (512 * 4)
    RX = (2 * 1024 * 1024) // (D * 4)
    gaddr = lambda p, t: (t // RM) * 2 * RM + p * RM + (t % RM)
    x1map = lambda T: ((T % NTK) // RX) * 2 * RX + (T // NTK) * RX + (T % RX)

    phase("ssd", emit_ssd, S, x_d=x_full, out=mixA[:, 256:512])
    phase("da", emit_diffattn, S, 0, x_d=x_full, out=mixA[:, 0:256])
    gather("e1", mixA, GA)
    phase("ffn", emit_ffn, 1, NTK, xin=x_rows, G=GA, out=x1_my, gaddr=gaddr)
    gather("e2", x1_my, x1_full)
    phase("nsa", emit_nsa, S, x_d=x1_full, out=mixC[:, 0:256], rowmap=x1map)
    phase("mla", emit_mla, S, x_d=x1_full, out=mixC[:, 256:512], rowmap=x1map)
    gather("e3", mixC, GC)
    phase("moe", emit_ffn, 8, NTK, xin=x1_my, G=GC, out=y, gaddr=gaddr)
    return nc


_PERM = np.concatenate([np.arange(0, 256), np.arange(512, 768), np.arange(256, 512), np.arange(768, 1024)])


def kernel(**inp):
    inp = {k: np.asarray(v) for k, v in inp.items()}
    x = np.ascontiguousarray(inp["x"], dtype=np.float32)
    B, S, _ = x.shape
    NTK = S // 2
    f = lambda k: inp[k][0]
    if "nc" not in _CACHE:
        _CACHE["nc"] = build_fused(S)
    nc = _CACHE["nc"]
    lay_ffn = ffn_layouts(f("ev_w_out")[_PERM], f("ev_norm_ffn"), inp["ffn_w_gate"], inp["ffn_w_up"], inp["ffn_w_down"])
    lay_moe = ffn_layouts(f("od_w_out")[_PERM], f("od_norm_ffn"), f("moe_w_gate"), f("moe_w_up"), f("moe_w_down"),
                          f("moe_router"))
    per_half = []
    for h in range(2):
        d = {}

        def add(tag, dd):
            for k, v in dd.items():
                if k != "x":
                    d[tag + "_" + k] = v
        add("ssd", ssd_inputs(h, x[0], f("ev_norm_mix"), f("ev_w_in"), f("ssm_conv_w"), f("ssm_conv_b"),
                              f("ssm_dt_bias"), f("ssm_a_log"), f("ssm_d"), f("ssm_norm_gain")))
        add("da", diffattn_inputs(h, x[0], f("ev_norm_mix"), f("ev_w_in"), f("da_q_gain"), f("da_k_gain"),
                                  f("da_lambda"), f("da_subln_gain"), S))
        add("nsa", nsa_inputs(h, x[0], f("od_norm_mix"), f("od_w_in"), f("nsa_q_gain"), f("nsa_k_gain"),
                              f("nsa_cmp_pos"), f("nsa_cmp_w1"), f("nsa_cmp_w2"), S))
        add("mla", mla_inputs(h, x[0], f("od_norm_mix"), f("od_w_in"), f("mla_cq_gain"), f("mla_ckv_gain"),
                              f("mla_w_uq"), f("mla_w_ukv"), f("mla_qn_gain"), f("mla_qr_gain"), f("mla_kn_gain"),
                              f("mla_kr_gain"), S))
        add("ffn", lay_ffn)
        add("moe", lay_moe)
        d["sel"] = np.ascontiguousarray(np.broadcast_to(np.array([[1.0 - h, float(h)]], np.float32), (128, 2)))
        per_half.append(d)
    ins = []
    for c in range(8):
        b, h = c // 2, c % 2
        d = dict(per_half[h])
        d["x_full"] = x[b]
        d["x_rows"] = np.ascontiguousarray(x[b, h * NTK:(h + 1) * NTK])
        ins.append(d)
    res = run_bass_kernel_spmd(nc, ins, core_ids=list(range(8)))
    out = np.empty((B, S, D), np.float32)
    for c in range(8):
        b, h = c // 2, c % 2
        out[b, h * NTK:(h + 1) * NTK] = res.results[c]["y"]
    return out
```

```python
from concourse.bass_utils import run_bass_kernel_spmd

from contextlib import ExitStack
import numpy as np
import concourse.bass as bass
import concourse.mybir as mybir

F32 = mybir.dt.float32
BF16 = mybir.dt.bfloat16
AF = mybir.ActivationFunctionType
ALU = mybir.AluOpType
AX = mybir.AxisListType

ENGS = ("pe", "dve", "act", "pool", "sp")
NRING = 8


class Buf:
    __slots__ = ("t", "lw", "rd", "name")

    def __init__(self, t, name=""):
        self.t = t
        self.lw = None
        self.rd = []
        self.name = name

    def __getitem__(self, k):
        return self.t[k]


class Op:
    __slots__ = ("eng", "fn", "deps", "sig", "idx", "dma", "ring", "ruse", "cc")

    def __init__(self, eng, fn, dma=False):
        self.eng = eng
        self.fn = fn
        self.deps = []
        self.sig = False
        self.idx = None
        self.dma = dma
        self.ring = None
        self.ruse = None
        self.cc = False


class Prog:
    def __init__(self, nc, same_engine_sync=True):
        self.nc = nc
        self.es = ExitStack()
        self.pes = None
        self.ops = {e: [] for e in ENGS}
        self.same = same_engine_sync
        self.nbuf = 0
        self.tag = "p0"
        self.persist = []
        self.sem = {e: self.es.enter_context(nc.semaphore(f"s_{e}")) for e in ENGS}
        self.ring = {"sp": [self.es.enter_context(nc.semaphore(f"r_sp{i}")) for i in range(NRING)]}
        self.ccsem = self.es.enter_context(nc.semaphore("s_cc"))
        self.cnt = {e: 0 for e in ENGS}
        self.nd = {e: 0 for e in ENGS}
        self.ncc = 0
        self.waited = {e: {} for e in ENGS}
        self.engh = {"pe": nc.tensor, "dve": nc.vector, "act": nc.scalar, "pool": nc.gpsimd, "sp": nc.sync}

    def begin_phase(self, tag):
        self.tag = tag
        self.pes = ExitStack()

    def end_phase(self):
        self._emit_ops()
        self._barrier()
        self.pes.close()
        self.pes = None
        for b in self.persist:
            b.lw = None
            b.rd = []

    def sb(self, shape, dt=F32, name=None):
        self.nbuf += 1
        name = f"{self.tag}_{name or 'sb'}_{self.nbuf}"
        t = self.pes.enter_context(self.nc.sbuf_tensor(name, list(shape), dt))
        return Buf(t, name)

    def ps(self, shape, dt=F32, name=None):
        self.nbuf += 1
        name = f"{self.tag}_{name or 'ps'}_{self.nbuf}"
        t = self.pes.enter_context(self.nc.psum_tensor(name, list(shape), dt))
        return Buf(t, name)

    def sub(self, ap, name=""):
        return Buf(ap, name)

    def dram(self, ap, name=""):
        b = Buf(ap, name)
        self.persist.append(b)
        return b

    def op(self, eng, fn, reads=(), writes=(), dma=False, cc=False):
        o = Op(eng, fn, dma)
        o.cc = cc
        deps = []
        for b in reads:
            if b.lw is not None:
                deps.append(b.lw)
        for b in writes:
            if b.lw is not None:
                deps.append(b.lw)
            deps.extend(b.rd)
        seen = set()
        for d in deps:
            if id(d) in seen or d is o:
                continue
            seen.add(id(d))
            if d.eng == eng and not d.dma and not d.cc:
                if eng == "pe" or not self.same:
                    continue
            d.sig = True
            o.deps.append(d)
        for b in reads:
            b.rd.append(o)
        for b in writes:
            b.lw = o
            b.rd = []
        if dma or cc:
            o.sig = True
        self.ops[eng].append(o)
        return o

    def mm(self, out, lhsT, rhs, start, stop, reads, writes):
        return self.op("pe", lambda e: e.matmul(out, lhsT=lhsT, rhs=rhs, start=start, stop=stop), reads, writes)

    def tr(self, out, in_, ident, reads, writes):
        return self.op("pe", lambda e: e.transpose(out=out, in_=in_, identity=ident), reads, writes)

    def actv(self, out, in_, func, reads, writes, **kw):
        return self.op("act", lambda e: e.activation(out=out, in_=in_, func=func, **kw), reads, writes)

    def cp(self, eng, out, in_, reads, writes):
        if eng == "act":
            return self.op("act", lambda e: e.copy(out=out, in_=in_), reads, writes)
        return self.op(eng, lambda e: e.tensor_copy(out=out, in_=in_), reads, writes)

    def tt(self, eng, out, in0, in1, op, reads, writes):
        return self.op(eng, lambda e: e.tensor_tensor(out=out, in0=in0, in1=in1, op=op), reads, writes)

    def ts(self, eng, out, in0, s1, op0, reads, writes, s2=None, op1=None, **kw):
        if op1 is None:
            return self.op(eng, lambda e: e.tensor_scalar(out=out, in0=in0, scalar1=s1, scalar2=None, op0=op0, **kw), reads, writes)
        return self.op(eng, lambda e: e.tensor_scalar(out=out, in0=in0, scalar1=s1, scalar2=s2, op0=op0, op1=op1, **kw), reads, writes)

    def stt(self, eng, out, in0, scalar, in1, op0, op1, reads, writes):
        return self.op(eng, lambda e: e.scalar_tensor_tensor(out=out, in0=in0, scalar=scalar, in1=in1, op0=op0, op1=op1), reads, writes)

    def dma(self, out, in_, reads=(), writes=(), eng="sp", **kw):
        return self.op(eng, lambda e: e.dma_start(out=out, in_=in_, **kw), reads, writes, dma=True)

    def _emit_ops(self):
        for e in ENGS:
            ops = self.ops[e]
            if ops and not ops[-1].dma and not ops[-1].cc:
                ops[-1].sig = True
            for o in ops:
                if o.dma:
                    o.ring = self.nd[e] % NRING
                    o.ruse = self.nd[e] // NRING
                    self.nd[e] += 1
                elif o.cc:
                    self.ncc += 1
                    o.idx = self.ncc
                elif o.sig:
                    self.cnt[e] += 1
                    o.idx = self.cnt[e]
        for e in ENGS:
            h = self.engh[e]
            waited = self.waited[e]
            for o in self.ops[e]:
                need = {}
                for d in o.deps:
                    if d.dma:
                        key = ("r", d.eng, d.ring)
                        val = 16 * (d.ruse + 1)
                    elif d.cc:
                        key = ("c",)
                        val = d.idx
                    else:
                        key = ("s", d.eng)
                        val = d.idx
                    if need.get(key, 0) < val:
                        need[key] = val
                if o.dma and o.ruse > 0:
                    key = ("r", e, o.ring)
                    val = 16 * o.ruse
                    if need.get(key, 0) < val:
                        need[key] = val
                for key, val in need.items():
                    if waited.get(key, 0) >= val:
                        continue
                    waited[key] = val
                    h.wait_ge(self._semof(key), val)
                ins = o.fn(h)
                if o.dma:
                    ins.then_inc(self.ring[e][o.ring], 16)
                elif o.cc:
                    ins.then_inc(self.ccsem)
                elif o.sig:
                    ins.then_inc(self.sem[e], 1)
            self.ops[e] = []

    def _semof(self, key):
        if key[0] == "s":
            return self.sem[key[1]]
        if key[0] == "c":
            return self.ccsem
        return self.ring[key[1]][key[2]]

    def _barrier(self):
        targets = {}
        for e in ENGS:
            if self.cnt[e] > 0:
                targets[("s", e)] = self.cnt[e]
        for q in self.ring:
            for r in range(NRING):
                n = len(range(r, self.nd[q], NRING))
                if n:
                    targets[("r", q, r)] = 16 * n
        if self.ncc:
            targets[("c",)] = self.ncc
        for e in ENGS:
            h = self.engh[e]
            waited = self.waited[e]
            for key, val in targets.items():
                if key == ("s", e) or waited.get(key, 0) >= val:
                    continue
                waited[key] = val
                h.wait_ge(self._semof(key), val)


D = 1024
DFF = 2816
NFG = 11
EPS = 1e-6


def emit_ffn(ctx, n_exp, ntok=4096):
    moe = n_exp > 1
    nc, P, tag = ctx.nc, ctx.P, ctx.tag
    NHALF = 2
    HT = ntok // NHALF
    NT = HT // 128
    NTB = HT // 512
    xin = ctx.xin
    Gd = ctx.G
    S_ = ctx.S
    sel_d = ctx.sel_d
    wout_d = nc.dram_tensor(tag + "wout_l", [128, 8 * D], F32, kind="ExternalInput").ap()
    gain_d = nc.dram_tensor(tag + "gain_l", [128, 8], F32, kind="ExternalInput").ap()
    ident_d = nc.dram_tensor(tag + "ident", [128, 128], F32, kind="ExternalInput").ap()
    wg_d = nc.dram_tensor(tag + "wg_l", [n_exp * NFG, 128, 2048], F32, kind="ExternalInput").ap()
    wu_d = nc.dram_tensor(tag + "wu_l", [n_exp * NFG, 128, 2048], F32, kind="ExternalInput").ap()
    wd_d = nc.dram_tensor(tag + "wd_l", [n_exp * NFG, 128, 2048], F32, kind="ExternalInput").ap()
    if moe:
        router_d = nc.dram_tensor(tag + "router_l", [128, 8 * 8], F32, kind="ExternalInput").ap()
    y = ctx.out

    identf = P.sb([128, 128], F32, "identf")
    identb = P.sb([128, 128], BF16, "identb")
    gain = P.sb([128, 8], F32, "gain")
    P.dma(identf[:], ident_d, writes=[identf])
    P.cp("dve", identb[:], identf[:], [identf], [identb])
    P.dma(gain[:], gain_d, writes=[gain])
    if moe:
        router = P.sb([128, 8, 8], F32, "router")
        P.dma(router[:].rearrange("p c e -> p (c e)"), router_d, writes=[router])

    sel = P.sb([128, 2], F32, "sel")
    P.dma(sel[:], sel_d, writes=[sel])
    acc_t = P.sb([128, NT, D], F32, "acc")
    acc = [P.sub(acc_t[:, t, :], f"acc{t}") for t in range(NT)]
    hT_t = P.sb([128, 8, HT], BF16, "hT")
    hT = [P.sub(hT_t[:, :, tb * 512:(tb + 1) * 512], f"hT{tb}") for tb in range(NTB)]
    gates_t = P.sb([128, NT, 8], F32, "gates")
    gates = [P.sub(gates_t[:, t, :], f"gates{t}") for t in range(NT)]
    wbuf = [P.sb([128, 6144], BF16, f"wbuf{i}") for i in range(2)]
    wgb = [P.sub(wbuf[i][:, 0:2048], f"wg{i}") for i in range(2)]
    wub = [P.sub(wbuf[i][:, 2048:4096], f"wu{i}") for i in range(2)]
    wdb = [P.sub(wbuf[i][:, 4096:6144], f"wd{i}") for i in range(2)]
    stage = [P.sb([128, 2048], F32, f"stage{i}") for i in range(3)]
    woutb = P.sb([128, 8, D], BF16, "woutb")
    actT = [[P.sb([128, 512], BF16, f"actT{i}{j}") for j in range(2)] for i in range(2)]
    sil = [P.sb([128, 512], BF16, f"sil{j}") for j in range(2)]
    X = [P.sb([128, D], F32, f"X{i}") for i in range(2)]
    M = [P.sb([128, D], F32, f"M{i}") for i in range(1)] * 2
    C0 = P.sb([128, D], F32, "C0")
    C1 = P.sb([128, D], F32, "C1")
    MB = P.sb([128, D], BF16, "MB")
    mixT = P.sb([128, 8, 128], BF16, "mixT")
    ss = P.sb([128, 1], F32, "ss")
    xnb = P.sb([128, D], BF16, "xnb")
    if moe:
        xn32 = P.sb([128, D], F32, "xn32")
        hT32 = P.sb([128, 8, 128], F32, "hT32")
        lg = P.sb([128, 8], F32, "lg")
        top8 = P.sb([128, 8], F32, "top8")
        msk = P.sb([128, 8], F32, "msk")
        wexp = P.sb([128, 8], F32, "wexp")
        den = P.sb([128, 1], F32, "den")
        negv1 = P.sb([128, 1], F32, "negv1")

    pg = [P.ps([128, 512], F32, f"pg{j}") for j in range(2)]
    pu = [P.ps([128, 512], F32, f"pu{j}") for j in range(2)]
    pd = [P.ps([128, 512], F32, f"pd{j}") for j in range(2)]
    ptr = P.ps([128, 8, 128], BF16, "ptr")
    pm = P.ps([128, 512], F32, "pm")

    for q in range(4):
        st = stage[q % 3]
        P.dma(st[:], wout_d[:, q * 2048:(q + 1) * 2048], writes=[st])
        P.cp("pool", woutb[:].rearrange("p c n -> p (c n)")[:, q * 2048:(q + 1) * 2048], st[:], [st], [woutb])

    gidx = 0
    for half in range(NHALF):
        r0 = half * HT
        for t in range(NT):
            Xt, Mt = X[t % 2], M[t % 2]
            rows = slice(r0 + t * 128, r0 + (t + 1) * 128)
            P.dma(Xt[:], xin[rows, :], writes=[Xt])
            rr = r0 + t * 128
            for p_ in range(2):
                a0 = ctx.gaddr(p_, rr)
                a1 = ctx.gaddr(p_, ntok + rr)
                P.dma(C0[:, p_ * 512:(p_ + 1) * 512], Gd[a0:a0 + 128, :], writes=[C0])
                P.dma(C1[:, p_ * 512:(p_ + 1) * 512], Gd[a1:a1 + 128, :], writes=[C1])
            P.ts("dve", Mt[:], C0[:], sel[:, 0:1], ALU.mult, [C0, sel], [Mt])
            P.stt("dve", Mt[:], C1[:], sel[:, 1:2], Mt[:], ALU.mult, ALU.add, [C1, sel, Mt], [Mt])
            P.cp("act", MB[:], Mt[:], [Mt], [MB])
            for c in range(8):
                P.tr(ptr[:, c, :], MB[:, c * 128:(c + 1) * 128], identb[:], [MB, identb], [ptr])
            P.cp("dve", mixT[:], ptr[:], [ptr], [mixT])
            A = acc[t]
            for hf in range(2):
                po = pd[hf]
                for c in range(8):
                    P.mm(po[:], mixT[:, c, :], woutb[:, c, hf * 512:(hf + 1) * 512], c == 0, c == 7,
                         [mixT, woutb], [po])
                P.tt("dve", A[:, hf * 512:(hf + 1) * 512], Xt[:, hf * 512:(hf + 1) * 512], po[:], ALU.add,
                     [Xt, po], [A])
            P.actv(Mt[:], A[:], AF.Square, [A], [Mt, ss], accum_out=ss[:])
            P.actv(ss[:], ss[:], AF.Ln, [ss], [ss], scale=1.0 / D, bias=EPS)
            P.actv(ss[:], ss[:], AF.Exp, [ss], [ss], scale=-0.5)
            P.ts("dve", xnb[:], A[:], ss[:, 0:1], ALU.mult, [A, ss], [xnb])
            for c in range(8):
                P.tr(ptr[:, c, :], xnb[:, c * 128:(c + 1) * 128], identb[:], [xnb, identb], [ptr])
            H = hT[t // 4]
            tok = slice((t % 4) * 128, (t % 4 + 1) * 128)
            P.tt("dve", H[:, :, tok], ptr[:], gain[:].unsqueeze(2).to_broadcast([128, 8, 128]), ALU.mult,
                 [ptr, gain], [H])
            if moe:
                G = gates[t]
                P.ts("dve", xn32[:], A[:], ss[:, 0:1], ALU.mult, [A, ss], [xn32])
                for rnd in range(2):
                    for c4 in range(4):
                        c = rnd * 4 + c4
                        P.tr(pm[:, c4 * 128:(c4 + 1) * 128], xn32[:, c * 128:(c + 1) * 128], identf[:],
                             [xn32, identf], [pm])
                    P.tt("dve", hT32[:, rnd * 4:(rnd + 1) * 4, :], pm[:].rearrange("p (c t) -> p c t", c=4),
                         gain[:, rnd * 4:(rnd + 1) * 4].unsqueeze(2).to_broadcast([128, 4, 128]), ALU.mult,
                         [pm, gain], [hT32])
                for c in range(8):
                    P.mm(pm[:, 0:8], hT32[:, c, :], router[:, c, :], c == 0, c == 7, [hT32, router], [pm])
                P.cp("dve", lg[:], pm[:, 0:8], [pm], [lg])
                P.op("dve", lambda e: e.max(out=top8[:], in_=lg[:]), [lg], [top8])
                P.ts("dve", msk[:], lg[:], top8[:, 1:2], ALU.is_ge, [lg, top8], [msk])
                P.ts("dve", negv1[:], top8[:, 0:1], -1.0, ALU.mult, [top8], [negv1])
                P.actv(wexp[:], lg[:], AF.Exp, [lg, negv1], [wexp], bias=negv1[:, 0:1], scale=1.0)
                P.tt("dve", wexp[:], wexp[:], msk[:], ALU.mult, [wexp, msk], [wexp])
                P.op("dve", lambda e: e.reduce_sum(out=den[:], in_=wexp[:], axis=AX.X), [wexp], [den])
                P.op("dve", lambda e: e.reciprocal(out=den[:], in_=den[:]), [den], [den])
                P.ts("dve", G[:], wexp[:], den[:, 0:1], ALU.mult, [wexp, den], [G])

        pend = None

        pd3 = [pd[0], pd[1], pm]
        dcnt = [0]

        def emit_down(job):
            k, e, tb, AT = job
            for tt_ in range(4):
                t = tb * 4 + tt_
                A = acc[t]
                for hf in range(2):
                    po = pd3[dcnt[0] % 3]
                    dcnt[0] += 1
                    for j in range(2):
                        P.mm(po[:], AT[j][:, tt_ * 128:(tt_ + 1) * 128],
                             wdb[k][:, j * 1024 + hf * 512: j * 1024 + (hf + 1) * 512], j == 0, j == 1,
                             [AT[j], wdb[k]], [po])
                    dst = A[:, hf * 512:(hf + 1) * 512]
                    if moe:
                        P.stt("dve", dst, po[:], gates[t][:, e:e + 1], dst, ALU.mult, ALU.add,
                              [po, gates[t], A], [A])
                    else:
                        P.tt("dve", dst, dst, po[:], ALU.add, [po, A], [A])
                    yield

        it = 0
        for e in range(n_exp):
            for fg in range(NFG):
                k = gidx % 2
                g = e * NFG + fg
                for wi, (src, dstb, ceng) in enumerate(((wg_d, wgb[k], "pool"), (wu_d, wub[k], "pool"),
                                                        (wd_d, wdb[k], "act"))):
                    st = stage[(gidx * 3 + wi) % 3]
                    P.dma(st[:], src[g], writes=[st])
                    P.cp(ceng, dstb[:], st[:], [st], [dstb])
                gidx += 1
                for tb in range(NTB):
                    AT = actT[it % 2]
                    it += 1
                    H = hT[tb]
                    dgen = emit_down(pend) if pend is not None else iter(())
                    nmm = 0
                    for j in range(2):
                        for c in range(8):
                            P.mm(pg[j][:], wgb[k][:, c * 256 + j * 128: c * 256 + (j + 1) * 128], H[:, c, :],
                                 c == 0, c == 7, [wgb[k], H], [pg[j]])
                            nmm += 1
                            if nmm % 4 == 0:
                                next(dgen, None)
                        for c in range(8):
                            P.mm(pu[j][:], wub[k][:, c * 256 + j * 128: c * 256 + (j + 1) * 128], H[:, c, :],
                                 c == 0, c == 7, [wub[k], H], [pu[j]])
                            nmm += 1
                            if nmm % 4 == 0:
                                next(dgen, None)
                        P.actv(sil[j][:], pg[j][:], AF.Silu, [pg[j]], [sil[j]])
                        P.tt("dve", AT[j][:], sil[j][:], pu[j][:], ALU.mult, [sil[j], pu[j]], [AT[j]])
                    for _ in dgen:
                        pass
                    pend = (k, e, tb, AT)
        for _ in emit_down(pend):
            pass
        pend = None
        for t in range(NT):
            P.dma(y[r0 + t * 128: r0 + (t + 1) * 128, :], acc[t][:], reads=[acc[t]])
    return


def ffn_layouts(w_out, gain, w_gate, w_up, w_down, router=None):
    E = w_gate.shape[0]
    d = {}
    d["wout_l"] = np.ascontiguousarray(w_out.reshape(8, 128, D).transpose(1, 0, 2).reshape(128, 8 * D))
    d["gain_l"] = np.ascontiguousarray(gain.reshape(8, 128).T)
    d["ident"] = np.eye(128, dtype=np.float32)

    def gu(w):
        return np.ascontiguousarray(
            w.reshape(E, 8, 128, NFG, 256).transpose(0, 3, 2, 1, 4).reshape(E * NFG, 128, 2048))
    d["wg_l"] = gu(w_gate)
    d["wu_l"] = gu(w_up)
    d["wd_l"] = np.ascontiguousarray(
        w_down.reshape(E, NFG, 2, 128, D).transpose(0, 1, 3, 2, 4).reshape(E * NFG, 128, 2048))
    if router is not None:
        d["router_l"] = np.ascontiguousarray(router.reshape(8, 128, 8).transpose(1, 0, 2).reshape(128, 64))
    return d


D = 1024
EPS = 1e-6
SEQ = 8192


class FrontEnd:
    def __init__(self, P, x_d, gain, identb, ptr, nx=2, rowmap=None):
        self.P, self.x_d, self.gain, self.identb, self.ptr = P, x_d, gain, identb, ptr
        self.X = [P.sb([128, D], F32, f"feX{i}") for i in range(nx)]
        self.ss = P.sb([128, 1], F32, "fess")
        self.xnb = P.sb([128, D], BF16, "fexnb")
        self.n = 0
        self.rowmap = rowmap

    def block(self, blk, H):
        P = self.P
        for tt_ in range(4):
            Xt = self.X[self.n % len(self.X)]
            self.n += 1
            r = blk * 512 + tt_ * 128
            if self.rowmap is not None:
                r = self.rowmap(r)
            P.dma(Xt[:], self.x_d[r:r + 128, :], writes=[Xt])
            P.actv(self.xnb[:], Xt[:], AF.Square, [Xt], [self.xnb, self.ss], accum_out=self.ss[:])
            P.actv(self.ss[:], self.ss[:], AF.Ln, [self.ss], [self.ss], scale=1.0 / D, bias=EPS)
            P.actv(self.ss[:], self.ss[:], AF.Exp, [self.ss], [self.ss], scale=-0.5)
            P.ts("dve", self.xnb[:], Xt[:], self.ss[:, 0:1], ALU.mult, [Xt, self.ss], [self.xnb])
            for c in range(8):
                P.tr(self.ptr[:, c, :], self.xnb[:, c * 128:(c + 1) * 128], self.identb[:],
                     [self.xnb, self.identb], [self.ptr])
            P.tt("dve", H[:, :, tt_ * 128:(tt_ + 1) * 128], self.ptr[:],
                 self.gain[:].unsqueeze(2).to_broadcast([128, 8, 128]), ALU.mult, [self.ptr, self.gain], [H])


def load_w_bf16(P, dst, src_d, ncols, stage, eng="pool"):
    tot = 8 * ncols
    flat = dst[:].rearrange("p c n -> p (c n)")
    q = 0
    i = 0
    SW = stage[0].t.shape[1]
    while q < tot:
        w = min(SW, tot - q)
        st = stage[i % len(stage)]
        P.dma(st[:, 0:w], src_d[:, q:q + w], writes=[st])
        P.cp(eng, flat[:, q:q + w], st[:, 0:w], [st], [dst])
        q += w
        i += 1


def emit_ssd(ctx, seq=SEQ):
    nc, P, tag = ctx.nc, ctx.P, ctx.tag
    NBLK = seq // 512
    x_d = ctx.x_d
    gain_d = nc.dram_tensor(tag + "gain_l", [128, 8], F32, kind="ExternalInput").ap()
    ident_d = nc.dram_tensor(tag + "ident", [128, 128], F32, kind="ExternalInput").ap()
    wch_d = nc.dram_tensor(tag + "wch_l", [128, 8 * 512], F32, kind="ExternalInput").ap()
    wzd_d = nc.dram_tensor(tag + "wzd_l", [128, 8 * 260], F32, kind="ExternalInput").ap()
    convw_d = nc.dram_tensor(tag + "convw_l", [128, 4 * 4], F32, kind="ExternalInput").ap()
    convb_d = nc.dram_tensor(tag + "convb_l", [128, 4], F32, kind="ExternalInput").ap()
    hp_d = nc.dram_tensor(tag + "hp_l", [128, 12], F32, kind="ExternalInput").ap()
    ng_d = nc.dram_tensor(tag + "ng_l", [128, 256], F32, kind="ExternalInput").ap()
    tri_d = nc.dram_tensor(tag + "tri", [128, 384], F32, kind="ExternalInput").ap()
    out = ctx.out

    identf = P.sb([128, 128], F32, "identf")
    identb = P.sb([128, 128], BF16, "identb")
    gain = P.sb([128, 8], F32, "gain")
    convw = P.sb([128, 4, 4], F32, "convw")
    convb = P.sb([128, 4], F32, "convb")
    hp = P.sb([128, 12], F32, "hp")
    ng = P.sb([128, 256], F32, "ng")
    trif = P.sb([128, 384], F32, "trif")
    trib = P.sb([128, 384], BF16, "trib")
    P.dma(identf[:], ident_d, writes=[identf])
    P.cp("dve", identb[:], identf[:], [identf], [identb])
    P.dma(gain[:], gain_d, writes=[gain])
    P.dma(convw[:].rearrange("p a b -> p (a b)"), convw_d, writes=[convw])
    P.dma(convb[:], convb_d, writes=[convb])
    P.dma(hp[:], hp_d, writes=[hp])
    P.dma(ng[:], ng_d, writes=[ng])
    P.dma(trif[:], tri_d, writes=[trif])
    P.cp("dve", trib[:], trif[:], [trif], [trib])
    tri = trib[:, 0:128]
    upp = trif[:, 128:256]
    ones = trib[:, 256:384]
    aneg = P.sb([128, 4], F32, "aneg")
    P.actv(aneg[:], hp[:, 4:8], AF.Exp, [hp], [aneg])
    P.ts("dve", aneg[:], aneg[:], -1.0, ALU.mult, [aneg], [aneg])
    cmask = trif[:, 0:128]

    stage = [P.sb([128, 2048], F32, f"stage{i}") for i in range(2)]
    wch = P.sb([128, 8, 512], BF16, "wch")
    wzd = P.sb([128, 8, 260], BF16, "wzd")
    load_w_bf16(P, wch, wch_d, 512, stage)
    load_w_bf16(P, wzd, wzd_d, 260, stage)

    ptr = P.ps([128, 8, 128], BF16, "ptr")
    pYS = P.ps([128, 512], F32, "pYS")
    pch = [pYS, pYS]
    pZA_2 = [P.ps([128, 512], F32, f"pZA{i}") for i in range(2)]
    pD_2 = [P.ps([128, 512], F32, f"pD{i}") for i in range(2)]
    pYd = [P.ps([128, 512], F32, f"pYd{i}") for i in range(2)]

    fe = FrontEnd(P, x_d, gain, identb, ptr, rowmap=getattr(ctx, 'rowmap', None))
    hTb = [P.sb([128, 8, 512], BF16, f"hT{i}") for i in range(2)]
    xpre = P.sb([128, 4, 515], F32, "xpre")
    xpre_ct = [P.sub(xpre[:, ct, :], f"xpre{ct}") for ct in range(4)]
    P.op("pool", lambda e: e.memset(xpre[:], 0.0), [], xpre_ct)
    cacc = [P.sb([128, 512], F32, f"cacc{i}") for i in range(2)]
    xc = [P.sb([128, 4, 512], BF16, f"xc{i}") for i in range(2)]
    S32 = P.sb([128, 256], F32, "S32")
    Sb = P.sb([128, 256], BF16, "Sb")
    P.op("pool", lambda e: e.memset(S32[:], 0.0), [], [S32])
    P.op("pool", lambda e: e.memset(Sb[:], 0.0), [], [Sb])
    xtok_2 = [P.sb([128, 256], BF16, f"xtok{_i}") for _i in range(2)]
    btok_2 = [P.sb([128, 128], BF16, f"btok{_i}") for _i in range(2)]
    zd_2 = [P.sb([128, 260], F32, f"zd{_i}") for _i in range(2)]
    dt_2 = [P.sb([128, 4], F32, f"dt{_i}") for _i in range(2)]
    ad_2 = [P.sb([128, 4], F32, f"ad{_i}") for _i in range(2)]
    adh_2 = [P.sb([128, 4], BF16, f"adh{_i}") for _i in range(2)]
    adl_2 = [P.sb([128, 4], BF16, f"adl{_i}") for _i in range(2)]
    cs_2 = [P.sb([128, 8], F32, f"cs{_i}") for _i in range(2)]
    ec_2 = [P.sb([128, 4], F32, f"ec{_i}") for _i in range(2)]
    ed_2 = [P.sb([128, 4], F32, f"ed{_i}") for _i in range(2)]
    et_2 = [P.sb([128, 4], F32, f"et{_i}") for _i in range(2)]
    wdt_2 = [P.sb([128, 4], F32, f"wdt{_i}") for _i in range(2)]
    Ah_2 = [P.sb([128, 4, 128], BF16, f"Ah{_i}") for _i in range(2)]
    Al_2 = [P.sb([128, 4, 128], BF16, f"Al{_i}") for _i in range(2)]
    Aful_2 = [P.sb([128, 4, 128], F32, f"Aful{_i}") for _i in range(2)]
    E_2 = [P.sb([128, 4, 128], F32, f"E{_i}") for _i in range(2)]
    Gm_2 = [P.sb([128, 128], F32, f"Gm{_i}") for _i in range(2)]
    Mh_2 = [P.sb([128, 4, 128], BF16, f"Mh{_i}") for _i in range(2)]
    xw_2 = [P.sb([128, 256], BF16, f"xw{_i}") for _i in range(2)]
    y1_2 = [P.sb([128, 256], F32, f"y1{_i}") for _i in range(2)]
    y2_2 = [P.sb([128, 256], F32, f"y2{_i}") for _i in range(2)]
    sz_2 = [P.sb([128, 256], F32, f"sz{_i}") for _i in range(2)]
    junk_2 = [P.sb([128, 256], F32, f"junk{_i}") for _i in range(2)]
    ssq_2 = [P.sb([128, 1], F32, f"ssq{_i}") for _i in range(2)]
    yo = [P.sb([128, 256], F32, f"yo{i}") for i in range(2)]

    for blk in range(NBLK):
        H = hTb[blk % 2]
        fe.block(blk, H)
        XC = xc[blk % 2]
        for ct in range(4):
            pc = pch[ct % 2]
            for c in range(8):
                P.mm(pc[:], wch[:, c, ct * 128:(ct + 1) * 128], H[:, c, :], c == 0, c == 7, [wch, H], [pc])
            xp = xpre_ct[ct]
            P.cp("act", xp[:, 3:515], pc[:], [pc], [xp])
            ca = cacc[ct % 2]
            P.ts("dve", ca[:], xp[:, 0:512], convw[:, ct, 0:1], ALU.mult, [xp, convw], [ca])
            for w in range(1, 4):
                P.stt("dve", ca[:], xp[:, w:w + 512], convw[:, ct, w:w + 1], ca[:], ALU.mult, ALU.add,
                      [xp, convw, ca], [ca])
            P.actv(XC[:, ct, :], ca[:], AF.Silu, [ca, convb], [XC], bias=convb[:, ct:ct + 1], scale=1.0)
            P.cp("pool", xp[:, 0:3], xp[:, 512:515], [xp], [xp])
        def do_tile(blk, tt_, H, XC):
            tok = slice(tt_ * 128, (tt_ + 1) * 128)
            r = blk * 512 + tt_ * 128
            pYd_ = pYd[tt_ % 2]
            pz = pZA_2[tt_ % 2]
            pA = pz
            pD = pD_2[tt_ % 2]
            po_ = 4 * (tt_ % 2)
            xtok = xtok_2[tt_ % 2]; btok = btok_2[tt_ % 2]; zd = zd_2[tt_ % 2]; dt = dt_2[tt_ % 2]; ad = ad_2[tt_ % 2]; adh = adh_2[tt_ % 2]; adl = adl_2[tt_ % 2]; cs = cs_2[tt_ % 2]; ec = ec_2[tt_ % 2]; ed = ed_2[tt_ % 2]; et = et_2[tt_ % 2]; wdt = wdt_2[tt_ % 2]; Ah = Ah_2[tt_ % 2]; Al = Al_2[tt_ % 2]; Aful = Aful_2[tt_ % 2]; E = E_2[tt_ % 2]; Gm = Gm_2[tt_ % 2]; Mh = Mh_2[tt_ % 2]; xw = xw_2[tt_ % 2]; y1 = y1_2[tt_ % 2]; y2 = y2_2[tt_ % 2]; sz = sz_2[tt_ % 2]; junk = junk_2[tt_ % 2]; ssq = ssq_2[tt_ % 2]
            yield
            for c in range(8):
                P.mm(pz[:, 0:260], H[:, c, tok], wzd[:, c, :], c == 0, c == 7, [H, wzd], [pz])
            yield
            P.cp("act", zd[:], pz[:, 0:260], [pz], [zd])
            yield
            P.tt("dve", dt[:], zd[:, 256:260], hp[:, 0:4], ALU.add, [zd, hp], [dt])
            yield
            P.actv(dt[:], dt[:], AF.Exp, [dt], [dt])
            yield
            P.actv(dt[:], dt[:], AF.Ln, [dt], [dt], bias=1.0, scale=1.0)
            yield
            P.tt("dve", ad[:], dt[:], aneg[:], ALU.mult, [dt, aneg], [ad])
            yield
            P.cp("dve", adh[:], ad[:], [ad], [adh])
            yield
            P.tt("dve", adl[:], ad[:], adh[:], ALU.subtract, [ad, adh], [adl])
            yield
            P.tr(ptr[:, po_ + 0, :], XC[:, 0, tok], identb[:], [XC, identb], [ptr])
            yield
            P.tr(ptr[:, po_ + 1, :], XC[:, 1, tok], identb[:], [XC, identb], [ptr])
            yield
            P.tr(ptr[:, po_ + 2, :], XC[:, 2, tok], identb[:], [XC, identb], [ptr])
            yield
            P.cp("act", xtok[:].rearrange("p (a b) -> p a b", a=2), ptr[:, po_:po_ + 2, :], [ptr], [xtok])
            yield
            P.cp("act", btok[:], ptr[:, po_ + 2, :], [ptr], [btok])
            yield
            P.mm(pA[:, 260:388], XC[:, 2, tok], XC[:, 3, tok], True, True, [XC], [pA])
            yield
            P.mm(pA[:, 388:392], tri, adh[:], True, False, [trib, adh], [pA])
            yield
            P.mm(pA[:, 388:392], tri, adl[:], False, True, [trib, adl], [pA])
            yield
            P.mm(pA[:, 392:396], ones, adh[:], True, False, [trib, adh], [pA])
            yield
            P.mm(pA[:, 392:396], ones, adl[:], False, True, [trib, adl], [pA])
            yield
            P.cp("dve", cs[:], pA[:, 388:396], [pA], [cs])
            yield
            P.tt("dve", Gm[:], pA[:, 260:388], cmask, ALU.mult, [pA, trif], [Gm])
            yield
            P.actv(ec[:], cs[:, 0:4], AF.Exp, [cs], [ec])
            yield
            P.actv(et[:], cs[:, 4:8], AF.Exp, [cs], [et])
            yield
            P.tt("dve", ed[:], cs[:, 4:8], cs[:, 0:4], ALU.subtract, [cs], [ed])
            yield
            P.actv(ed[:], ed[:], AF.Exp, [ed], [ed])
            yield
            P.tt("dve", wdt[:], ed[:], dt[:], ALU.mult, [ed, dt], [wdt])
            yield
            for h in range(4):
                P.ts("dve", Aful[:, h, :], upp, ad[:, h:h + 1], ALU.mult, [trif, ad], [Aful])
            yield
            P.cp("dve", Ah[:], Aful[:], [Aful], [Ah])
            yield
            P.tt("dve", Al[:], Aful[:], Ah[:], ALU.subtract, [Aful, Ah], [Al])
            yield
            for h in range(4):
                P.mm(pD[:, h * 128:(h + 1) * 128], Ah[:, h, :], tri, True, False, [Ah, trib], [pD])
                P.mm(pD[:, h * 128:(h + 1) * 128], Al[:, h, :], tri, False, True, [Al, trib], [pD])
            yield
            P.actv(E[:].rearrange("p a b -> p (a b)"), pD[:], AF.Exp, [pD], [E])
            yield
            for h in range(4):
                P.stt("dve", Mh[:, h, :], E[:, h, :], dt[:, h:h + 1], Gm[:], ALU.mult, ALU.mult,
                      [E, dt, Gm], [Mh])
            yield
            for h in range(4):
                P.mm(pYd_[:, h * 64:(h + 1) * 64], Mh[:, h, :], xtok[:, h * 64:(h + 1) * 64], True, True,
                     [Mh, xtok], [pYd_])
            yield
            P.tt("dve", xw[:].rearrange("p (h d) -> p h d", h=4), xtok[:].rearrange("p (h d) -> p h d", h=4),
                 wdt[:].unsqueeze(2).to_broadcast([128, 4, 64]), ALU.mult, [xtok, wdt], [xw])

            def stage_b():
                P.mm(pYS[:, 0:256], XC[:, 3, tok], Sb[:], True, True, [XC, Sb], [pYS])
                P.mm(pYS[:, 256:512], btok[:], xw[:], True, True, [btok, xw], [pYS])
                P.tt("dve", y1[:].rearrange("p (h d) -> p h d", h=4), pYS[:, 0:256].rearrange("p (h d) -> p h d", h=4),
                     ec[:].unsqueeze(2).to_broadcast([128, 4, 64]), ALU.mult, [pYS, ec], [y1])
                P.tt("dve", y1[:], y1[:], pYd_[:, 0:256], ALU.add, [y1, pYd_], [y1])
                P.tt("dve", S32[:].rearrange("p (h d) -> p h d", h=4), S32[:].rearrange("p (h d) -> p h d", h=4),
                     et[:].unsqueeze(2).to_broadcast([128, 4, 64]), ALU.mult, [S32, et], [S32])
                P.tt("dve", S32[:], S32[:], pYS[:, 256:512], ALU.add, [S32, pYS], [S32])
                P.cp("dve", Sb[:], S32[:], [S32], [Sb])
                P.tt("pool", y2[:].rearrange("p (h d) -> p h d", h=4), xtok[:].rearrange("p (h d) -> p h d", h=4),
                     hp[:, 8:12].unsqueeze(2).to_broadcast([128, 4, 64]), ALU.mult, [xtok, hp], [y2])
                P.tt("dve", y1[:], y1[:], y2[:], ALU.add, [y1, y2], [y1])
                P.actv(sz[:], zd[:, 0:256], AF.Silu, [zd], [sz])
                P.tt("dve", y1[:], y1[:], sz[:], ALU.mult, [y1, sz], [y1])
                P.actv(junk[:], y1[:], AF.Square, [y1], [junk, ssq], accum_out=ssq[:])
                P.actv(ssq[:], ssq[:], AF.Ln, [ssq], [ssq], scale=1.0 / 256, bias=EPS)
                P.actv(ssq[:], ssq[:], AF.Exp, [ssq], [ssq], scale=-0.5)
                YO = yo[tt_ % 2]
                P.stt("dve", YO[:], y1[:], ssq[:, 0:1], ng[:], ALU.mult, ALU.mult, [y1, ssq, ng], [YO])
                P.dma(out[r:r + 128, :], YO[:], reads=[YO])

            return stage_b

        for pr_ in range(2):
            gens = [do_tile(blk, 2 * pr_ + i_, H, XC) for i_ in range(2)]
            stage_bs = [None, None]
            live = [True, True]
            while any(live):
                for i_ in range(2):
                    if live[i_]:
                        try:
                            next(gens[i_])
                        except StopIteration as fin_:
                            stage_bs[i_] = fin_.value
                            live[i_] = False
            for fb_ in stage_bs:
                fb_()
    return


def ssd_consts():
    m = np.arange(128)
    tri = (m[:, None] <= m[None, :]).astype(np.float32)
    upp = (m[:, None] > m[None, :]).astype(np.float32)
    ones = np.ones((128, 128), np.float32)
    return {"tri": np.concatenate([tri, upp, ones], 1), "ident": np.eye(128, dtype=np.float32)}


def wl(w):
    n = w.shape[1]
    return np.ascontiguousarray(w.reshape(8, 128, n).transpose(1, 0, 2).reshape(128, 8 * n))


def rep(v, n=128):
    v = np.asarray(v, np.float32).reshape(1, -1)
    return np.ascontiguousarray(np.broadcast_to(v, (n, v.shape[1])))


def ssd_inputs(g, x_b, norm_gain, w_in, conv_w, conv_b, dt_bias, a_log, d_skip, ssm_norm_gain):
    o_z = 512 + 512 + 512
    o_xbc = o_z + 512
    o_dt = o_xbc + 1024
    zc = w_in[:, o_z + g * 256: o_z + (g + 1) * 256]
    xcols = w_in[:, o_xbc + g * 256: o_xbc + (g + 1) * 256]
    bcols = w_in[:, o_xbc + 512 + g * 128: o_xbc + 512 + (g + 1) * 128]
    ccols = w_in[:, o_xbc + 768 + g * 128: o_xbc + 768 + (g + 1) * 128]
    dtc = w_in[:, o_dt + g * 4: o_dt + (g + 1) * 4]
    chan = np.concatenate([np.arange(g * 256, (g + 1) * 256), 512 + np.arange(g * 128, (g + 1) * 128),
                           768 + np.arange(g * 128, (g + 1) * 128)])
    cw = conv_w[:, chan]
    cb = conv_b[chan]
    d = dict(ssd_consts())
    d["x"] = np.ascontiguousarray(x_b)
    d["gain_l"] = np.ascontiguousarray(norm_gain.reshape(8, 128).T)
    d["wch_l"] = wl(np.concatenate([xcols, bcols, ccols], 1))
    d["wzd_l"] = wl(np.concatenate([zc, dtc], 1))
    d["convw_l"] = np.ascontiguousarray(cw.reshape(4, 4, 128).transpose(2, 1, 0).reshape(128, 16))
    d["convb_l"] = np.ascontiguousarray(cb.reshape(4, 128).T)
    hs = slice(4 * g, 4 * g + 4)
    d["hp_l"] = rep(np.concatenate([dt_bias[hs], a_log[hs], d_skip[hs]]))
    d["ng_l"] = rep(ssm_norm_gain[g * 256:(g + 1) * 256])
    return d


NEG = -30000.0


def run_attention(P, steps, ST, PT, scale):
    n = len(steps)

    def do_qk(i):
        s = steps[i]
        st = ST[i % 2]
        m = len(s["qk"])
        for a, (l, r, rd) in enumerate(s["qk"]):
            P.mm(st[:], l, r, a == 0, a == m - 1, rd, [st])

    def do_pv(i):
        s = steps[i]
        st, pt = ST[i % 2], PT[i % 2]
        P.actv(pt[:], st[:], AF.Exp, [st], [pt], scale=scale)
        vr, vrd = s["v"]
        for (ob, oap, cs, first, last) in s["pv"]:
            P.op("pe", lambda e, oap=oap, l_=pt[:, cs], vr=vr, first=first, last=last: e.matmul(oap, lhsT=l_, rhs=vr, start=first, stop=last, skip_group_check=True), [pt] + vrd, [ob])
        if s.get("fin"):
            s["fin"]()

    pending = None
    for i in range(n):
        if pending is not None and steps[pending].get("sync"):
            do_pv(pending)
            pending = None
        do_qk(i)
        if pending is not None:
            do_pv(pending)
        pending = i
    do_pv(pending)


def causal_steps(nsb, qk_fn, v_fn, O_fn, fin_fn, identb, maskd, extra_fn=None):
    steps = []
    for sb in range(nsb):
        nk = 4 * sb + 4
        for kt in range(nk):
            t = kt - 4 * sb
            qk = list(qk_fn(sb, kt))
            if extra_fn is not None:
                qk += extra_fn(sb, kt)
            if t >= 0:
                qk.append((identb[:], maskd[:, t, :], [identb, maskd]))
            pv = []
            for j in range(4):
                if t > j:
                    continue
                ob, oap = O_fn(sb, j)
                pv.append((ob, oap, slice(j * 128, (j + 1) * 128), kt == 0 and j % 2 == 0, kt == 4 * sb + j))
            steps.append(dict(qk=qk, pv=pv, v=v_fn(kt), fin=(fin_fn(sb) if kt == nk - 1 else None)))
    return steps


def tok_l(a):
    n = a.shape[1]
    return np.ascontiguousarray(a.reshape(-1, 128, n).transpose(1, 0, 2).reshape(128, -1))


def diag_masks():
    k = np.arange(128)[:, None]
    q = np.arange(512)[None, :]
    m = np.stack([np.where(128 * t + k <= q, 0.0, NEG) for t in range(4)], 1)
    return np.ascontiguousarray(m.reshape(128, 4 * 512).astype(np.float32))


def rope_tables(seq, dim):
    inv = 1.0 / (10000.0 ** (np.arange(0, dim, 2, dtype=np.float32) / dim))
    ang = np.arange(seq, dtype=np.float32)[:, None] * inv[None, :].astype(np.float32)
    return np.cos(ang).astype(np.float32), np.sin(ang).astype(np.float32)


def emit_rmsrope(P, src32, nh, dh, gainrep, cos_ap, sin_ap, dst, scr, cs_reads, rope_cols=None):
    sq, ssq, qn, ta, tb = scr["sq"], scr["ssq"], scr["qn"], scr["ta"], scr["tb"]
    W = nh * dh
    hh = dh // 2
    v3 = lambda b: b[:, 0:W].rearrange("p (h d) -> p h d", h=nh)
    P.tt("pool", sq[:, 0:W], src32[:, 0:W], src32[:, 0:W], ALU.mult, [src32], [sq])
    P.op("dve", lambda e: e.reduce_sum(out=ssq[:, 0:nh], in_=v3(sq), axis=AX.X), [sq], [ssq])
    P.actv(ssq[:, 0:nh], ssq[:, 0:nh], AF.Ln, [ssq], [ssq], scale=1.0 / dh, bias=EPS)
    P.actv(ssq[:, 0:nh], ssq[:, 0:nh], AF.Exp, [ssq], [ssq], scale=-0.5)
    P.tt("dve", v3(qn), v3(src32), ssq[:, 0:nh].unsqueeze(2).to_broadcast([128, nh, dh]), ALU.mult,
         [src32, ssq], [qn])
    P.tt("pool", qn[:, 0:W], qn[:, 0:W], gainrep[:, 0:W], ALU.mult, [qn, gainrep], [qn])
    if cos_ap is None:
        P.cp("dve", dst[:, 0:W], qn[:, 0:W], [qn], [dst])
        return
    cb = cos_ap.unsqueeze(1).to_broadcast([128, nh, hh])
    sb_ = sin_ap.unsqueeze(1).to_broadcast([128, nh, hh])
    q3 = v3(qn)
    d3 = v3(dst)
    a3 = ta[:, 0:nh * hh].rearrange("p (h d) -> p h d", h=nh)
    b3 = tb[:, 0:nh * hh].rearrange("p (h d) -> p h d", h=nh)
    P.tt("dve", a3, q3[:, :, 0:hh], cb, ALU.mult, [qn] + cs_reads, [ta])
    P.tt("pool", b3, q3[:, :, hh:dh], sb_, ALU.mult, [qn] + cs_reads, [tb])
    P.tt("dve", d3[:, :, 0:hh], a3, b3, ALU.subtract, [ta, tb], [dst])
    P.tt("dve", a3, q3[:, :, hh:dh], cb, ALU.mult, [qn] + cs_reads, [ta])
    P.tt("pool", b3, q3[:, :, 0:hh], sb_, ALU.mult, [qn] + cs_reads, [tb])
    P.tt("dve", d3[:, :, hh:dh], a3, b3, ALU.add, [ta, tb], [dst])


def emit_diffattn(ctx, seq=SEQ, layer_idx=0):
    import math
    lam_init = 0.8 - 0.6 * math.exp(-0.3 * layer_idx)
    nc, P, tag = ctx.nc, ctx.P, ctx.tag
    NBLK = seq // 512
    NT = seq // 128
    x_d = ctx.x_d
    gain_d = nc.dram_tensor(tag + "gain_l", [128, 8], F32, kind="ExternalInput").ap()
    ident_d = nc.dram_tensor(tag + "ident", [128, 128], F32, kind="ExternalInput").ap()
    wqk_d = nc.dram_tensor(tag + "wqk_l", [128, 8 * 512], F32, kind="ExternalInput").ap()
    wv_d = nc.dram_tensor(tag + "wv_l", [128, 8 * 256], F32, kind="ExternalInput").ap()
    gqk_d = nc.dram_tensor(tag + "gqk_l", [128, 512], F32, kind="ExternalInput").ap()
    cos_d = nc.dram_tensor(tag + "cos", [128, NT * 32], F32, kind="ExternalInput").ap()
    sin_d = nc.dram_tensor(tag + "sin", [128, NT * 32], F32, kind="ExternalInput").ap()
    lam_d = nc.dram_tensor(tag + "lam_l", [128, 256], F32, kind="ExternalInput").ap()
    sg_d = nc.dram_tensor(tag + "sg_l", [128, 128], F32, kind="ExternalInput").ap()
    mask_d = nc.dram_tensor(tag + "maskd_d", [128, 4 * 512], F32, kind="ExternalInput").ap()
    out = ctx.out

    identf = P.sb([128, 128], F32, "identf")
    identb = P.sb([128, 128], BF16, "identb")
    gain = P.sb([128, 8], F32, "gain")
    gqk = P.sb([128, 512], F32, "gqk")
    lam = P.sb([128, 256], F32, "lam")
    sg = P.sb([128, 128], F32, "sg")
    cos_t = P.sb([128, NT, 32], F32, "cos_t")
    sin_t = P.sb([128, NT, 32], F32, "sin_t")
    maskd = P.sb([128, 4, 512], BF16, "maskd")
    stage = [P.sb([128, 2048], F32, f"stage{i}") for i in range(2)]
    P.dma(identf[:], ident_d, writes=[identf])
    P.cp("dve", identb[:], identf[:], [identf], [identb])
    P.dma(gain[:], gain_d, writes=[gain])
    P.dma(gqk[:], gqk_d, writes=[gqk])
    P.dma(lam[:], lam_d, writes=[lam])
    P.dma(sg[:], sg_d, writes=[sg])
    P.dma(cos_t[:].rearrange("p t d -> p (t d)"), cos_d, writes=[cos_t])
    P.dma(sin_t[:].rearrange("p t d -> p (t d)"), sin_d, writes=[sin_t])
    P.dma(stage[0][:], mask_d, writes=[stage[0]])
    P.cp("dve", maskd[:].rearrange("p a b -> p (a b)"), stage[0][:], [stage[0]], [maskd])
    P.ts("dve", sg[:], sg[:], 1.0 - lam_init, ALU.mult, [sg], [sg])
    lp = P.sb([128, 128], F32, "lp")
    ls = P.sb([128, 2], F32, "ls")
    neglam = P.sb([128, 1], F32, "neglam")
    P.tt("dve", lp[:, 0:64], lam[:, 0:64], lam[:, 64:128], ALU.mult, [lam], [lp])
    P.tt("dve", lp[:, 64:128], lam[:, 128:192], lam[:, 192:256], ALU.mult, [lam], [lp])
    P.op("dve", lambda e: e.reduce_sum(out=ls[:], in_=lp[:].rearrange("p (a b) -> p a b", a=2), axis=AX.X), [lp], [ls])
    P.actv(ls[:], ls[:], AF.Exp, [ls], [ls])
    P.tt("dve", neglam[:], ls[:, 1:2], ls[:, 0:1], ALU.subtract, [ls], [neglam])
    P.ts("dve", neglam[:], neglam[:], -lam_init, ALU.add, [neglam], [neglam])

    wqk = P.sb([128, 8, 512], BF16, "wqk")
    wv = P.sb([128, 8, 256], BF16, "wv")
    load_w_bf16(P, wqk, wqk_d, 512, stage)
    load_w_bf16(P, wv, wv_d, 256, stage)

    QT = P.sb([128, 2, seq], BF16, "QT")
    KT = P.sb([128, 2, seq], BF16, "KT")
    Vaug = P.sb([128, NT, 2, 129], BF16, "Vaug")
    P.op("dve", lambda e: e.memset(Vaug[:, :, :, 128:129], 1.0), [], [Vaug])

    ST = [P.ps([128, 512], F32, f"ST{i}") for i in range(2)]
    OA = [P.ps([128, 2, 256], F32, f"OA{i}") for i in range(2)]
    OB = [P.ps([128, 2, 256], F32, f"OB{i}") for i in range(2)]
    ptr = P.ps([128, 8, 128], BF16, "ptr")
    PT = [P.sb([128, 512], BF16, f"PT{i}") for i in range(2)]

    fe = FrontEnd(P, x_d, gain, identb, ptr, rowmap=getattr(ctx, 'rowmap', None))
    hTb = [P.sb([128, 8, 512], BF16, f"hT{i}") for i in range(2)]
    qk32_2 = [P.sb([128, 512], F32, f"qk32{i}") for i in range(2)]
    scr_2 = [dict(sq=P.sb([128, 512], F32, f"sq{i}"), ssq=P.sb([128, 8], F32, f"ssq{i}"), qn=P.sb([128, 512], F32, f"qn{i}"),
                  ta=P.sb([128, 256], F32, f"ta{i}"), tb=P.sb([128, 256], F32, f"tb{i}")) for i in range(2)]
    qkr_2 = [P.sb([128, 512], BF16, f"qkr{i}") for i in range(2)]

    for blk in range(NBLK):
        H = hTb[blk % 2]
        fe.block(blk, H)
        for tt_ in range(4):
            tok = slice(tt_ * 128, (tt_ + 1) * 128)
            ti = blk * 4 + tt_
            qk32, scr, qkr = qk32_2[ti % 2], scr_2[ti % 2], qkr_2[ti % 2]
            pq = OA[ti % 2]
            pv = OB[ti % 2]
            pqf = pq[:].rearrange("p a b -> p (a b)")
            pvf = pv[:].rearrange("p a b -> p (a b)")
            for c in range(8):
                P.mm(pqf, H[:, c, tok], wqk[:, c, :], c == 0, c == 7, [H, wqk], [pq])
            for c in range(8):
                P.mm(pvf[:, 0:256], H[:, c, tok], wv[:, c, :], c == 0, c == 7, [H, wv], [pv])
            P.cp("act", qk32[:], pqf, [pq], [qk32])
            P.cp("dve", Vaug[:, ti, :, 0:128], pvf[:, 0:256].rearrange("p (h d) -> p h d", h=2), [pv], [Vaug])
            emit_rmsrope(P, qk32, 8, 64, gqk, cos_t[:, ti, :], sin_t[:, ti, :], qkr, scr, [cos_t, sin_t])
            for g4 in range(4):
                P.tr(ptr[:, g4, :], qkr[:, g4 * 128:(g4 + 1) * 128], identb[:], [qkr, identb], [ptr])
            gt = slice(ti * 128, (ti + 1) * 128)
            P.cp("act", QT[:, :, gt], ptr[:, 0:2, :], [ptr], [QT])
            P.cp("act", KT[:, :, gt], ptr[:, 2:4, :], [ptr], [KT])

    tmpo = [P.sb([128, 4, 128], F32, f"tmpo{i}") for i in range(2)]
    rs = P.sb([128, 4], F32, "rs")
    o32 = P.sb([128, 128], F32, "o32")
    junk = P.sb([128, 128], F32, "junk")
    s1 = P.sb([128, 1], F32, "s1")
    yo = [P.sb([128, 128], F32, f"yo{i}") for i in range(4)]
    cnt = [0]
    scale = 64 ** -0.5
    for head in range(2):
        steps = []
        for sb in range(NBLK):
            for comp in range(2):
                Oset = OA if comp == 0 else OB
                ps = slice(comp * 64, (comp + 1) * 64)

                def qk_fn(sb_, kt, head=head, ps=ps):
                    return [(KT[ps, head, kt * 128:(kt + 1) * 128], QT[ps, head, sb_ * 512:(sb_ + 1) * 512], [KT, QT])]

                def v_fn(kt, head=head):
                    return (Vaug[:, kt, head, :], [Vaug])

                def O_fn(sb_, j, Oset=Oset):
                    return (Oset[j // 2], Oset[j // 2][:, j % 2, 0:129])

                def fin_fn(sb_, comp=comp, head=head, Oset=Oset):
                    def fin():
                        T = tmpo[sb_ % 2]
                        if comp == 0:
                            for j in range(4):
                                ob = Oset[j // 2]
                                P.op("dve", lambda e, ob=ob, j=j: e.reciprocal(out=rs[:, j:j + 1], in_=ob[:, j % 2, 128:129]),
                                     [ob], [rs])
                                P.ts("dve", T[:, j, :], ob[:, j % 2, 0:128], rs[:, j:j + 1], ALU.mult, [ob, rs], [T])
                        else:
                            for j in range(4):
                                ob = Oset[j // 2]
                                P.op("dve", lambda e, ob=ob, j=j: e.reciprocal(out=rs[:, j:j + 1], in_=ob[:, j % 2, 128:129]),
                                     [ob], [rs])
                                P.tt("dve", rs[:, j:j + 1], rs[:, j:j + 1], neglam[:], ALU.mult, [rs, neglam], [rs])
                                P.stt("dve", o32[:], ob[:, j % 2, 0:128], rs[:, j:j + 1], T[:, j, :], ALU.mult, ALU.add,
                                      [ob, rs, T], [o32])
                                P.actv(junk[:], o32[:], AF.Square, [o32], [junk, s1], accum_out=s1[:])
                                P.actv(s1[:], s1[:], AF.Ln, [s1], [s1], scale=1.0 / 128, bias=EPS)
                                P.actv(s1[:], s1[:], AF.Exp, [s1], [s1], scale=-0.5)
                                Y = yo[cnt[0] % 4]
                                cnt[0] += 1
                                P.stt("dve", Y[:], o32[:], s1[:, 0:1], sg[:], ALU.mult, ALU.mult, [o32, s1, sg], [Y])
                                r = sb_ * 512 + j * 128
                                P.dma(out[r:r + 128, head * 128:(head + 1) * 128], Y[:], reads=[Y])
                    return fin

                nk = 4 * sb + 4
                for kt in range(nk):
                    t = kt - 4 * sb
                    qk = qk_fn(sb, kt)
                    if t >= 0:
                        qk.append((identb[:], maskd[:, t, :], [identb, maskd]))
                    pv = []
                    for j in range(4):
                        if t > j:
                            continue
                        ob, oap = O_fn(sb, j)
                        pv.append((ob, oap, slice(j * 128, (j + 1) * 128), kt == 0 and j % 2 == 0, kt == 4 * sb + j))
                    steps.append(dict(qk=qk, pv=pv, v=v_fn(kt), fin=(fin_fn(sb) if kt == nk - 1 else None)))
        run_attention(P, steps, ST, PT, scale)
    return


def diffattn_inputs(hp, x_b, norm_gain, w_in, q_gain, k_gain, lam, subln_gain, seq):
    q = w_in[:, hp * 256:(hp + 1) * 256]
    k = w_in[:, 512 + hp * 256: 512 + (hp + 1) * 256]
    v = w_in[:, 1024 + hp * 256: 1024 + (hp + 1) * 256]
    cos, sin = rope_tables(seq, 64)
    d = {"ident": np.eye(128, dtype=np.float32)}
    d["x"] = np.ascontiguousarray(x_b)
    d["gain_l"] = np.ascontiguousarray(norm_gain.reshape(8, 128).T)
    d["wqk_l"] = wl(np.concatenate([q, k], 1))
    d["wv_l"] = wl(v)
    d["gqk_l"] = rep(np.concatenate([np.tile(q_gain, 4), np.tile(k_gain, 4)]))
    d["cos"] = tok_l(cos)
    d["sin"] = tok_l(sin)
    d["lam_l"] = rep(lam.reshape(-1))
    d["sg_l"] = rep(subln_gain)
    d["maskd_d"] = diag_masks()
    return d


def rmsrope3(P, src3, src_reads, nh, dh, gain3, gain_reads, cos_ap, sin_ap, cs_reads, dsts, scr):
    sq, ssq, qn, ta, tb = scr["sq"], scr["ssq"], scr["qn"], scr["ta"], scr["tb"]
    W = nh * dh
    hh = dh // 2
    v3 = lambda b, w=dh: b[:, 0:nh * w].rearrange("p (h d) -> p h d", h=nh)
    P.tt("pool", v3(sq), src3, src3, ALU.mult, src_reads, [sq])
    P.op("dve", lambda e: e.reduce_sum(out=ssq[:, 0:nh], in_=v3(sq), axis=AX.X), [sq], [ssq])
    P.actv(ssq[:, 0:nh], ssq[:, 0:nh], AF.Ln, [ssq], [ssq], scale=1.0 / dh, bias=EPS)
    P.actv(ssq[:, 0:nh], ssq[:, 0:nh], AF.Exp, [ssq], [ssq], scale=-0.5)
    P.tt("dve", v3(qn), src3, ssq[:, 0:nh].unsqueeze(2).to_broadcast([128, nh, dh]), ALU.mult,
         src_reads + [ssq], [qn])
    if cos_ap is None:
        for (d3, db) in dsts:
            P.tt("pool", d3, v3(qn), gain3, ALU.mult, [qn] + gain_reads, [db])
        return
    P.tt("pool", v3(qn), v3(qn), gain3, ALU.mult, [qn] + gain_reads, [qn])
    cb = cos_ap.unsqueeze(1).to_broadcast([128, nh, hh])
    sb_ = sin_ap.unsqueeze(1).to_broadcast([128, nh, hh])
    q3 = v3(qn)
    a3 = v3(ta, hh)
    b3 = v3(tb, hh)
    P.tt("dve", a3, q3[:, :, 0:hh], cb, ALU.mult, [qn] + cs_reads, [ta])
    P.tt("pool", b3, q3[:, :, hh:dh], sb_, ALU.mult, [qn] + cs_reads, [tb])
    for (d3, db) in dsts:
        P.tt("dve", d3[:, :, 0:hh], a3, b3, ALU.subtract, [ta, tb], [db])
    P.tt("dve", a3, q3[:, :, hh:dh], cb, ALU.mult, [qn] + cs_reads, [ta])
    P.tt("pool", b3, q3[:, :, 0:hh], sb_, ALU.mult, [qn] + cs_reads, [tb])
    for (d3, db) in dsts:
        P.tt("dve", d3[:, :, hh:dh], a3, b3, ALU.add, [ta, tb], [db])


class BankStart:
    def __init__(self):
        self.started = {}

    def new_round(self, buf):
        self.started[id(buf)] = False

    def flag(self, buf):
        if not self.started.get(id(buf), False):
            self.started[id(buf)] = True
            return True
        return False


def emit_mla(ctx, seq=SEQ):
    nc, P, tag = ctx.nc, ctx.P, ctx.tag
    NBLK = seq // 512
    NT = seq // 128
    x_d = ctx.x_d
    gain_d = nc.dram_tensor(tag + "gain_l", [128, 8], F32, kind="ExternalInput").ap()
    ident_d = nc.dram_tensor(tag + "ident", [128, 128], F32, kind="ExternalInput").ap()
    win_d = nc.dram_tensor(tag + "win_l", [128, 8 * 448], F32, kind="ExternalInput").ap()
    wuq_d = nc.dram_tensor(tag + "wuq_l", [128, 2 * 384], F32, kind="ExternalInput").ap()
    wukv_d = nc.dram_tensor(tag + "wukv_l", [128, 512], F32, kind="ExternalInput").ap()
    g1_d = nc.dram_tensor(tag + "g1_l", [128, 448], F32, kind="ExternalInput").ap()
    g2_d = nc.dram_tensor(tag + "g2_l", [128, 320], F32, kind="ExternalInput").ap()
    cos_d = nc.dram_tensor(tag + "cos", [128, NT * 32], F32, kind="ExternalInput").ap()
    sin_d = nc.dram_tensor(tag + "sin", [128, NT * 32], F32, kind="ExternalInput").ap()
    mask_d = nc.dram_tensor(tag + "maskd_d", [128, 4 * 512], F32, kind="ExternalInput").ap()
    out = ctx.out

    identf = P.sb([128, 128], F32, "identf")
    identb = P.sb([128, 128], BF16, "identb")
    gain = P.sb([128, 8], F32, "gain")
    g1 = P.sb([128, 448], F32, "g1")
    g2 = P.sb([128, 320], F32, "g2")
    cst = [P.sb([128, 32], F32, f"cst{i}") for i in range(2)]
    maskd = P.sb([128, 4, 512], BF16, "maskd")
    stage = [P.sb([128, 2048], F32, f"stage{i}") for i in range(1)] * 2
    P.dma(identf[:], ident_d, writes=[identf])
    P.cp("dve", identb[:], identf[:], [identf], [identb])
    P.dma(gain[:], gain_d, writes=[gain])
    P.dma(g1[:], g1_d, writes=[g1])
    P.dma(g2[:], g2_d, writes=[g2])
    P.dma(stage[0][:], mask_d, writes=[stage[0]])
    P.cp("dve", maskd[:].rearrange("p a b -> p (a b)"), stage[0][:], [stage[0]], [maskd])
    win = P.sb([128, 8, 448], BF16, "win")
    wuq = P.sb([128, 2, 384], BF16, "wuq")
    wukv = P.sb([128, 512], BF16, "wukv")
    load_w_bf16(P, win, win_d, 448, stage)
    P.dma(stage[1][:, 0:768], wuq_d, writes=[stage[1]])
    P.cp("pool", wuq[:].rearrange("p c n -> p (c n)"), stage[1][:, 0:768], [stage[1]], [wuq])
    P.dma(stage[0][:, 0:512], wukv_d, writes=[stage[0]])
    P.cp("pool", wukv[:], stage[0][:, 0:512], [stage[0]], [wukv])

    QnT = P.sb([128, 2, seq], BF16, "QnT")
    KnT = P.sb([128, 2, seq], BF16, "KnT")
    QrT = P.sb([128, seq], BF16, "QrT")
    KrT = P.sb([128, seq], BF16, "KrT")
    Vaug = P.sb([128, NT, 2, 129], BF16, "Vaug")
    P.op("dve", lambda e: e.memset(Vaug[:, :, :, 128:129], 1.0), [], [Vaug])

    ST = [P.ps([128, 512], F32, f"ST{i}") for i in range(2)]
    OA = [P.ps([128, 2, 256], F32, f"OA{i}") for i in range(2)]
    OB = [P.ps([128, 2, 256], F32, f"OB{i}") for i in range(2)]
    ptr = P.ps([128, 8, 128], BF16, "ptr")
    PT = [P.sb([128, 512], BF16, f"PT{i}") for i in range(2)]

    fe = FrontEnd(P, x_d, gain, identb, ptr, rowmap=getattr(ctx, 'rowmap', None))
    hTb = [P.sb([128, 8, 512], BF16, f"hT{i}") for i in range(1)] * 2
    c32_2 = [P.sb([128, 448], F32, f"c32{i}") for i in range(2)]
    cn_2 = [P.sb([128, 512], BF16, f"cn{i}") for i in range(2)]
    cT_2 = [P.sb([128, 3, 128], BF16, f"cT{i}") for i in range(2)]
    q32_2 = [P.sb([128, 384], F32, f"q32{i}") for i in range(2)]
    kv32_2 = [P.sb([128, 512], F32, f"kv32{i}") for i in range(2)]
    qkb_2 = [P.sb([128, 5, 128], BF16, f"qkb{i}") for i in range(2)]
    scr_2 = [dict(sq=P.sb([128, 256], F32, f"sq{i}"), ssq=P.sb([128, 8], F32, f"ssq{i}"), qn=P.sb([128, 256], F32, f"qn{i}"),
                  ta=P.sb([128, 128], F32, f"ta{i}"), tb=P.sb([128, 128], F32, f"tb{i}")) for i in range(2)]

    for blk in range(NBLK):
        H = hTb[blk % 2]
        fe.block(blk, H)
        for tt_ in range(4):
            tok = slice(tt_ * 128, (tt_ + 1) * 128)
            ti = blk * 4 + tt_
            gt = slice(ti * 128, (ti + 1) * 128)
            c32, cn, cT, q32, kv32, qkb, scr = (c32_2[ti % 2], cn_2[ti % 2], cT_2[ti % 2], q32_2[ti % 2],
                                                kv32_2[ti % 2], qkb_2[ti % 2], scr_2[ti % 2])
            cs_, sn_ = cst[0], cst[1]
            P.dma(cs_[:], cos_d[:, ti * 32:(ti + 1) * 32], writes=[cs_])
            P.dma(sn_[:], sin_d[:, ti * 32:(ti + 1) * 32], writes=[sn_])
            pc = OA[0]
            pcf = pc[:].rearrange("p a b -> p (a b)")
            for c in range(8):
                P.mm(pcf[:, 0:448], H[:, c, tok], win[:, c, :], c == 0, c == 7, [H, win], [pc])
            P.cp("act", c32[:], pcf[:, 0:448], [pc], [c32])
            rmsrope3(P, c32[:, 0:256].rearrange("p (h d) -> p h d", h=1), [c32], 1, 256,
                     g1[:, 0:256].rearrange("p (h d) -> p h d", h=1), [g1], None, None, [],
                     [(cn[:, 0:256].rearrange("p (h d) -> p h d", h=1), cn)], scr)
            rmsrope3(P, c32[:, 256:384].rearrange("p (h d) -> p h d", h=1), [c32], 1, 128,
                     g1[:, 256:384].rearrange("p (h d) -> p h d", h=1), [g1], None, None, [],
                     [(cn[:, 256:384].rearrange("p (h d) -> p h d", h=1), cn)], scr)
            rmsrope3(P, c32[:, 384:448].rearrange("p (h d) -> p h d", h=1), [c32], 1, 64,
                     g1[:, 384:448].rearrange("p (h d) -> p h d", h=1), [g1], cs_[:], sn_[:],
                     [cs_, sn_],
                     [(cn[:, 384:448].rearrange("p (h d) -> p h d", h=1), cn),
                      (cn[:, 448:512].rearrange("p (h d) -> p h d", h=1), cn)], scr)
            for c in range(4):
                P.tr(ptr[:, c, :], cn[:, c * 128:(c + 1) * 128], identb[:], [cn, identb], [ptr])
            P.cp("act", cT[:], ptr[:, 0:3, :], [ptr], [cT])
            P.cp("act", KrT[:, gt], ptr[:, 3, :], [ptr], [KrT])
            pq = OA[1]
            pkv = OB[0]
            pqf = pq[:].rearrange("p a b -> p (a b)")
            pkvf = pkv[:].rearrange("p a b -> p (a b)")
            for c in range(2):
                P.mm(pqf[:, 0:384], cT[:, c, :], wuq[:, c, :], c == 0, c == 1, [cT, wuq], [pq])
            P.mm(pkvf, cT[:, 2, :], wukv[:], True, True, [cT, wukv], [pkv])
            P.cp("act", q32[:], pqf[:, 0:384], [pq], [q32])
            P.cp("act", kv32[:], pkvf, [pkv], [kv32])
            q3 = q32[:].rearrange("p (h d) -> p h d", h=2)
            kv3 = kv32[:].rearrange("p (h d) -> p h d", h=2)
            rmsrope3(P, q3[:, :, 0:128], [q32], 2, 128, g2[:, 0:128].unsqueeze(1).to_broadcast([128, 2, 128]), [g2],
                     None, None, [], [(qkb[:, 0:2, :], qkb)], scr)
            rmsrope3(P, q3[:, :, 128:192], [q32], 2, 64, g2[:, 128:192].unsqueeze(1).to_broadcast([128, 2, 64]), [g2],
                     cs_[:], sn_[:], [cs_, sn_],
                     [(qkb[:, 2, :].rearrange("p (h d) -> p h d", h=2), qkb)], scr)
            rmsrope3(P, kv3[:, :, 0:128], [kv32], 2, 128, g2[:, 192:320].unsqueeze(1).to_broadcast([128, 2, 128]), [g2],
                     None, None, [], [(qkb[:, 3:5, :], qkb)], scr)
            P.cp("dve", Vaug[:, ti, :, 0:128], kv3[:, :, 128:256], [kv32], [Vaug])
            for c in range(5):
                P.tr(ptr[:, c, :], qkb[:, c, :], identb[:], [qkb, identb], [ptr])
            P.cp("act", QnT[:, :, gt], ptr[:, 0:2, :], [ptr], [QnT])
            P.cp("act", QrT[:, gt], ptr[:, 2, :], [ptr], [QrT])
            P.cp("act", KnT[:, :, gt], ptr[:, 3:5, :], [ptr], [KnT])

    rs = P.sb([128, 4], F32, "rs")
    yo = [P.sb([128, 128], F32, f"yo{i}") for i in range(4)]
    cnt = [0]
    scale = 192 ** -0.5
    bs = BankStart()
    for head in range(2):
        steps = []
        ps = slice(head * 64, (head + 1) * 64)
        for sb in range(NBLK):
            Oset = OA if sb % 2 == 0 else OB

            def fin_fn(sb_=sb, Oset=Oset, head=head):
                def fin():
                    for j in range(4):
                        ob = Oset[j // 2]
                        P.op("dve", lambda e, ob=ob, j=j: e.reciprocal(out=rs[:, j:j + 1], in_=ob[:, j % 2, 128:129]),
                             [ob], [rs])
                        Y = yo[cnt[0] % 4]
                        cnt[0] += 1
                        P.ts("dve", Y[:], ob[:, j % 2, 0:128], rs[:, j:j + 1], ALU.mult, [ob, rs], [Y])
                        r = sb_ * 512 + j * 128
                        P.dma(out[r:r + 128, head * 128:(head + 1) * 128], Y[:], reads=[Y])
                return fin

            nk = 4 * sb + 4
            for kt in range(nk):
                t = kt - 4 * sb
                ks = slice(kt * 128, (kt + 1) * 128)
                qs = slice(sb * 512, (sb + 1) * 512)
                qk = [(KnT[:, head, ks], QnT[:, head, qs], [KnT, QnT]),
                      (KrT[ps, ks], QrT[ps, qs], [KrT, QrT])]
                if t >= 0:
                    qk.append((identb[:], maskd[:, t, :], [identb, maskd]))
                pv = []
                for j in range(4):
                    if t > j:
                        continue
                    ob = Oset[j // 2]
                    pv.append((ob, ob[:, j % 2, 0:129], slice(j * 128, (j + 1) * 128),
                               kt == 0 and j % 2 == 0, kt == 4 * sb + j))
                steps.append(dict(qk=qk, pv=pv, v=(Vaug[:, kt, head, :], [Vaug]),
                                  fin=(fin_fn() if kt == nk - 1 else None)))
        run_attention(P, steps, ST, PT, scale)
    return


def mla_inputs(hp, x_b, norm_gain, w_in, cq_gain, ckv_gain, w_uq, w_ukv, qn_gain, qr_gain, kn_gain, kr_gain, seq):
    o = 512 + 6 * 128 + 24
    cols = w_in[:, o:o + 448]
    cos, sin = rope_tables(seq, 64)
    d = {"ident": np.eye(128, dtype=np.float32)}
    d["x"] = np.ascontiguousarray(x_b)
    d["gain_l"] = np.ascontiguousarray(norm_gain.reshape(8, 128).T)
    d["win_l"] = wl(cols)
    uq = w_uq[:, hp * 384:(hp + 1) * 384]
    d["wuq_l"] = np.ascontiguousarray(uq.reshape(2, 128, 384).transpose(1, 0, 2).reshape(128, 768))
    d["wukv_l"] = np.ascontiguousarray(w_ukv[:, hp * 512:(hp + 1) * 512])
    d["g1_l"] = rep(np.concatenate([cq_gain, ckv_gain, kr_gain]))
    d["g2_l"] = rep(np.concatenate([qn_gain, qr_gain, kn_gain]))
    d["cos"] = tok_l(cos)
    d["sin"] = tok_l(sin)
    d["maskd_d"] = diag_masks()
    return d


def nsa_consts(seq):
    NT = seq // 128
    n_cmp = seq // 16 - 1
    NCT = (n_cmp + 127) // 128
    n_sel = seq // 64
    k = np.arange(128)[:, None]
    q = np.arange(512)[None, :]
    mw = np.stack([np.where((q < 128 * t + k) & (128 * t + k <= q + 512), 0.0, NEG) for t in range(8)], 1)
    mc = np.stack([np.where(16 * k + 31 <= 512 * m + q, 0.0, NEG) for m in range(5)], 1)
    n = np.arange(NCT * 128)[:, None]
    j = np.arange(128)[None, :]
    ov = ((16 * n < 64 * j + 64) & (16 * n + 32 > 64 * j) & (n < n_cmp) & (j < n_sel)).astype(np.float32)
    ovl = ov.reshape(NCT, 128, 128).transpose(1, 0, 2).reshape(128, NCT * 128)
    kk = np.arange(seq)[None, :]
    esel = (kk // 64 == np.arange(128)[:, None]).astype(np.float32)
    qq = np.arange(seq)[:, None]
    cur = qq // 64
    jj = np.arange(128)[None, :]
    forced = (jj == 0) | (jj == cur) | (jj == cur - 1)
    future = (jj * 64 > qq) | (jj >= n_sel)
    A = np.where(future | forced, 0.0, 1.0)
    B = np.where(future, -1e6, np.where(forced, 1e6, 0.0))
    ab = np.concatenate([A, B], 1).astype(np.float32)
    cosf, sinf = rope_tables(seq, 64)
    ce = np.minimum(np.arange(NCT * 128) * 16 + 31, seq - 1)
    d = dict(maskd_d=diag_masks(),
             maskw_d=np.ascontiguousarray(mw.reshape(128, 8 * 512).astype(np.float32)),
             maskc_d=np.ascontiguousarray(mc.reshape(128, 5 * 512).astype(np.float32)),
             ovl_d=np.ascontiguousarray(ovl), esel_d=esel, ab_d=tok_l(ab),
             cos=tok_l(cosf), sin=tok_l(sinf), cosc=tok_l(cosf[ce]), sinc=tok_l(sinf[ce]),
             ident=np.eye(128, dtype=np.float32))
    return d


def emit_nsa(ctx, seq=SEQ):
    nc, P, tag = ctx.nc, ctx.P, ctx.tag
    NBLK = seq // 512
    NT = seq // 128
    n_cmp = seq // 16 - 1
    NCT = (n_cmp + 127) // 128
    NCP = NCT * 128
    x_d = ctx.x_d
    gain_d = nc.dram_tensor(tag + "gain_l", [128, 8], F32, kind="ExternalInput").ap()
    ident_d = nc.dram_tensor(tag + "ident", [128, 128], F32, kind="ExternalInput").ap()
    win_d = nc.dram_tensor(tag + "win_l", [128, 8 * 652], F32, kind="ExternalInput").ap()
    gq_d = nc.dram_tensor(tag + "gq_l", [128, 448], F32, kind="ExternalInput").ap()
    w1_d = nc.dram_tensor(tag + "w1_l", [128, 2048], F32, kind="ExternalInput").ap()
    w1c_d = nc.dram_tensor(tag + "w1c_l", [128, 2048], F32, kind="ExternalInput").ap()
    pos_d = nc.dram_tensor(tag + "pos_l", [128, 32], F32, kind="ExternalInput").ap()
    w2_d = nc.dram_tensor(tag + "w2_l", [64, 128], F32, kind="ExternalInput").ap()
    cos_d = nc.dram_tensor(tag + "cos", [128, NT * 32], F32, kind="ExternalInput").ap()
    sin_d = nc.dram_tensor(tag + "sin", [128, NT * 32], F32, kind="ExternalInput").ap()
    cosc_d = nc.dram_tensor(tag + "cosc", [128, NCT * 32], F32, kind="ExternalInput").ap()
    sinc_d = nc.dram_tensor(tag + "sinc", [128, NCT * 32], F32, kind="ExternalInput").ap()
    maskd_d = nc.dram_tensor(tag + "maskd_d", [128, 4 * 512], F32, kind="ExternalInput").ap()
    maskw_d = nc.dram_tensor(tag + "maskw_d", [128, 8 * 512], F32, kind="ExternalInput").ap()
    maskc_d = nc.dram_tensor(tag + "maskc_d", [128, 5 * 512], F32, kind="ExternalInput").ap()
    ovl_d = nc.dram_tensor(tag + "ovl_d", [128, NCT * 128], F32, kind="ExternalInput").ap()
    esel_d = nc.dram_tensor(tag + "esel_d", [128, seq], F32, kind="ExternalInput").ap()
    ab_d = nc.dram_tensor(tag + "ab_d", [128, NT * 256], F32, kind="ExternalInput").ap()
    out = ctx.out

    identf = P.sb([128, 128], F32, "identf")
    identb = P.sb([128, 128], BF16, "identb")
    gain = P.sb([128, 8], F32, "gain")
    gq = P.sb([128, 448], F32, "gq")
    stage = [P.sb([128, 512], F32, f"stage{i}") for i in range(2)]
    P.dma(identf[:], ident_d, writes=[identf])
    P.cp("dve", identb[:], identf[:], [identf], [identb])
    P.dma(gain[:], gain_d, writes=[gain])
    P.dma(gq[:], gq_d, writes=[gq])

    def load_const_bf16(dst_flat_ap, dst_buf, src_d, n):
        q = 0
        i = 0
        while q < n:
            w = min(512, n - q)
            st = stage[i % 2]
            P.dma(st[:, 0:w], src_d[:, q:q + w], writes=[st])
            P.cp("pool", dst_flat_ap[:, q:q + w], st[:, 0:w], [st], [dst_buf])
            q += w
            i += 1

    maskd = P.sb([128, 4, 512], BF16, "maskd")
    maskw = P.sb([128, 8, 512], BF16, "maskw")
    maskc = P.sb([128, 5, 512], BF16, "maskc")
    esel = P.sb([128, seq], BF16, "esel")
    load_const_bf16(maskd[:].rearrange("p a b -> p (a b)"), maskd, maskd_d, 2048)
    load_const_bf16(maskw[:].rearrange("p a b -> p (a b)"), maskw, maskw_d, 4096)
    load_const_bf16(maskc[:].rearrange("p a b -> p (a b)"), maskc, maskc_d, 2560)
    load_const_bf16(esel[:], esel, esel_d, seq)
    win = P.sb([128, 8, 652], BF16, "win")
    load_w_bf16(P, win, win_d, 652, stage)
    w1 = P.sb([128, 32, 64], BF16, "w1")
    load_const_bf16(w1[:].rearrange("p a b -> p (a b)"), w1, w1_d, 2048)
    w1c = P.sb([128, 2, 16, 64], BF16, "w1c")
    load_const_bf16(w1c[:].rearrange("p a b c -> p (a b c)"), w1c, w1c_d, 2048)
    posb = P.sb([128, 2, 16], BF16, "posb")
    w2 = P.sb([64, 2, 64], BF16, "w2")
    cosc = P.sb([128, NCT, 32], F32, "cosc_t")
    sinc = P.sb([128, NCT, 32], F32, "sinc_t")
    P.dma(stage[0][:, 0:32], pos_d, writes=[stage[0]])
    P.cp("pool", posb[:].rearrange("p a b -> p (a b)"), stage[0][:, 0:32], [stage[0]], [posb])
    P.dma(stage[1][0:64, 0:128], w2_d, writes=[stage[1]])
    P.cp("pool", w2[:].rearrange("p a b -> p (a b)"), stage[1][0:64, 0:128], [stage[1]], [w2])
    P.dma(cosc[:].rearrange("p t d -> p (t d)"), cosc_d, writes=[cosc])
    P.dma(sinc[:].rearrange("p t d -> p (t d)"), sinc_d, writes=[sinc])

    import os
    if os.environ.get("NSA_PAD"):
        pad_ = P.sb([128, int(os.environ["NSA_PAD"])], F32, "pad_")
    QT2 = P.sb([128, 2, seq], BF16, "QT2")
    KsT2 = P.sb([128, seq], BF16, "KsT2")
    KwT2 = P.sb([128, seq], BF16, "KwT2")
    kcvT = P.sb([128, seq], BF16, "kcvT")
    Vs = P.sb([128, NT, 65], BF16, "Vs")
    Vw = P.sb([128, NT, 65], BF16, "Vw")
    gts = P.sb([128, NT, 12], F32, "gts")
    CkT2 = P.sb([128, NCP], BF16, "CkT2")
    Cv = P.sb([128, NCT, 193], BF16, "Cv")
    P.op("dve", lambda e: e.memset(Vs[:, :, 64:65], 1.0), [], [Vs])
    P.op("dve", lambda e: e.memset(Vw[:, :, 64:65], 1.0), [], [Vw])
    P.op("dve", lambda e: e.memset(Cv[:, :, 64:65], 1.0), [], [Cv])
    st = stage[0]
    P.dma(st[:, 0:NCT * 128], ovl_d, writes=[st])
    P.cp("dve", Cv[:, :, 65:193], st[:, 0:NCT * 128].rearrange("p (a b) -> p a b", a=NCT), [st], [Cv])

    ST = [P.ps([128, 512], F32, f"ST{i}") for i in range(2)]
    Oc = [P.ps([128, 2, 256], F32, f"Oc{i}") for i in range(2)]
    OX = P.ps([128, 4, 128], F32, "OX")
    OY = P.ps([128, 4, 128], F32, "OY")
    ptr = P.ps([128, 8, 128], BF16, "ptr")
    PT = [P.sb([128, 512], BF16, f"PT{i}") for i in range(2)]

    fe = FrontEnd(P, x_d, gain, identb, ptr, nx=1, rowmap=getattr(ctx, 'rowmap', None))
    hTb = [P.sb([128, 8, 512], BF16, f"hT{i}") for i in range(1)]
    p32 = P.sb([128, 652], F32, "p32")
    qb = P.sb([128, 256], BF16, "qb")
    kb = P.sb([128, 384], BF16, "kb")
    cst = [P.sb([128, 64], F32, f"cst{i}") for i in range(2)]
    scr = dict(sq=P.sb([128, 256], F32, "sq"), ssq=P.sb([128, 8], F32, "ssq"), qn=P.sb([128, 256], F32, "qn"),
               ta=P.sb([128, 128], F32, "ta"), tb=P.sb([128, 128], F32, "tb"))
    g1 = P.sb([128, 12], F32, "g1")

    for blk in range(NBLK):
        H = hTb[0]
        fe.block(blk, H)
        for tt_ in range(4):
            tok = slice(tt_ * 128, (tt_ + 1) * 128)
            ti = blk * 4 + tt_
            gt = slice(ti * 128, (ti + 1) * 128)
            pA = Oc[0]
            pB = Oc[1]
            pAf = pA[:].rearrange("p a b -> p (a b)")
            pBf = pB[:].rearrange("p a b -> p (a b)")
            for c in range(8):
                P.mm(pAf, H[:, c, tok], win[:, c, 0:512], c == 0, c == 7, [H, win], [pA])
            for c in range(8):
                P.mm(pBf[:, 0:140], H[:, c, tok], win[:, c, 512:652], c == 0, c == 7, [H, win], [pB])
            P.cp("act", p32[:, 0:512], pAf, [pA], [p32])
            P.cp("act", p32[:, 512:652], pBf[:, 0:140], [pB], [p32])
            cs_, sn_ = cst[0], cst[1]
            P.dma(cs_[:, 0:32], cos_d[:, ti * 32:(ti + 1) * 32], writes=[cs_])
            P.dma(sn_[:, 0:32], sin_d[:, ti * 32:(ti + 1) * 32], writes=[sn_])
            one = lambda ap: ap.rearrange("p (h d) -> p h d", h=1)
            rmsrope3(P, p32[:, 0:256].rearrange("p (h d) -> p h d", h=4), [p32], 4, 64,
                     gq[:, 0:256].rearrange("p (h d) -> p h d", h=4), [gq], cs_[:, 0:32], sn_[:, 0:32], [cs_, sn_],
                     [(qb[:].rearrange("p (h d) -> p h d", h=4), qb)], scr)
            rmsrope3(P, one(p32[:, 384:448]), [p32], 1, 64, one(gq[:, 256:320]), [gq], cs_[:, 0:32], sn_[:, 0:32],
                     [cs_, sn_], [(one(kb[:, 0:64]), kb), (one(kb[:, 64:128]), kb)], scr)
            rmsrope3(P, one(p32[:, 512:576]), [p32], 1, 64, one(gq[:, 320:384]), [gq], cs_[:, 0:32], sn_[:, 0:32],
                     [cs_, sn_], [(one(kb[:, 128:192]), kb), (one(kb[:, 192:256]), kb)], scr)
            P.cp("dve", kb[:, 256:384], p32[:, 256:384], [p32], [kb])
            P.cp("dve", Vs[:, ti, 0:64], p32[:, 448:512], [p32], [Vs])
            P.cp("dve", Vw[:, ti, 0:64], p32[:, 576:640], [p32], [Vw])
            P.actv(g1[:], p32[:, 640:652], AF.Exp, [p32], [g1], scale=-1.0)
            P.ts("dve", g1[:], g1[:], 1.0, ALU.add, [g1], [g1])
            P.op("dve", lambda e, ti=ti: e.reciprocal(out=gts[:, ti, :], in_=g1[:]), [g1], [gts])
            P.tr(ptr[:, 0, :], qb[:, 0:128], identb[:], [qb, identb], [ptr])
            P.tr(ptr[:, 1, :], qb[:, 128:256], identb[:], [qb, identb], [ptr])
            for c in range(3):
                P.tr(ptr[:, 2 + c, :], kb[:, c * 128:(c + 1) * 128], identb[:], [kb, identb], [ptr])
            P.cp("act", QT2[:, :, gt], ptr[:, 0:2, :], [ptr], [QT2])
            P.cp("act", KsT2[:, gt], ptr[:, 2, :], [ptr], [KsT2])
            P.cp("act", KwT2[:, gt], ptr[:, 3, :], [ptr], [KwT2])
            P.cp("act", kcvT[:, gt], ptr[:, 4, :], [ptr], [kcvT])

    hmid = P.sb([64, 512], BF16, "hmid")
    cb16 = P.sb([64, NCP], BF16, "cb16")
    cbias = P.sb([64, 2], F32, "cbias")
    ctok = P.sb([128, 64], F32, "ctok")
    ckn = P.sb([128, 128], BF16, "ckn")
    P.op("dve", lambda e: e.memset(cb16[:], 0.0), [], [cb16])
    for kv in range(2):
        ps = slice(kv * 64, (kv + 1) * 64)
        pc = Oc[kv]
        pcf = pc[:].rearrange("p a b -> p (a b)")
        for c in range(16):
            P.mm(pcf[0:64, 511:512], w1c[:, kv, c, :], posb[:, kv, c:c + 1], c == 0, c == 15, [w1c, posb], [pc])
        P.cp("dve", cbias[:, kv:kv + 1], pcf[0:64, 511:512], [pc], [cbias])
        for pos in range(32):
            rhs = kcvT[ps, pos: pos + 16 * (n_cmp - 1) + 1: 16]
            P.mm(pcf[0:64, 0:n_cmp], w1[ps, pos, :], rhs, pos == 0, pos == 31, [w1, kcvT], [pc])
        P.actv(hmid[:, 0:n_cmp], pcf[0:64, 0:n_cmp], AF.Silu, [pc, cbias], [hmid], bias=cbias[:, kv:kv + 1], scale=1.0)
        P.mm(pcf[0:64, 0:n_cmp], w2[:, kv, :], hmid[:, 0:n_cmp], True, True, [w2, hmid], [pc])
        P.cp("act", cb16[:, 0:n_cmp], pcf[0:64, 0:n_cmp], [pc], [cb16])
        for nt in range(NCT):
            P.tr(ptr[:, nt, 0:64], cb16[:, nt * 128:(nt + 1) * 128], identb[0:64, 0:64], [cb16, identb], [ptr])
        if kv == 0:
            for nt in range(NCT):
                P.cp("act", ctok[:], ptr[:, nt, 0:64], [ptr], [ctok])
                one = lambda ap: ap.rearrange("p (h d) -> p h d", h=1)
                rmsrope3(P, one(ctok[:]), [ctok], 1, 64, one(gq[:, 384:448]), [gq], cosc[:, nt, :], sinc[:, nt, :],
                         [cosc, sinc], [(one(ckn[:, 0:64]), ckn), (one(ckn[:, 64:128]), ckn)], scr)
                P.tr(ptr[:, 4 + nt % 4, :], ckn[:], identb[:], [ckn, identb], [ptr])
                P.cp("act", CkT2[:, nt * 128:(nt + 1) * 128], ptr[:, 4 + nt % 4, :], [ptr], [CkT2])
        else:
            P.cp("dve", Cv[:, :, 0:64], ptr[:, 0:NCT, 0:64], [ptr], [Cv])

    bs = BankStart()
    scale = 64 ** -0.5
    oc = [P.sb([128, 4, 4, 64], F32, f"oc{i}") for i in range(1)]
    imp = [P.sb([128, 4, 128], F32, f"imp{i}") for i in range(1)] * 2
    negsel = [P.sb([128, 512], BF16, f"negsel{i}") for i in range(2)]
    ABt = [P.sb([128, 256], F32, f"ABt{i}") for i in range(2)]
    impf = P.sb([128, 128], F32, "impf")
    imp2 = P.sb([128, 128], F32, "imp2")
    m8a = P.sb([128, 8], F32, "m8a")
    m8b = P.sb([128, 8], F32, "m8b")
    selb = P.sb([128, 128], BF16, "selb")
    rs = P.sb([128, 4], F32, "rs")
    acc = [P.sb([128, 4, 64], F32, f"acc{i}") for i in range(2)]
    yout = [P.sb([128, 4, 256], F32, f"yout{i}") for i in range(1)]
    steps = []
    for sb in range(NBLK):
        qs = slice(sb * 512, (sb + 1) * 512)
        OC, IMP, NS, YO = oc[0], imp[sb % 2], negsel[sb % 2], yout[0]
        nts = [nt for nt in range(NCT) if sb - 4 * nt >= 0]
        for h in range(4):
            pair, hh = h // 2, h % 2
            ps = slice(hh * 64, (hh + 1) * 64)

            def fin_c(h=h, OC=OC, IMP=IMP, sb=sb, NS=NS):
                for j in range(4):
                    ob = Oc[j // 2]
                    P.ts("dve", rs[:, j:j + 1], ob[:, j % 2, 64:65], 1e-30, ALU.max, [ob], [rs])
                    P.op("dve", lambda e, j=j: e.reciprocal(out=rs[:, j:j + 1], in_=rs[:, j:j + 1]), [rs], [rs])
                    P.ts("dve", OC[:, j, h, :], ob[:, j % 2, 0:64], rs[:, j:j + 1], ALU.mult, [ob, rs], [OC])
                    if h == 0:
                        P.ts("dve", IMP[:, j, :], ob[:, j % 2, 65:193], rs[:, j:j + 1], ALU.mult, [ob, rs], [IMP])
                    else:
                        P.stt("dve", IMP[:, j, :], ob[:, j % 2, 65:193], rs[:, j:j + 1], IMP[:, j, :], ALU.mult, ALU.add,
                              [ob, rs, IMP], [IMP])
                if h == 3:
                    for j in range(4):
                        ti = sb * 4 + j
                        AB = ABt[j % 2]
                        P.dma(AB[:], ab_d[:, ti * 256:(ti + 1) * 256], writes=[AB])
                        P.tt("dve", impf[:], IMP[:, j, :], AB[:, 0:128], ALU.mult, [IMP, AB], [impf])
                        P.tt("dve", impf[:], impf[:], AB[:, 128:256], ALU.add, [impf, AB], [impf])
                        P.op("dve", lambda e: e.max(out=m8a[:], in_=impf[:]), [impf], [m8a])
                        P.op("dve", lambda e: e.match_replace(out=imp2[:], in_to_replace=m8a[:], in_values=impf[:],
                                                              imm_value=-3.0e38), [m8a, impf], [imp2])
                        P.op("dve", lambda e: e.max(out=m8b[:], in_=imp2[:]), [imp2], [m8b])
                        P.ts("dve", selb[:], impf[:], m8b[:, 7:8], ALU.is_ge, [impf, m8b], [selb])
                        P.tr(ptr[:, j, :], selb[:], identb[:], [selb, identb], [ptr])
                        P.ts("dve", NS[:, j * 128:(j + 1) * 128], ptr[:, j, :], -1.0, ALU.add, [ptr], [NS],
                             s2=-NEG, op1=ALU.mult)

            for a, nt in enumerate(nts):
                m = sb - 4 * nt
                qk = [(CkT2[ps, nt * 128:(nt + 1) * 128], QT2[ps, pair, qs], [CkT2, QT2])]
                if m <= 4:
                    qk.append((identb[:], maskc[:, m, :], [identb, maskc]))
                pv = []
                for j in range(4):
                    ob = Oc[j // 2]
                    pv.append((ob, ob[:, j % 2, 0:193], slice(j * 128, (j + 1) * 128),
                               a == 0 and j % 2 == 0, a == len(nts) - 1))
                steps.append(dict(qk=qk, pv=pv, v=(Cv[:, nt, :], [Cv]),
                                  fin=(fin_c if a == len(nts) - 1 else None),
                                  sync=(h == 3 and a == len(nts) - 1)))
        for h in range(4):
            pair, hh = h // 2, h % 2
            ps = slice(hh * 64, (hh + 1) * 64)
            ACC = acc[h % 2]

            def fin_s(h=h, ACC=ACC, OC=OC, sb=sb):
                for j in range(4):
                    ti = sb * 4 + j
                    P.op("dve", lambda e, j=j: e.reciprocal(out=rs[:, j:j + 1], in_=OX[:, j, 64:65]), [OX], [rs])
                    P.tt("dve", rs[:, j:j + 1], rs[:, j:j + 1], gts[:, ti, h * 3 + 1:h * 3 + 2], ALU.mult, [rs, gts], [rs])
                    P.ts("dve", ACC[:, j, :], OX[:, j, 0:64], rs[:, j:j + 1], ALU.mult, [OX, rs], [ACC])
                    import os
                    _m = os.environ.get("NSA_DBG", "")
                    if _m in ("w", "c"):
                        P.ts("dve", ACC[:, j, :], ACC[:, j, :], 0.0, ALU.mult, [ACC], [ACC])
                    if _m in ("", "c"):
                        P.stt("dve", ACC[:, j, :], OC[:, j, h, :], gts[:, ti, h * 3:h * 3 + 1], ACC[:, j, :], ALU.mult, ALU.add,
                              [OC, gts, ACC], [ACC])

            def fin_w(h=h, ACC=ACC, YO=YO, sb=sb):
                for j in range(4):
                    ti = sb * 4 + j
                    P.op("dve", lambda e, j=j: e.reciprocal(out=rs[:, j:j + 1], in_=OY[:, j, 64:65]), [OY], [rs])
                    P.tt("dve", rs[:, j:j + 1], rs[:, j:j + 1], gts[:, ti, h * 3 + 2:h * 3 + 3], ALU.mult, [rs, gts], [rs])
                    import os
                    if os.environ.get("NSA_DBG", "") in ("s", "c"):
                        P.ts("dve", rs[:, j:j + 1], rs[:, j:j + 1], 0.0, ALU.mult, [rs], [rs])
                    P.stt("dve", YO[:, j, h * 64:(h + 1) * 64], OY[:, j, 0:64], rs[:, j:j + 1], ACC[:, j, :],
                          ALU.mult, ALU.add, [OY, rs, ACC], [YO])
                if h == 3:
                    for j in range(4):
                        r = sb * 512 + j * 128
                        P.dma(out[r:r + 128, :], YO[:, j, :], reads=[YO])

            nk = 4 * sb + 4
            for kt in range(nk):
                t = kt - 4 * sb
                ks = slice(kt * 128, (kt + 1) * 128)
                qk = [(KsT2[ps, ks], QT2[ps, pair, qs], [KsT2, QT2]),
                      (esel[:, ks], NS[:], [esel, NS])]
                if t >= 0:
                    qk.append((identb[:], maskd[:, t, :], [identb, maskd]))
                pv = []
                for j in range(4):
                    if t > j:
                        continue
                    pv.append((OX, OX[:, j, 0:65], slice(j * 128, (j + 1) * 128), kt == 0 and j == 0, kt == 4 * sb + j))
                steps.append(dict(qk=qk, pv=pv, v=(Vs[:, kt, :], [Vs]), fin=(fin_s if kt == nk - 1 else None)))
            kts = [kt for kt in range(4 * sb - 4, 4 * sb + 4) if kt >= 0]
            first_done = False
            for kt in kts:
                t8 = kt - (4 * sb - 4)
                ks = slice(kt * 128, (kt + 1) * 128)
                qk = [(KwT2[ps, ks], QT2[ps, pair, qs], [KwT2, QT2]),
                      (identb[:], maskw[:, t8, :], [identb, maskw])]
                pv = []
                for j in range(4):
                    if not (j <= t8 <= j + 4):
                        continue
                    pv.append((OY, OY[:, j, 0:65], slice(j * 128, (j + 1) * 128), not first_done, t8 == j + 4))
                    first_done = True
                steps.append(dict(qk=qk, pv=pv, v=(Vw[:, kt, :], [Vw]), fin=(fin_w if kt == kts[-1] else None)))
    run_attention(P, steps, ST, PT, scale)
    if os.environ.get("NSA_DUMP"):
        dbg = nc.dram_tensor(tag + "dbg", [128, 4 * 1024], F32, kind="ExternalOutput").ap()
        for i, (src, srcb) in enumerate(((QT2[:, 0, 0:1024], QT2), (KsT2[:, 0:1024], KsT2), (KwT2[:, 0:1024], KwT2),
                                         (CkT2[:, 0:512], CkT2))):
            w = 1024 if i < 3 else 512
            for hf in range(w // 256):
                P.cp("dve", impf[:].rearrange("p a -> p a")[:, 0:128], impf[:, 0:128], [impf], [impf]) if False else None
                t_ = yout[0]
                P.cp("dve", t_[:, 0, 0:256], src[:, hf * 256:(hf + 1) * 256], [srcb], [t_])
                P.dma(dbg[:, i * 1024 + hf * 256: i * 1024 + (hf + 1) * 256], t_[:, 0, 0:256], reads=[t_])
    return


def nsa_inputs(g, x_b, norm_gain, w_in, q_gain, k_gain, cmp_pos, cmp_w1, cmp_w2, seq):
    q = w_in[:, g * 256:(g + 1) * 256]
    kvs = [w_in[:, 512 + i * 128 + g * 64: 512 + i * 128 + (g + 1) * 64] for i in range(6)]
    gl = w_in[:, 512 + 768 + g * 12: 512 + 768 + (g + 1) * 12]
    d = nsa_consts(seq)
    d["x"] = np.ascontiguousarray(x_b)
    d["gain_l"] = np.ascontiguousarray(norm_gain.reshape(8, 128).T)
    d["win_l"] = wl(np.concatenate([q] + kvs + [gl], 1))
    d["gq_l"] = rep(np.concatenate([np.tile(q_gain, 4), k_gain[1], k_gain[2], k_gain[0]]))
    w1 = cmp_w1.reshape(2, 32, 64, 64).transpose(0, 2, 1, 3).reshape(128, 2048)
    d["w1_l"] = np.ascontiguousarray(w1)
    d["w1c_l"] = np.ascontiguousarray(cmp_w1.reshape(2, 16, 128, 64).transpose(2, 0, 1, 3).reshape(128, 2048))
    d["pos_l"] = np.ascontiguousarray(cmp_pos.reshape(2, 16, 128).transpose(2, 0, 1).reshape(128, 32))
    d["w2_l"] = np.ascontiguousarray(cmp_w2.transpose(1, 0, 2).reshape(64, 128))
    return d


PAIRS = [[0, 1], [2, 3], [4, 5], [6, 7]]
_CACHE = {}


class Ctx:
    pass


def build_fused(S=SEQ):
    nc = bass.Bass("TRN2", target_bir_lowering=False)
    P = Prog(nc)
    NTK = S // 2
    x_full = nc.dram_tensor("x_full", [S, D], F32, kind="ExternalInput").ap()
    x_rows = nc.dram_tensor("x_rows", [NTK, D], F32, kind="ExternalInput").ap()
    sel_d = nc.dram_tensor("sel", [128, 2], F32, kind="ExternalInput").ap()
    y = nc.dram_tensor("y", [NTK, D], F32, kind="ExternalOutput").ap()
    mixA = nc.dram_tensor("mixA_my", [S, 512], F32, kind="Internal").ap()
    GA = nc.dram_tensor("GA_all", [2 * S, 512], F32, kind="Internal").ap()
    x1_my = nc.dram_tensor("x1_my", [NTK, D], F32, kind="Internal").ap()
    x1_full = nc.dram_tensor("x1_full", [S, D], F32, kind="Internal").ap()
    mixC = nc.dram_tensor("mixC_my", [S, 512], F32, kind="Internal").ap()
    GC = nc.dram_tensor("GC_all", [2 * S, 512], F32, kind="Internal").ap()

    def phase(tag, fn, *args, **kw):
        ctx = Ctx()
        ctx.nc, ctx.P, ctx.tag = nc, P, tag + "_"
        ctx.S, ctx.sel_d = S, sel_d
        for k, v in kw.items():
            setattr(ctx, k, v)
        P.begin_phase(tag)
        fn(ctx, *args)
        P.end_phase()

    def gather(tag, src, dst):
        rows, width = src.shape
        R = (2 * 1024 * 1024) // (width * 4)
        P.begin_phase(tag)
        for k in range(rows // R):
            P.op("pool", lambda e, k=k: e.collective_compute(
                "AllGather", ALU.bypass, replica_groups=PAIRS,
                ins=[src[k * R:(k + 1) * R, :]], outs=[dst[2 * k * R:2 * (k + 1) * R, :]]), cc=True)
        P.end_phase()

    RM = (2 * 1024 * 1024) // (512 * 4)
    RX = (2 * 1024 * 1024) // (D * 4)
    gaddr = lambda p, t: (t // RM) * 2 * RM + p * RM + (t % RM)
    x1map = lambda T: ((T % NTK) // RX) * 2 * RX + (T // NTK) * RX + (T % RX)

    phase("ssd", emit_ssd, S, x_d=x_full, out=mixA[:, 256:512])
    phase("da", emit_diffattn, S, 0, x_d=x_full, out=mixA[:, 0:256])
    gather("e1", mixA, GA)
    phase("ffn", emit_ffn, 1, NTK, xin=x_rows, G=GA, out=x1_my, gaddr=gaddr)
    gather("e2", x1_my, x1_full)
    phase("nsa", emit_nsa, S, x_d=x1_full, out=mixC[:, 0:256], rowmap=x1map)
    phase("mla", emit_mla, S, x_d=x1_full, out=mixC[:, 256:512], rowmap=x1map)
    gather("e3", mixC, GC)
    phase("moe", emit_ffn, 8, NTK, xin=x1_my, G=GC, out=y, gaddr=gaddr)
    return nc


_PERM = np.concatenate([np.arange(0, 256), np.arange(512, 768), np.arange(256, 512), np.arange(768, 1024)])


def kernel(**inp):
    inp = {k: np.asarray(v) for k, v in inp.items()}
    x = np.ascontiguousarray(inp["x"], dtype=np.float32)
    B, S, _ = x.shape
    NTK = S // 2
    f = lambda k: inp[k][0]
    if "nc" not in _CACHE:
        _CACHE["nc"] = build_fused(S)
    nc = _CACHE["nc"]
    lay_ffn = ffn_layouts(f("ev_w_out")[_PERM], f("ev_norm_ffn"), inp["ffn_w_gate"], inp["ffn_w_up"], inp["ffn_w_down"])
    lay_moe = ffn_layouts(f("od_w_out")[_PERM], f("od_norm_ffn"), f("moe_w_gate"), f("moe_w_up"), f("moe_w_down"),
                          f("moe_router"))
    per_half = []
    for h in range(2):
        d = {}

        def add(tag, dd):
            for k, v in dd.items():
                if k != "x":
                    d[tag + "_" + k] = v
        add("ssd", ssd_inputs(h, x[0], f("ev_norm_mix"), f("ev_w_in"), f("ssm_conv_w"), f("ssm_conv_b"),
                              f("ssm_dt_bias"), f("ssm_a_log"), f("ssm_d"), f("ssm_norm_gain")))
        add("da", diffattn_inputs(h, x[0], f("ev_norm_mix"), f("ev_w_in"), f("da_q_gain"), f("da_k_gain"),
                                  f("da_lambda"), f("da_subln_gain"), S))
        add("nsa", nsa_inputs(h, x[0], f("od_norm_mix"), f("od_w_in"), f("nsa_q_gain"), f("nsa_k_gain"),
                              f("nsa_cmp_pos"), f("nsa_cmp_w1"), f("nsa_cmp_w2"), S))
        add("mla", mla_inputs(h, x[0], f("od_norm_mix"), f("od_w_in"), f("mla_cq_gain"), f("mla_ckv_gain"),
                              f("mla_w_uq"), f("mla_w_ukv"), f("mla_qn_gain"), f("mla_qr_gain"), f("mla_kn_gain"),
                              f("mla_kr_gain"), S))
        add("ffn", lay_ffn)
        add("moe", lay_moe)
        d["sel"] = np.ascontiguousarray(np.broadcast_to(np.array([[1.0 - h, float(h)]], np.float32), (128, 2)))
        per_half.append(d)
    ins = []
    for c in range(8):
        b, h = c // 2, c % 2
        d = dict(per_half[h])
        d["x_full"] = x[b]
        d["x_rows"] = np.ascontiguousarray(x[b, h * NTK:(h + 1) * NTK])
        ins.append(d)
    res = run_bass_kernel_spmd(nc, ins, core_ids=list(range(8)))
    out = np.empty((B, S, D), np.float32)
    for c in range(8):
        b, h = c // 2, c % 2
        out[b, h * NTK:(h + 1) * NTK] = res.results[c]["y"]
    return out
```
